# Optimizing a Trainium2 kernel written in Bass

```python
import jax
import jax.numpy as jnp
from jax import lax
import numpy as np

D_MODEL = 1024
BATCH = 2
SEQ = 8192
DEPTH = 4

N_EVEN = (DEPTH + 1) // 2
N_ODD = DEPTH // 2

NSA_HEADS = 8
NSA_KV_GROUPS = 2
NSA_QPG = NSA_HEADS // NSA_KV_GROUPS
NSA_HD = 64
NSA_W = NSA_HEADS * NSA_HD
NSA_KV_W = NSA_KV_GROUPS * NSA_HD
CMP_BLOCK = 32
CMP_STRIDE = 16
SLC_BLOCK = 64
N_SELECT = 16
WINDOW = 512
Q_BLOCK = 128
CMP_RATIO = CMP_BLOCK // CMP_STRIDE
SEL_RATIO = SLC_BLOCK // CMP_STRIDE
SEL_AGG_W = tuple(float(w) for w in np.convolve(np.ones(SEL_RATIO), np.ones(CMP_RATIO)))

HG_HEADS = 4
HG_DK = 128
HG_DV = 128
HG_W = HG_HEADS * HG_DV
HG_CHUNK = 64

MIX_W = NSA_W + HG_W
IN_SIZES = (NSA_W,) + (NSA_KV_W,) * 6 + (NSA_HEADS * 3, HG_HEADS * HG_DK, HG_HEADS * HG_DK, HG_W, HG_W)
N_IN = sum(IN_SIZES)
IN_OFFSETS = tuple(int(v) for v in np.cumsum(IN_SIZES)[:-1])

CONV_W = 31
D_CONV = D_MODEL

MOE_GROUPS = 4
MOE_EPG = 8
MOE_EXPERTS = MOE_GROUPS * MOE_EPG
MOE_TOPK = 2
EXPERT_FF = 512
MOE_BLOCK = 128

EPS = 1e-6
NEG = -1e30
FORCE = 1e4

kernel_name = 'hybrid_nsa_hgrn2_conformer_hmoe'


def rms_norm(x, g):
    xf = x.astype(jnp.float32)
    y = xf * lax.rsqrt(jnp.mean(xf * xf, axis=-1, keepdims=True) + EPS)
    return (y * g.astype(jnp.float32)).astype(x.dtype)


def layer_norm(x, g, b):
    xf = x.astype(jnp.float32)
    mu = jnp.mean(xf, axis=-1, keepdims=True)
    var = jnp.mean(jnp.square(xf - mu), axis=-1, keepdims=True)
    y = (xf - mu) * lax.rsqrt(var + EPS) * g.astype(jnp.float32) + b.astype(jnp.float32)
    return y.astype(x.dtype)


def masked_softmax(s, valid):
    p = jax.nn.softmax(jnp.where(valid, s, NEG), axis=-1)
    return p * valid


def nsa_mixer(q, k_cmp, v_cmp, k_slc, v_slc, k_win, v_win, gate_logits, w_ck, w_cv, cmp_pe, q_gain, k_gain):
    B, S = q.shape[0], q.shape[1]
    G, J, HD = NSA_KV_GROUPS, NSA_QPG, NSA_HD
    scale = HD ** -0.5
    out_dtype = q.dtype
    qn = rms_norm(q.reshape(B, S, G, J, HD), q_gain)
    gates = jax.nn.sigmoid(gate_logits.astype(jnp.float32)).reshape(B, S, G, J, 3)

    def kv(t):
        return t.reshape(B, S, G, HD)

    def compress(t, w):
        c = kv(t).reshape(B, S // CMP_STRIDE, CMP_STRIDE, G, HD)
        n = S // CMP_STRIDE - CMP_RATIO + 1
        blocks = jnp.concatenate([c[:, r:r + n] for r in range(CMP_RATIO)], axis=2) + cmp_pe[:, None, :]
        return jnp.einsum('bnlgd,lde->bnge', blocks, w.reshape(CMP_BLOCK, HD, HD))

    kc = rms_norm(compress(k_cmp, w_ck), k_gain[0])
    vc = compress(v_cmp, w_cv)
    n_cmp = kc.shape[1]
    cmp_end = jnp.arange(n_cmp) * CMP_STRIDE + CMP_BLOCK - 1
    n_slc = S // SLC_BLOCK
    n_sel = min(N_SELECT, n_slc)
    ks_blocks = rms_norm(kv(k_slc), k_gain[1]).reshape(B, n_slc, SLC_BLOCK, G, HD).transpose(0, 3, 1, 2, 4)
    vs_blocks = kv(v_slc).reshape(B, n_slc, SLC_BLOCK, G, HD).transpose(0, 3, 1, 2, 4)
    pad_w = ((0, 0), (WINDOW, 0), (0, 0), (0, 0))
    kw_pad = jnp.pad(rms_norm(kv(k_win), k_gain[2]), pad_w)
    vw_pad = jnp.pad(kv(v_win), pad_w)
    b_idx = jnp.arange(B)[:, None, None, None]
    g_idx = jnp.arange(G)[None, :, None, None]
    slc_ids = jnp.arange(n_slc)[None, :]

    def block_fn(i):
        t0 = i * Q_BLOCK
        pos = t0 + jnp.arange(Q_BLOCK)
        qb = lax.dynamic_slice_in_dim(qn, t0, Q_BLOCK, axis=1)
        gb = lax.dynamic_slice_in_dim(gates, t0, Q_BLOCK, axis=1)
        s_c = jnp.einsum('bqgjd,bngd->bgjqn', qb, kc).astype(jnp.float32) * scale
        p_c = masked_softmax(s_c, cmp_end[None, :] <= pos[:, None])
        o_c = jnp.einsum('bgjqn,bngd->bqgjd', p_c.astype(vc.dtype), vc)
        imp = p_c.sum(axis=2)
        imp_pad = jnp.pad(imp, ((0, 0), (0, 0), (0, 0), (CMP_RATIO - 1, SEL_RATIO)))
        imp_s = sum(w * imp_pad[..., o:o + SEL_RATIO * n_slc:SEL_RATIO] for o, w in enumerate(SEL_AGG_W))
        q_blk = (pos // SLC_BLOCK)[:, None]
        forced = (slc_ids == 0) | (slc_ids == q_blk) | (slc_ids == q_blk - 1)
        score = jnp.where(slc_ids > q_blk, NEG, jnp.where(forced, FORCE, imp_s))
        top_val, top_idx = lax.top_k(score, n_sel)
        k_sel = ks_blocks[b_idx, g_idx, top_idx]
        v_sel = vs_blocks[b_idx, g_idx, top_idx]
        key_pos = top_idx[..., None] * SLC_BLOCK + jnp.arange(SLC_BLOCK)
        valid_s = (top_val[..., None] > NEG / 2) & (key_pos <= pos[None, None, :, None, None])
        s_s = jnp.einsum('bqgjd,bgqnld->bgjqnl', qb, k_sel).astype(jnp.float32) * scale
        n_keys = n_sel * SLC_BLOCK
        p_s = masked_softmax(s_s.reshape(B, G, J, Q_BLOCK, n_keys), valid_s.reshape(B, G, 1, Q_BLOCK, n_keys))
        o_s = jnp.einsum('bgjqk,bgqkd->bqgjd', p_s.astype(v_sel.dtype), v_sel.reshape(B, G, Q_BLOCK, n_keys, HD))
        kwb = lax.dynamic_slice_in_dim(kw_pad, t0, Q_BLOCK + WINDOW, axis=1)
        vwb = lax.dynamic_slice_in_dim(vw_pad, t0, Q_BLOCK + WINDOW, axis=1)
        kpos = (t0 - WINDOW + jnp.arange(Q_BLOCK + WINDOW))[None, :]
        valid_w = (kpos >= 0) & (kpos <= pos[:, None]) & (kpos > pos[:, None] - WINDOW)
        s_w = jnp.einsum('bqgjd,bkgd->bgjqk', qb, kwb).astype(jnp.float32) * scale
        p_w = masked_softmax(s_w, valid_w)
        o_w = jnp.einsum('bgjqk,bkgd->bqgjd', p_w.astype(vwb.dtype), vwb)
        return (gb[..., 0:1] * o_c + gb[..., 1:2] * o_s + gb[..., 2:3] * o_w).astype(out_dtype)

    o = lax.map(block_fn, jnp.arange(S // Q_BLOCK))
    return o.transpose(1, 0, 2, 3, 4, 5).reshape(B, S, NSA_W)


def hgrn2_mixer(hq, hf, hi, hg, lb, o_gain):
    B, S = hq.shape[0], hq.shape[1]
    H, DK, DV, C = HG_HEADS, HG_DK, HG_DV, HG_CHUNK
    nc = S // C
    f32 = jnp.float32
    lbh = lb.astype(f32).reshape(H, DK)
    q = jax.nn.silu(hq.astype(f32)).reshape(B, S, H, DK)
    f = lbh + (1.0 - lbh) * jax.nn.sigmoid(hf.astype(f32).reshape(B, S, H, DK))
    log_f = jnp.log(f)
    k = 1.0 - f
    v = hi.astype(f32).reshape(B, S, H, DV)

    def to_chunks(t):
        return t.reshape(B, nc, C, H, t.shape[-1]).transpose(1, 0, 3, 2, 4)

    causal = jnp.tril(jnp.ones((C, C), bool))[:, :, None]

    def chunk_step(state, inp):
        qc, kc, vc, lfc = inp
        b = jnp.cumsum(lfc, axis=2)
        decay = jnp.exp(jnp.where(causal, b[:, :, :, None, :] - b[:, :, None, :, :], -jnp.inf))
        attn = jnp.einsum('bhtd,bhsd,bhtsd->bhts', qc, kc, decay)
        o = attn @ vc + jnp.einsum('bhtd,bhde->bhte', qc * jnp.exp(b), state)
        b_end = b[:, :, -1]
        state = jnp.exp(b_end)[..., None] * state + jnp.einsum('bhsd,bhse->bhde', kc * jnp.exp(b_end[:, :, None] - b), vc)
        return state, o

    state0 = jnp.zeros((B, H, DK, DV), f32)
    _, o = lax.scan(chunk_step, state0, (to_chunks(q), to_chunks(k), to_chunks(v), to_chunks(log_f)))
    o = o.transpose(1, 0, 3, 2, 4).reshape(B, S, H, DV)
    o = rms_norm(o, o_gain) * jax.nn.silu(hg.astype(f32).reshape(B, S, H, DV))
    return o.reshape(B, S, HG_W).astype(hq.dtype)


def even_mixer(h, w_in, w_out, w_ck, w_cv, cmp_pe, q_gain, k_gain, lb, o_gain):
    z = h @ w_in
    q, k_c, v_c, k_s, v_s, k_w, v_w, g_nsa, hq, hf, hi, hg = jnp.split(z, IN_OFFSETS, axis=-1)
    a = nsa_mixer(q, k_c, v_c, k_s, v_s, k_w, v_w, g_nsa, w_ck, w_cv, cmp_pe, q_gain, k_gain)
    r = hgrn2_mixer(hq, hf, hi, hg, lb, o_gain)
    return jnp.concatenate([a, r], axis=-1) @ w_out


def conformer_conv(h, w_pw1, dw, dw_b, ln_g, ln_b, w_pw2):
    u = h @ w_pw1
    a, g = jnp.split(u, 2, axis=-1)
    u = a * jax.nn.sigmoid(g)
    u = lax.conv_general_dilated(u, dw[:, None, :], window_strides=(1,), padding=((CONV_W - 1, 0),),
                                 dimension_numbers=('NWC', 'WIO', 'NWC'), feature_group_count=D_CONV) + dw_b
    u = jax.nn.silu(layer_norm(u, ln_g, ln_b))
    return u @ w_pw2


def hier_moe(h, w_group, w_expert, w1, w3, w2):
    B, S, D = h.shape
    T = B * S
    x = h.reshape(T, D)
    f32 = jnp.float32
    g_logits = (x @ w_group).astype(f32)
    g_sel = jnp.argmax(g_logits, axis=-1)
    g_gate = jnp.take_along_axis(jax.nn.softmax(g_logits, axis=-1), g_sel[:, None], axis=1)
    e_logits = (x @ w_expert).astype(f32).reshape(T, MOE_GROUPS, MOE_EPG)
    e_logits = jnp.take_along_axis(e_logits, g_sel[:, None, None], axis=1)[:, 0]
    e_val, e_loc = lax.top_k(jax.nn.softmax(e_logits, axis=-1), MOE_TOPK)
    gate = g_gate * e_val / jnp.sum(e_val, axis=-1, keepdims=True)
    expert = g_sel[:, None] * MOE_EPG + e_loc
    A = T * MOE_TOPK
    e_flat = expert.reshape(A)
    w_flat = gate.reshape(A)
    tok_flat = jnp.repeat(jnp.arange(T, dtype=jnp.int32), MOE_TOPK)
    order = jnp.argsort(e_flat)
    e_sorted = e_flat[order]
    counts = jnp.bincount(e_flat, length=MOE_EXPERTS)
    starts = jnp.cumsum(counts) - counts
    padded = (counts + MOE_BLOCK - 1) // MOE_BLOCK * MOE_BLOCK
    pends = jnp.cumsum(padded)
    pstarts = pends - padded
    dest = pstarts[e_sorted] + jnp.arange(A) - starts[e_sorted]
    n_rows = A + MOE_EXPERTS * MOE_BLOCK
    n_blocks = n_rows // MOE_BLOCK
    row_tok = jnp.full((n_rows,), T, jnp.int32).at[dest].set(tok_flat[order])
    row_w = jnp.zeros((n_rows,), f32).at[dest].set(w_flat[order])
    blk_exp = jnp.minimum(jnp.searchsorted(pends, jnp.arange(n_blocks) * MOE_BLOCK, side='right'), MOE_EXPERTS - 1)
    x_pad = jnp.concatenate([x, jnp.zeros((1, D), x.dtype)], axis=0)

    def expert_block(args):
        tok, e = args
        xb = x_pad[tok]
        return (jax.nn.silu(xb @ w1[e]) * (xb @ w3[e])) @ w2[e]

    y = lax.map(expert_block, (row_tok.reshape(n_blocks, MOE_BLOCK), blk_exp))
    y = y.reshape(n_rows, D) * row_w[:, None].astype(y.dtype)
    out = jnp.zeros((T + 1, D), y.dtype).at[row_tok].add(y)[:T]
    return out.reshape(B, S, D).astype(h.dtype)


def setup_inputs(seed: int = 0) -> dict:
    key = jax.random.key(seed)
    ks = jax.random.split(key, 26)
    D = D_MODEL

    def nrm(k, shape, scale):
        return jax.random.normal(k, shape, jnp.float32) * scale

    return {
        'x': nrm(ks[0], (BATCH, SEQ, D), 1.0),
        'c': nrm(ks[1], (BATCH, D), 1.0),
        'ada_w': nrm(ks[2], (DEPTH, D, 6 * D), 0.5 * D ** -0.5),
        'ada_b': nrm(ks[3], (DEPTH, 6 * D), 0.02),
        'norm_mix': 1.0 + nrm(ks[4], (DEPTH, D), 0.05),
        'norm_ffn': 1.0 + nrm(ks[5], (DEPTH, D), 0.05),
        'mix_w_in': nrm(ks[6], (N_EVEN, D, N_IN), D ** -0.5),
        'mix_w_out': nrm(ks[7], (N_EVEN, MIX_W, D), MIX_W ** -0.5),
        'nsa_cmp_wk': nrm(ks[8], (N_EVEN, CMP_BLOCK * NSA_HD, NSA_HD), (CMP_BLOCK * NSA_HD) ** -0.5),
        'nsa_cmp_wv': nrm(ks[9], (N_EVEN, CMP_BLOCK * NSA_HD, NSA_HD), (CMP_BLOCK * NSA_HD) ** -0.5),
        'nsa_cmp_pe': nrm(ks[10], (N_EVEN, CMP_BLOCK, NSA_HD), 0.1),
        'nsa_q_gain': 1.0 + nrm(ks[11], (N_EVEN, NSA_HD), 0.05),
        'nsa_k_gain': 1.0 + nrm(ks[12], (N_EVEN, 3, NSA_HD), 0.05),
        'hgrn_lb_logits': nrm(ks[13], (N_EVEN, HG_HEADS * HG_DK), 1.0),
        'hgrn_o_gain': 1.0 + nrm(ks[14], (N_EVEN, HG_DV), 0.05),
        'conv_w_pw1': nrm(ks[15], (N_ODD, D, 2 * D_CONV), D ** -0.5),
        'conv_dw': nrm(ks[16], (N_ODD, CONV_W, D_CONV), CONV_W ** -0.5),
        'conv_dw_b': nrm(ks[17], (N_ODD, D_CONV), 0.02),
        'conv_ln_g': 1.0 + nrm(ks[18], (N_ODD, D_CONV), 0.05),
        'conv_ln_b': nrm(ks[19], (N_ODD, D_CONV), 0.02),
        'conv_w_pw2': nrm(ks[20], (N_ODD, D_CONV, D), D_CONV ** -0.5),
        'moe_w_group': nrm(ks[21], (DEPTH, D, MOE_GROUPS), D ** -0.5),
        'moe_w_expert': nrm(ks[22], (DEPTH, D, MOE_EXPERTS), D ** -0.5),
        'moe_w1': nrm(ks[23], (DEPTH, MOE_EXPERTS, D, EXPERT_FF), D ** -0.5),
        'moe_w3': nrm(ks[24], (DEPTH, MOE_EXPERTS, D, EXPERT_FF), D ** -0.5),
        'moe_w2': nrm(ks[25], (DEPTH, MOE_EXPERTS, EXPERT_FF, D), EXPERT_FF ** -0.5),
    }


def reference(x, c, ada_w, ada_b, norm_mix, norm_ffn, mix_w_in, mix_w_out, nsa_cmp_wk, nsa_cmp_wv, nsa_cmp_pe,
              nsa_q_gain, nsa_k_gain, hgrn_lb_logits, hgrn_o_gain, conv_w_pw1, conv_dw, conv_dw_b, conv_ln_g,
              conv_ln_b, conv_w_pw2, moe_w_group, moe_w_expert, moe_w1, moe_w3, moe_w2):
    lb_p = jax.nn.softmax(hgrn_lb_logits.astype(jnp.float32), axis=0)
    lower_bounds = jnp.cumsum(lb_p, axis=0) - lb_p[0]
    c_act = jax.nn.silu(c)
    for layer in range(DEPTH):
        mod = (c_act @ ada_w[layer] + ada_b[layer])[:, None, :]
        sh_m, sc_m, g_m, sh_f, sc_f, g_f = jnp.split(mod, 6, axis=-1)
        h = rms_norm(x, norm_mix[layer]) * (1.0 + sc_m) + sh_m
        i = layer // 2
        if layer % 2 == 0:
            y = even_mixer(h, mix_w_in[i], mix_w_out[i], nsa_cmp_wk[i], nsa_cmp_wv[i], nsa_cmp_pe[i],
                           nsa_q_gain[i], nsa_k_gain[i], lower_bounds[i], hgrn_o_gain[i])
        else:
            y = conformer_conv(h, conv_w_pw1[i], conv_dw[i], conv_dw_b[i], conv_ln_g[i], conv_ln_b[i], conv_w_pw2[i])
        x = x + g_m * y
        h = rms_norm(x, norm_ffn[layer]) * (1.0 + sc_f) + sh_f
        x = x + g_f * hier_moe(h, moe_w_group[layer], moe_w_expert[layer], moe_w1[layer], moe_w3[layer], moe_w2[layer])
    return x
```

```python
import numpy as np
from contextlib import ExitStack
import concourse.bass as bass
import concourse.mybir as mybir
from concourse.bass_utils import run_bass_kernel_spmd

F32 = mybir.dt.float32
BF16 = mybir.dt.bfloat16
I32 = mybir.dt.int32
AF = mybir.ActivationFunctionType
ALU = mybir.AluOpType
AX = mybir.AxisListType

EPS = 1e-6
NCORES = 8


class Tile:
    __slots__ = ("t", "w", "r", "name", "psum")

    def __init__(self, t, name="", psum=False):
        self.t = t
        self.w = None
        self.r = {}
        self.name = name
        self.psum = psum

    def __getitem__(self, k):
        return self.t[k]


class _Eng:
    def __init__(self, name, eng):
        self.name = name
        self.eng = eng
        self.sem = None
        self.sid = None
        self.count = 0
        self.seen = {}
        self.pending = False


class KB:
    NDMA = 24
    ROT = 3500

    def __init__(self, nc):
        self.nc = nc
        self.es = ExitStack()
        self.nuniq = 0
        self.sems = []
        self.engs = {}
        for name, eng in (("pe", nc.tensor), ("act", nc.scalar), ("dve", nc.vector),
                          ("pool", nc.gpsimd), ("sp", nc.sync)):
            e = _Eng(name, eng)
            self.engs[name] = e
            self._rot(e)
        self.slots = []
        self.qslots = {}
        self.qrr = {}
        for q in ("sp", "pool"):
            self.qslots[q] = []
            self.qrr[q] = 0
            for i in range(self.NDMA // 2):
                sid = self._newsem("dq%s%d" % (q, i))
                sl = [sid, 0]
                self.slots.append(sl)
                self.qslots[q].append(sl)
        self.scopes = []

    def _newsem(self, name):
        self.nuniq += 1
        s = self.es.enter_context(self.nc.semaphore("%s_%d" % (name, self.nuniq)))
        self.sems.append(s)
        return len(self.sems) - 1

    def _rot(self, e):
        e.sid = self._newsem("e_" + e.name)
        e.sem = self.sems[e.sid]
        e.count = 0

    def _stack(self):
        return self.scopes[-1] if self.scopes else self.es

    def sb(self, name, shape, dt):
        self.nuniq += 1
        t = self._stack().enter_context(self.nc.sbuf_tensor("%s_%d" % (name, self.nuniq), list(shape), dt))
        return Tile(t, name)

    def ps(self, name, shape, dt):
        self.nuniq += 1
        t = self._stack().enter_context(self.nc.psum_tensor("%s_%d" % (name, self.nuniq), list(shape), dt))
        return Tile(t, name, psum=True)

    def push(self):
        self.scopes.append(ExitStack())

    def pop(self):
        self.barrier()
        self.scopes.pop().close()

    def _waits(self, E, r, w):
        need = {}

        def add(ev):
            if ev is None:
                return
            s, v = ev
            if need.get(s, 0) < v:
                need[s] = v

        for t in r:
            add(t.w)
            if t.psum:
                for s, v in t.r.items():
                    if s != E.sid:
                        add((s, v))
        for t in w:
            add(t.w)
            for s, v in t.r.items():
                add((s, v))
        for s, v in need.items():
            if s == E.sid and E.name == "pe":
                continue
            if E.seen.get(s, 0) >= v:
                continue
            for F in self.engs.values():
                if F.sid == s:
                    assert v <= F.count, "wait on unsignaled event (%s waits %s)" % (E.name, F.name)
            E.eng.wait_ge(self.sems[s], v)
            E.seen[s] = v

    def _mark(self, ev, r, w):
        s, v = ev
        for t in r:
            if t.r.get(s, 0) < v:
                t.r[s] = v
        for t in w:
            t.w = ev
            t.r = {}

    def op(self, en, fn, r=(), w=(), sig=True):
        E = self.engs[en]
        if E.count >= self.ROT and not E.pending:
            self._rot(E)
        self._waits(E, r, w)
        ins = fn(E.eng)
        if sig:
            E.count += 1
            ins.then_inc(E.sem, 1)
            ev = (E.sid, E.count)
            E.pending = False
        else:
            ev = (E.sid, E.count + 1)
            E.pending = True
        self._mark(ev, r, w)
        return ins

    def pe(self, fn, r=(), w=(), sig=True):
        return self.op("pe", fn, r, w, sig)

    def act(self, fn, r=(), w=(), sig=True):
        return self.op("act", fn, r, w, sig)

    def dve(self, fn, r=(), w=(), sig=True):
        return self.op("dve", fn, r, w, sig)

    def pool(self, fn, r=(), w=(), sig=True):
        return self.op("pool", fn, r, w, sig)

    def dmaf(self, qn, fn, r=(), w=()):
        Q = self.engs[qn]
        self._waits(Q, r, w)
        slot = self.qslots[qn][self.qrr[qn]]
        self.qrr[qn] = (self.qrr[qn] + 1) % len(self.qslots[qn])
        if slot[1] >= self.ROT:
            slot[0] = self._newsem("dq")
            slot[1] = 0
        sid, val = slot
        if val > 0 and Q.seen.get(sid, 0) < val:
            Q.eng.wait_ge(self.sems[sid], val)
            Q.seen[sid] = val
        ins = fn(Q.eng)
        ins.then_inc(self.sems[sid], 16)
        slot[1] = val + 16
        self._mark((sid, slot[1]), r, w)
        return ins

    def dma(self, qn, out, in_, r=(), w=()):
        return self.dmaf(qn, lambda e: e.dma_start(out=out, in_=in_), r, w)

    def barrier(self):
        evs = []
        for F in self.engs.values():
            if F.count > 0:
                evs.append((F.sid, F.count))
        for sid, val in self.slots:
            if val > 0:
                evs.append((sid, val))
        for E in self.engs.values():
            for s, v in evs:
                if s == E.sid:
                    continue
                if E.seen.get(s, 0) >= v:
                    continue
                E.eng.wait_ge(self.sems[s], v)
                E.seen[s] = v

    def finish(self):
        self.barrier()
        while self.scopes:
            self.scopes.pop().close()
        self.es.close()


def _bcast_rows(ap_row, n=128):
    return ap_row.partition_broadcast(n)


C_CAP = 512
NS = C_CAP // 128
NSLOT = 32 * C_CAP
DUMMY = NSLOT


def moe_consts():
    c = np.zeros((128, 416), np.float32)
    c[:, 0:128] = np.eye(128, dtype=np.float32)
    tp = np.arange(128)
    c[:, 128:256] = (tp[:, None] < tp[None, :]).astype(np.float32)
    c[:, 256:384] = 1.0
    c[:, 384:416] = (np.arange(32) * C_CAP)[None, :].astype(np.float32)
    return c


def build_moe_program():
    nc = bass.Bass("TRN2", target_bir_lowering=False)
    T = 2048
    NT = T // 128
    xin = nc.dram_tensor("xin", [T, 1024], F32, kind="ExternalInput").ap()
    mixT = nc.dram_tensor("mixT", [1024, T], F32, kind="ExternalInput").ap()
    wo = nc.dram_tensor("wo", [1024, 1024], F32, kind="ExternalInput").ap()
    modv = nc.dram_tensor("modv", [2, 6, 1024], F32, kind="ExternalInput").ap()
    nrm = nc.dram_tensor("nrm", [1, 1024], F32, kind="ExternalInput").ap()
    wr = nc.dram_tensor("wr", [1024, 36], F32, kind="ExternalInput").ap()
    w1 = nc.dram_tensor("w1", [32, 1024, 512], F32, kind="ExternalInput").ap()
    w3 = nc.dram_tensor("w3", [32, 1024, 512], F32, kind="ExternalInput").ap()
    w2 = nc.dram_tensor("w2", [32, 512, 1024], F32, kind="ExternalInput").ap()
    cst = nc.dram_tensor("cst", [128, 416], F32, kind="ExternalInput").ap()
    xout = nc.dram_tensor("xout", [T, 1024], F32, kind="ExternalOutput").ap()
    Xg = nc.dram_tensor("Xg", [NSLOT + 128, 1024], BF16).ap()
    Yd = nc.dram_tensor("Yd", [NSLOT + 128, 1024], F32).ap()

    K = KB(nc)
    xg_t = Tile(Xg, "Xg")
    yd_t = Tile(Yd, "Yd")
    dram_in = Tile(None, "dram_in")

    cst_f = K.sb("cst_f", [128, 416], F32)
    cst_b = K.sb("cst_b", [128, 384], BF16)
    gf_rep = [K.sb("gf_rep%d" % b, [128, 1024], F32) for b in range(2)]
    xmid = [K.sb("xmid%d" % i, [128, 1024], F32) for i in range(NT)]
    dest_i = K.sb("dest_i", [128, 2 * NT], I32)
    wts = K.sb("wts", [128, 2 * NT], F32)
    wbufs = {}

    def load_expert(e):
        w1b, w3b, w2b = wbufs["w1b"], wbufs["w3b"], wbufs["w2b"]
        j = e % 2
        K.dma("pool", w1b[j][:], w1[e].rearrange("(kc p) f -> p kc f", p=128), w=[w1b[j]])
        K.dma("pool", w3b[j][:], w3[e].rearrange("(kc p) f -> p kc f", p=128), w=[w3b[j]])
        K.dma("pool", w2b[j][:], w2[e].rearrange("(fc p) d -> p fc d", p=128), w=[w2b[j]])

    K.dma("sp", cst_f[:], cst, w=[cst_f])
    K.dma("pool", cst_b[:], cst[:, 0:384], w=[cst_b])
    for b in range(2):
        K.dma("sp", gf_rep[b][:], _bcast_rows(modv[b, 5:6, :]), w=[gf_rep[b]])
    ident_f = cst_f[:, 0:128]
    slotbase = cst_f[:, 384:416]
    ident_b = cst_b[:, 0:128]
    ltri_b = cst_b[:, 128:256]
    ones_b = cst_b[:, 256:384]

    psA = [K.ps("psA%d" % j, [128, 512], F32) for j in range(4)]
    psB = [K.ps("psB%d" % j, [128, 512], F32) for j in range(2)]
    psT = [K.ps("psT%d" % j, [128, 1024], BF16) for j in range(2)]

    K.push()
    mix_sb = K.sb("mix_sb", [128, 8, T], BF16)
    wo_sb = K.sb("wo_sb", [128, 8, 1024], BF16)
    gm_rep2 = [K.sb("gm_rep%d" % b, [128, 1024], F32) for b in range(2)]
    Gf2 = [K.sb("Gf%d" % b, [128, 1024], F32) for b in range(2)]
    shf_rep2 = [K.sb("shf_rep%d" % b, [128, 1024], F32) for b in range(2)]
    tmp_rep = K.sb("tmp_rep", [128, 1024], F32)
    wr_sb = K.sb("wr_sb", [128, 8, 36], F32)
    masks_b = [K.sb("masks_b%d" % i, [128, 32], BF16) for i in range(NT)]
    zero_t = K.sb("zero_t", [128, 1024], F32)
    xt = [K.sb("xt%d" % j, [128, 1024], F32) for j in range(2)]
    ytmp = [K.sb("ytmp%d" % j, [128, 1024], F32) for j in range(2)]
    junk = K.sb("junk", [128, 1024], BF16)
    hf = [K.sb("hf%d" % j, [128, 1024], F32) for j in range(2)]
    hb = [K.sb("hb%d" % j, [128, 1024], BF16) for j in range(2)]
    hT = [K.sb("hT%d" % j, [128, 8, 128], F32) for j in range(2)]
    sm = [K.sb("sm%d" % j, [128, 256], F32) for j in range(2)]

    K.dma("pool", mix_sb[:], mixT.rearrange("(kc p) t -> p kc t", p=128), w=[mix_sb])
    K.dma("pool", wo_sb[:], wo.rearrange("(kc p) n -> p kc n", p=128), w=[wo_sb])
    for b in range(2):
        K.dma("sp", gm_rep2[b][:], _bcast_rows(modv[b, 2:3, :]), w=[gm_rep2[b]])
        K.dma("sp", shf_rep2[b][:], _bcast_rows(modv[b, 3:4, :]), w=[shf_rep2[b]])
        K.dma("sp", tmp_rep[:], _bcast_rows(modv[b, 4:5, :]), w=[tmp_rep])
        K.dma("sp", Gf2[b][:], _bcast_rows(nrm[0:1, :]), w=[Gf2[b]])
        K.dve(lambda e: e.scalar_tensor_tensor(out=Gf2[b][:], in0=tmp_rep[:], scalar=1.0, in1=Gf2[b][:],
                                               op0=ALU.add, op1=ALU.mult), r=[tmp_rep, Gf2[b]], w=[Gf2[b]])
    with nc.allow_non_contiguous_dma(reason="small router weight load"):
        K.dma("sp", wr_sb[:], wr.rearrange("(kc p) n -> p kc n", p=128), w=[wr_sb])
    K.pool(lambda e: e.memset(zero_t[:], 0.0), w=[zero_t])
    K.dma("sp", Yd[NSLOT:NSLOT + 128, :], zero_t[:], r=[zero_t])

    for i in range(NT):
        j = i % 2
        x_i = xmid[i]
        bsel = 0 if i < NT // 2 else 1
        gm_rep, Gf, shf_rep = gm_rep2[bsel], Gf2[bsel], shf_rep2[bsel]
        K.dma("sp", xt[j][:], xin[i * 128:(i + 1) * 128, :], w=[xt[j]])
        for half in range(2):
            pt = psA[half]
            for kc in range(8):
                K.pe(lambda e, kc=kc, half=half, pt=pt: e.matmul(
                    pt[:], mix_sb[:, kc, i * 128:(i + 1) * 128], wo_sb[:, kc, half * 512:(half + 1) * 512],
                    start=(kc == 0), stop=(kc == 7)), r=[mix_sb, wo_sb], w=[pt], sig=(kc == 7))
            K.dve(lambda e, half=half, pt=pt: e.tensor_tensor(
                out=ytmp[j][:, half * 512:(half + 1) * 512], in0=pt[:], in1=gm_rep[:, half * 512:(half + 1) * 512],
                op=ALU.mult), r=[pt, gm_rep], w=[ytmp[j]])
        K.pool(lambda e: e.tensor_tensor(out=x_i[:], in0=xt[j][:], in1=ytmp[j][:], op=ALU.add),
               r=[xt[j], ytmp[j]], w=[x_i])
        s = sm[j]
        K.act(lambda e: e.activation(out=junk[:], in_=x_i[:], func=AF.Square, accum_out=s[:, 0:1]),
              r=[x_i], w=[junk, s])
        K.dve(lambda e: e.tensor_scalar(out=s[:, 1:2], in0=s[:, 0:1], scalar1=1.0 / 1024, scalar2=EPS,
                                        op0=ALU.mult, op1=ALU.add), r=[s], w=[s])
        K.act(lambda e: e.sqrt(out=s[:, 2:3], in_=s[:, 1:2]), r=[s], w=[s])
        K.dve(lambda e: e.reciprocal(out=s[:, 3:4], in_=s[:, 2:3]), r=[s], w=[s])
        K.dve(lambda e: e.scalar_tensor_tensor(out=hf[j][:], in0=x_i[:], scalar=s[:, 3:4], in1=Gf[:],
                                               op0=ALU.mult, op1=ALU.mult), r=[x_i, s, Gf], w=[hf[j]])
        K.pool(lambda e: e.tensor_tensor(out=hf[j][:], in0=hf[j][:], in1=shf_rep[:], op=ALU.add),
               r=[hf[j], shf_rep], w=[hf[j]])
        K.act(lambda e: e.copy(out=hb[j][:], in_=hf[j][:]), r=[hf[j]], w=[hb[j]])
        for g in range(2):
            pt = psB[g]
            for q in range(4):
                kc = g * 4 + q
                K.pe(lambda e, kc=kc, q=q, pt=pt: e.transpose(
                    pt[:, q * 128:(q + 1) * 128], hf[j][:, kc * 128:(kc + 1) * 128], ident_f),
                    r=[hf[j], cst_f], w=[pt], sig=(q == 3))
            K.act(lambda e, g=g, pt=pt: e.copy(out=hT[j][:, g * 4:(g + 1) * 4, :],
                                               in_=pt[:].rearrange("p (a b) -> p a b", a=4)),
                  r=[pt], w=[hT[j]])
        pl = psA[2]
        for kc in range(8):
            K.pe(lambda e, kc=kc: e.matmul(pl[:, 0:36], hT[j][:, kc, :], wr_sb[:, kc, :],
                                           start=(kc == 0), stop=(kc == 7)),
                 r=[hT[j], wr_sb], w=[pl], sig=(kc == 7))
        lg = s[:, 16:52]
        gl = s[:, 16:20]
        el = s[:, 20:52].rearrange("p (g e) -> p g e", g=4)
        K.dve(lambda e: e.tensor_copy(out=lg, in_=pl[:, 0:36]), r=[pl], w=[s])
        c = lambda a, b=None: s[:, a:(a + 1 if b is None else b)]
        GMAX, GSUM, GGATE, L1, L2, DD, ED, DEN, W1, W2 = 4, 5, 6, 7, 8, 9, 10, 11, 12, 13
        OHG = (56, 60)
        GSH = (60, 64)
        ES = (64, 72)
        M1 = (72, 80)
        ES2 = (80, 88)
        M2 = (88, 96)
        MK1 = (96, 128)
        MK2 = (128, 160)
        MKU = (160, 192)
        POSB = (192, 224)
        OK = (224, 256)
        sw = [s]
        D = lambda fn: K.dve(fn, r=sw, w=sw)
        D(lambda e: e.reduce_max(out=c(GMAX), in_=gl, axis=AX.X))
        D(lambda e: e.tensor_scalar(out=c(*OHG), in0=gl, scalar1=c(GMAX), scalar2=None, op0=ALU.is_ge))
        D(lambda e: e.tensor_scalar(out=c(*GSH), in0=gl, scalar1=c(GMAX), scalar2=None, op0=ALU.subtract))
        K.act(lambda e: e.activation(out=c(*GSH), in_=c(*GSH), func=AF.Exp, accum_out=c(GSUM)), r=sw, w=sw)
        D(lambda e: e.reciprocal(out=c(GGATE), in_=c(GSUM)))
        D(lambda e: e.tensor_scalar(out=c(*ES), in0=el[:, 0, :], scalar1=c(OHG[0]), scalar2=None, op0=ALU.mult))
        for g in range(1, 4):
            D(lambda e, g=g: e.scalar_tensor_tensor(out=c(*ES), in0=el[:, g, :], scalar=c(OHG[0] + g),
                                                    in1=c(*ES), op0=ALU.mult, op1=ALU.add))
        D(lambda e: e.reduce_max(out=c(L1), in_=c(*ES), axis=AX.X))
        D(lambda e: e.tensor_scalar(out=c(*M1), in0=c(*ES), scalar1=c(L1), scalar2=None, op0=ALU.is_ge))
        D(lambda e: e.scalar_tensor_tensor(out=c(*ES2), in0=c(*M1), scalar=-1e30, in1=c(*ES),
                                           op0=ALU.mult, op1=ALU.add))
        D(lambda e: e.reduce_max(out=c(L2), in_=c(*ES2), axis=AX.X))
        D(lambda e: e.tensor_scalar(out=c(*M2), in0=c(*ES2), scalar1=c(L2), scalar2=None, op0=ALU.is_ge))
        D(lambda e: e.tensor_tensor(out=c(DD), in0=c(L2), in1=c(L1), op=ALU.subtract))
        K.act(lambda e: e.activation(out=c(ED), in_=c(DD), func=AF.Exp), r=sw, w=sw)
        D(lambda e: e.tensor_scalar(out=c(DEN), in0=c(ED), scalar1=1.0, scalar2=None, op0=ALU.add))
        D(lambda e: e.reciprocal(out=c(DEN), in_=c(DEN)))
        D(lambda e: e.tensor_tensor(out=c(W1), in0=c(GGATE), in1=c(DEN), op=ALU.mult))
        D(lambda e: e.tensor_tensor(out=c(W2), in0=c(W1), in1=c(ED), op=ALU.mult))
        ohg3 = c(*OHG).unsqueeze(2).to_broadcast([128, 4, 8])
        for (MK, MM) in ((MK1, M1), (MK2, M2)):
            D(lambda e, MK=MK, MM=MM: e.tensor_tensor(
                out=c(*MK).rearrange("p (g e) -> p g e", g=4), in0=ohg3,
                in1=c(*MM).unsqueeze(1).to_broadcast([128, 4, 8]), op=ALU.mult))
        D(lambda e: e.tensor_tensor(out=c(*MKU), in0=c(*MK1), in1=c(*MK2), op=ALU.add))
        K.dve(lambda e: e.tensor_copy(out=masks_b[i][:], in_=c(*MKU)), r=sw, w=[masks_b[i]])
        pp = psA[3]
        for i2 in range(i + 1):
            K.pe(lambda e, i2=i2: e.matmul(pp[:, 0:32], (ones_b if i2 < i else ltri_b), masks_b[i2][:],
                                           start=(i2 == 0), stop=(i2 == i)),
                 r=[cst_b, masks_b[i2]], w=[pp], sig=(i2 == i))
        K.dve(lambda e: e.tensor_tensor(out=c(*POSB), in0=pp[:, 0:32], in1=slotbase, op=ALU.add),
              r=[pp, cst_f], w=sw)
        K.dve(lambda e: e.tensor_single_scalar(out=c(*OK), in_=pp[:, 0:32], scalar=C_CAP - 0.5, op=ALU.is_lt),
              r=[pp], w=sw)
        for k, (MK, WW) in enumerate(((MK1, W1), (MK2, W2))):
            D(lambda e, MK=MK: e.tensor_tensor(out=c(*MK), in0=c(*MK), in1=c(*OK), op=ALU.mult))
            D(lambda e, MK=MK: e.reduce_sum(out=c(14), in_=c(*MK), axis=AX.X))
            D(lambda e, MK=MK: e.tensor_tensor(out=c(*MK), in0=c(*MK), in1=c(*POSB), op=ALU.mult))
            D(lambda e, MK=MK: e.reduce_sum(out=c(15), in_=c(*MK), axis=AX.X))
            D(lambda e: e.scalar_tensor_tensor(out=c(15), in0=c(14), scalar=-float(DUMMY), in1=c(15),
                                               op0=ALU.mult, op1=ALU.add))
            D(lambda e: e.tensor_scalar(out=c(15), in0=c(15), scalar1=float(DUMMY), scalar2=None, op0=ALU.add))
            K.dve(lambda e, k=k: e.tensor_copy(out=dest_i[:, 2 * i + k:2 * i + k + 1], in_=c(15)),
                  r=sw, w=[dest_i])
            K.dve(lambda e, k=k, WW=WW: e.tensor_tensor(out=wts[:, 2 * i + k:2 * i + k + 1], in0=c(WW), in1=c(14),
                                                        op=ALU.mult), r=sw, w=[wts])
            K.dmaf("pool", lambda e, k=k: e.indirect_dma_start(
                out=Xg, out_offset=bass.IndirectOffsetOnAxis(ap=dest_i[:, 2 * i + k:2 * i + k + 1], axis=0),
                in_=hb[j][:], in_offset=None), r=[dest_i, hb[j]])
    K.pop()

    K.push()
    w1b = [K.sb("w1b%d" % j, [128, 8, 512], BF16) for j in range(2)]
    w3b = [K.sb("w3b%d" % j, [128, 8, 512], BF16) for j in range(2)]
    w2b = [K.sb("w2b%d" % j, [128, 4, 1024], BF16) for j in range(2)]
    wbufs.update(w1b=w1b, w3b=w3b, w2b=w2b)
    load_expert(0)
    load_expert(1)
    xe = [K.sb("xe%d" % j, [128, NS, 1024], BF16) for j in range(2)]
    xeT = [K.sb("xeT%d" % j, [128, 8, C_CAP], BF16) for j in range(2)]
    gact = [K.sb("gact%d" % j, [128, C_CAP], F32) for j in range(2)]
    actT = [K.sb("actT%d" % j, [128, 4, C_CAP], BF16) for j in range(2)]
    ye = [K.sb("ye%d" % j, [128, 1024], F32) for j in range(3)]
    yec = 0
    for ex in range(32):
        j = ex % 2
        K.dma("sp", xe[j][:], Xg[ex * C_CAP:(ex + 1) * C_CAP, :].rearrange("(s p) d -> p s d", p=128),
              w=[xe[j]])
        for s_ in range(NS):
            pt = psT[s_ % 2]
            for kc in range(8):
                K.pe(lambda e: e.transpose(
                    pt[:, kc * 128:(kc + 1) * 128], xe[j][:, s_, kc * 128:(kc + 1) * 128], ident_b),
                    r=[xe[j], cst_b], w=[pt], sig=(kc == 7))
            K.act(lambda e: e.copy(out=xeT[j][:, :, s_ * 128:(s_ + 1) * 128],
                                   in_=pt[:].rearrange("p (a b) -> p a b", a=8)),
                  r=[pt], w=[xeT[j]])
        for fc in range(4):
            ph1 = psA[fc % 2]
            ph3 = psA[2 + fc % 2]
            for (ph, wb) in ((ph1, w1b[j]), (ph3, w3b[j])):
                for kc in range(8):
                    K.pe(lambda e: e.matmul(
                        ph[:, 0:C_CAP], wb[:, kc, fc * 128:(fc + 1) * 128], xeT[j][:, kc, :],
                        start=(kc == 0), stop=(kc == 7)), r=[wb, xeT[j]], w=[ph], sig=(kc == 7))
            ga = gact[fc % 2]
            K.act(lambda e: e.activation(out=ga[:], in_=ph1[:, 0:C_CAP], func=AF.Silu), r=[ph1], w=[ga])
            K.dve(lambda e: e.tensor_tensor(out=actT[j][:, fc, :], in0=ga[:], in1=ph3[:, 0:C_CAP],
                                            op=ALU.mult), r=[ph3, ga], w=[actT[j]])
        for s_ in range(NS):
            yt = ye[yec % 3]
            yec += 1
            for dh in range(2):
                py = psB[dh]
                for fc in range(4):
                    K.pe(lambda e: e.matmul(
                        py[:], actT[j][:, fc, s_ * 128:(s_ + 1) * 128], w2b[j][:, fc, dh * 512:(dh + 1) * 512],
                        start=(fc == 0), stop=(fc == 3)), r=[actT[j], w2b[j]], w=[py], sig=(fc == 3))
                if dh == 0:
                    K.act(lambda e: e.copy(out=yt[:, 0:512], in_=py[:]), r=[py], w=[yt])
                else:
                    K.dve(lambda e: e.tensor_copy(out=yt[:, 512:1024], in_=py[:]), r=[py], w=[yt])
            r0 = ex * C_CAP + s_ * 128
            K.dma("sp", Yd[r0:r0 + 128, :], yt[:], r=[yt])
        if ex + 2 < 32:
            load_expert(ex + 2)
    K.pop()

    K.push()
    y1 = [K.sb("y1_%d" % j, [128, 1024], F32) for j in range(2)]
    y2 = [K.sb("y2_%d" % j, [128, 1024], F32) for j in range(2)]
    xo = [K.sb("xo_%d" % j, [128, 1024], F32) for j in range(2)]
    for i in range(NT):
        j = i % 2
        for k, yy in enumerate((y1[j], y2[j])):
            K.dmaf("pool", lambda e, k=k, yy=yy: e.indirect_dma_start(
                out=yy[:], out_offset=None, in_=Yd,
                in_offset=bass.IndirectOffsetOnAxis(ap=dest_i[:, 2 * i + k:2 * i + k + 1], axis=0)),
                r=[dest_i], w=[yy])
        K.dve(lambda e: e.tensor_scalar(out=y1[j][:], in0=y1[j][:], scalar1=wts[:, 2 * i:2 * i + 1], scalar2=None,
                                        op0=ALU.mult), r=[y1[j], wts], w=[y1[j]])
        K.dve(lambda e: e.scalar_tensor_tensor(out=y2[j][:], in0=y2[j][:], scalar=wts[:, 2 * i + 1:2 * i + 2],
                                               in1=y1[j][:], op0=ALU.mult, op1=ALU.add),
              r=[y1[j], y2[j], wts], w=[y2[j]])
        K.pool(lambda e: e.tensor_tensor(out=y2[j][:], in0=y2[j][:], in1=gf_rep[0 if i < NT // 2 else 1][:], op=ALU.mult),
               r=[y2[j], gf_rep[0 if i < NT // 2 else 1]], w=[y2[j]])
        K.pool(lambda e: e.tensor_tensor(out=xo[j][:], in0=y2[j][:], in1=xmid[i][:], op=ALU.add),
               r=[y2[j], xmid[i]], w=[xo[j]])
        K.dma("sp", xout[i * 128:(i + 1) * 128, :], xo[j][:], r=[xo[j]])
    K.pop()
    K.finish()
    return nc


_PROGS = {}


def _prog(name, builder):
    if name not in _PROGS:
        _PROGS[name] = builder()
    return _PROGS[name]


def _f32(a):
    return np.ascontiguousarray(a, dtype=np.float32)


def launch_moe(x, mix, w_o, mod_l, nrm_l, w_group, w_expert, w1, w3, w2):
    nc = _prog("moe", build_moe_program)
    wr = _f32(np.concatenate([w_group, w_expert], axis=1))
    cst = moe_consts()
    w_o = _f32(w_o)
    w1 = _f32(w1)
    w3 = _f32(w3)
    w2 = _f32(w2)
    nrm = _f32(nrm_l).reshape(1, 1024)
    xf = x.reshape(128, 128, 1024)
    mf = mix.reshape(128, 128, 1024)
    modv = _f32(mod_l.reshape(2, 6, 1024))
    in_maps = []
    for c in range(NCORES):
        in_maps.append({
            "xin": _f32(xf[c::8].reshape(2048, 1024)),
            "mixT": _f32(mf[c::8].reshape(2048, 1024).T),
            "wo": w_o, "modv": modv,
            "nrm": nrm, "wr": wr, "w1": w1, "w3": w3, "w2": w2, "cst": cst,
        })
    res = run_bass_kernel_spmd(nc, in_maps, core_ids=list(range(NCORES)))
    out = np.empty((128, 128, 1024), np.float32)
    for c in range(NCORES):
        out[c::8] = res.results[c]["xout"].reshape(16, 128, 1024)
    out = out.reshape(2, 8192, 1024)
    return out


def build_mod_program():
    nc = bass.Bass("TRN2", target_bir_lowering=False)
    cT = nc.dram_tensor("cT", [128, 16], F32, kind="ExternalInput").ap()
    aw = nc.dram_tensor("aw", [4, 1024, 768], F32, kind="ExternalInput").ap()
    ab = nc.dram_tensor("ab", [1, 3072], F32, kind="ExternalInput").ap()
    mo = nc.dram_tensor("mo", [2, 3072], F32, kind="ExternalOutput").ap()
    K = KB(nc)
    c_sb = K.sb("c_sb", [128, 16], F32)
    ca = K.sb("ca", [128, 16], F32)
    bias = K.sb("bias", [2, 3072], F32)
    osb = K.sb("osb", [2, 3072], F32)
    wsb = [K.sb("wsb%d" % j, [128, 8, 768], F32) for j in range(2)]
    ps = [K.ps("ps%d" % j, [128, 512], F32) for j in range(4)]
    K.dma("sp", c_sb[:], cT, w=[c_sb])
    K.dma("sp", bias[:], ab.partition_broadcast(2), w=[bias])
    K.act(lambda e: e.activation(out=ca[:], in_=c_sb[:], func=AF.Silu), r=[c_sb], w=[ca])
    for l in range(4):
        w = wsb[l % 2]
        K.dma("sp", w[:], aw[l].rearrange("(kc p) n -> p kc n", p=128), w=[w])
        for h_, (n0, n1) in enumerate(((0, 512), (512, 768))):
            p = ps[(2 * l + h_) % 4]
            for kc in range(8):
                K.pe(lambda e: e.matmul(p[0:2, 0:n1 - n0], ca[:, 2 * kc:2 * kc + 2], w[:, kc, n0:n1],
                                        start=(kc == 0), stop=(kc == 7)), r=[ca, w], w=[p], sig=(kc == 7))
            K.dve(lambda e: e.tensor_tensor(out=osb[:, l * 768 + n0:l * 768 + n1], in0=p[0:2, 0:n1 - n0],
                                            in1=bias[:, l * 768 + n0:l * 768 + n1], op=ALU.add),
                  r=[p, bias], w=[osb])
    K.dma("sp", mo, osb[:], r=[osb])
    K.finish()
    return nc


def launch_mod(c, ada_w, ada_b):
    nc = _prog("mod", build_mod_program)
    cT = _f32(c.T.reshape(8, 128, 2).transpose(1, 0, 2).reshape(128, 16))
    in_maps = []
    for k in range(NCORES):
        sl = slice(k * 768, (k + 1) * 768)
        in_maps.append({"cT": cT, "aw": _f32(ada_w[:, :, sl]), "ab": _f32(ada_b[:, sl].reshape(1, 3072))})
    res = run_bass_kernel_spmd(nc, in_maps, core_ids=list(range(NCORES)))
    mod = np.empty((4, 2, 6144), np.float32)
    for k in range(NCORES):
        o = res.results[k]["mo"].reshape(2, 4, 768)
        mod[:, :, k * 768:(k + 1) * 768] = o.transpose(1, 0, 2)
    return mod


def emit_norm_T(K, x_t, G, SH, junk, sm, hf, hb, psT, ident_b, cst_t, hT_out_ap, hT_tile):
    K.act(lambda e: e.activation(out=junk[:], in_=x_t[:], func=AF.Square, accum_out=sm[:, 0:1]),
          r=[x_t], w=[junk, sm])
    K.dve(lambda e: e.tensor_scalar(out=sm[:, 1:2], in0=sm[:, 0:1], scalar1=1.0 / 1024, scalar2=EPS,
                                    op0=ALU.mult, op1=ALU.add), r=[sm], w=[sm])
    K.act(lambda e: e.sqrt(out=sm[:, 2:3], in_=sm[:, 1:2]), r=[sm], w=[sm])
    K.dve(lambda e: e.reciprocal(out=sm[:, 3:4], in_=sm[:, 2:3]), r=[sm], w=[sm])
    K.dve(lambda e: e.scalar_tensor_tensor(out=hf[:], in0=x_t[:], scalar=sm[:, 3:4], in1=G[:],
                                           op0=ALU.mult, op1=ALU.mult), r=[x_t, sm, G], w=[hf])
    K.pool(lambda e: e.tensor_tensor(out=hb[:], in0=hf[:], in1=SH[:], op=ALU.add), r=[hf, SH], w=[hb])
    for kc in range(8):
        K.pe(lambda e: e.transpose(psT[:, kc * 128:(kc + 1) * 128], hb[:, kc * 128:(kc + 1) * 128], ident_b),
             r=[hb, cst_t], w=[psT], sig=(kc == 7))
    K.act(lambda e: e.copy(out=hT_out_ap, in_=psT[:].rearrange("p (a b) -> p a b", a=8)),
          r=[psT], w=[hT_tile])


def emit_mod_setup(K, modv, nrm, G, SH, tmp):
    K.dma("sp", SH[:], _bcast_rows(modv[0:1, :]), w=[SH])
    K.dma("sp", tmp[:], _bcast_rows(modv[1:2, :]), w=[tmp])
    K.dma("sp", G[:], _bcast_rows(nrm[0:1, :]), w=[G])
    K.dve(lambda e: e.scalar_tensor_tensor(out=G[:], in0=tmp[:], scalar=1.0, in1=G[:],
                                           op0=ALU.add, op1=ALU.mult), r=[tmp, G], w=[G])


def hgrn_consts():
    c = np.zeros((128, 128 + 512 + 64), np.float32)
    c[:, 0:128] = np.eye(128, dtype=np.float32)
    m = np.ones(512, np.float32)
    m[0::64] = 0.0
    c[:, 128:640] = m[None, :]
    s = np.arange(64)
    c[0:64, 640:704] = (s[:, None] <= s[None, :]).astype(np.float32)
    return c


class _Stop(Exception):
    pass


def build_hgrn_program(S=8192, dbg=None):
    nc = bass.Bass("TRN2", target_bir_lowering=False)
    x = nc.dram_tensor("x", [S, 1024], F32, kind="ExternalInput").ap()
    modv = nc.dram_tensor("modv", [6, 1024], F32, kind="ExternalInput").ap()
    nrm = nc.dram_tensor("nrm", [1, 1024], F32, kind="ExternalInput").ap()
    wqf = nc.dram_tensor("wqf", [1024, 256], F32, kind="ExternalInput").ap()
    wig = nc.dram_tensor("wig", [1024, 256], F32, kind="ExternalInput").ap()
    lbl = nc.dram_tensor("lbl", [128, 4], F32, kind="ExternalInput").ap()
    og = nc.dram_tensor("og", [1, 128], F32, kind="ExternalInput").ap()
    cst = nc.dram_tensor("cst", [128, 704], F32, kind="ExternalInput").ap()
    ro = nc.dram_tensor("ro", [S, 128], F32, kind="ExternalOutput").ap()
    K = KB(nc)

    def ck(k):
        if dbg == k:
            raise _Stop()
    cst_f = K.sb("cst_f", [128, 704], F32)
    cst_b = K.sb("cst_b", [128, 128], BF16)
    G = K.sb("G", [128, 1024], F32)
    SH = K.sb("SH", [128, 1024], F32)
    tmp = K.sb("tmp", [128, 1024], F32)
    wqf_sb = K.sb("wqf_sb", [128, 8, 256], BF16)
    wig_sb = K.sb("wig_sb", [128, 8, 256], BF16)
    lb_sb = K.sb("lb_sb", [128, 16], F32)
    og_rep = K.sb("og_rep", [64, 128], F32)
    state = K.sb("state", [128, 128], F32)
    state_b = K.sb("state_b", [128, 128], BF16)
    def early(k):
        if dbg == k:
            K.finish()
            return True
        return False
    K.dma("sp", cst_f[:], cst, w=[cst_f])
    K.dma("pool", cst_b[:], cst[:, 0:128], w=[cst_b])
    if early(-1):
        return nc
    with nc.allow_non_contiguous_dma(reason="weight slices"):
        K.dma("pool", wqf_sb[:], wqf.rearrange("(kc p) n -> p kc n", p=128), w=[wqf_sb])
        K.dma("pool", wig_sb[:], wig.rearrange("(kc p) n -> p kc n", p=128), w=[wig_sb])
    if early(-2):
        return nc
    K.dma("sp", lb_sb[:, 0:4], lbl, w=[lb_sb])
    if early(-3):
        return nc
    K.dma("sp", og_rep[:], og.partition_broadcast(64), w=[og_rep])
    if early(-4):
        return nc
    emit_mod_setup(K, modv, nrm, G, SH, tmp)
    if early(-5):
        return nc
    ident_b = cst_b[:, 0:128]
    rmask = cst_f[:, 128:640]
    triT = cst_f[0:64, 640:704]
    L = lambda a, b=None: lb_sb[:, a:(a + 1 if b is None else b)]
    lw = [lb_sb]
    K.dve(lambda e: e.tensor_tensor(out=L(4), in0=L(1), in1=L(0), op=ALU.subtract), r=lw, w=lw)
    K.act(lambda e: e.activation(out=L(5), in_=L(4), func=AF.Exp), r=lw, w=lw)
    K.dve(lambda e: e.tensor_scalar(out=L(5), in0=L(5), scalar1=1.0, scalar2=None, op0=ALU.add), r=lw, w=lw)
    K.dve(lambda e: e.reciprocal(out=L(6), in_=L(5)), r=lw, w=lw)
    K.dve(lambda e: e.tensor_scalar(out=L(7), in0=L(6), scalar1=-1.0, scalar2=1.0, op0=ALU.mult, op1=ALU.add),
          r=lw, w=lw)
    K.dve(lambda e: e.tensor_tensor(out=L(8), in0=L(2), in1=L(6), op=ALU.mult), r=lw, w=lw)
    K.dve(lambda e: e.tensor_tensor(out=L(9), in0=L(3), in1=L(7), op=ALU.mult), r=lw, w=lw)
    K.dve(lambda e: e.tensor_tensor(out=L(8), in0=L(8), in1=L(9), op=ALU.add), r=lw, w=lw)
    K.dve(lambda e: e.tensor_tensor(out=L(10), in0=L(8), in1=L(6), op=ALU.subtract), r=lw, w=lw)
    K.dve(lambda e: e.tensor_scalar(out=L(11), in0=L(10), scalar1=-1.0, scalar2=1.0, op0=ALU.mult, op1=ALU.add),
          r=lw, w=lw)
    LB, OML = L(10), L(11)
    if early(-6):
        return nc
    K.dve(lambda e: e.memset(state[:], 0.0), w=[state])
    K.dve(lambda e: e.memset(state_b[:], 0.0), w=[state_b])

    psT = [K.ps("psT%d" % j, [128, 1024], BF16) for j in range(2)]
    psP = [K.ps("psP%d" % j, [128, 512], F32) for j in range(4)]
    psO = [K.ps("psO%d" % j, [128, 512], F32) for j in range(2)]

    xt = [K.sb("xt%d" % j, [128, 1024], F32) for j in range(2)]
    junk = K.sb("junk", [128, 1024], BF16)
    sm = [K.sb("sm%d" % j, [128, 8], F32) for j in range(2)]
    hf = [K.sb("hf%d" % j, [128, 1024], F32) for j in range(2)]
    hb = [K.sb("hb%d" % j, [128, 1024], BF16) for j in range(2)]
    hT = [K.sb("hT%d" % j, [128, 8, 512], BF16) for j in range(2)]
    qf = K.sb("qf", [128, 512], F32)
    ff = K.sb("ff", [128, 512], F32)
    lf = K.sb("lf", [128, 512], F32)
    kf = K.sb("kf", [128, 512], F32)
    bc = K.sb("bc", [128, 512], F32)
    e1 = K.sb("e1", [128, 512], F32)
    e2 = K.sb("e2", [128, 512], F32)
    Qt = [K.sb("Qt%d" % j, [128, 512], BF16) for j in range(2)]
    Kt = [K.sb("Kt%d" % j, [128, 512], BF16) for j in range(2)]
    Qh = [K.sb("Qh%d" % j, [128, 512], BF16) for j in range(2)]
    Kh = [K.sb("Kh%d" % j, [128, 512], BF16) for j in range(2)]
    ebe = [K.sb("ebe%d" % j, [128, 8], F32) for j in range(2)]
    vi = [K.sb("vi%d" % j, [64, 8, 128], BF16) for j in range(2)]
    gs = [K.sb("gs%d" % j, [64, 8, 128], F32) for j in range(2)]
    KhT = [K.sb("KhT%d" % j, [64, 8, 128], BF16) for j in range(2)]
    att = [K.sb("att%d" % j, [64, 8, 64], BF16) for j in range(2)]
    rsb = [K.sb("rsb%d" % j, [64, 8, 128], F32) for j in range(2)]
    so = [K.sb("so%d" % j, [64, 8], F32) for j in range(2)]
    osb = [K.sb("osb%d" % j, [64, 8, 128], F32) for j in range(2)]
    attf = K.sb("attf", [64, 512], F32)

    NB = S // 512
    try:
      ck(0)
      for blk in range(NB):
          jb = blk % 2
          for ti in range(4):
              g = blk * 4 + ti
              j = g % 2
              K.dma("sp", xt[j][:], x[g * 128:(g + 1) * 128, :], w=[xt[j]])
              emit_norm_T(K, xt[j], G, SH, junk, sm[j], hf[j], hb[j], psT[j], ident_b, cst_b,
                          hT[jb][:, :, ti * 128:(ti + 1) * 128], hT[jb])
          ck(1)
          pq, pf = psP[0], psP[1]
          for (pp, c0) in ((pq, 0), (pf, 128)):
              for kc in range(8):
                  K.pe(lambda e: e.matmul(pp[:], wqf_sb[:, kc, c0:c0 + 128], hT[jb][:, kc, :],
                                          start=(kc == 0), stop=(kc == 7)), r=[wqf_sb, hT[jb]], w=[pp], sig=(kc == 7))
          ck(11)
          for rnd in range(4):
              pv = psP[2 + rnd % 2]
              for cc in range(2):
                  c = rnd * 2 + cc
                  for kc in range(8):
                      K.pe(lambda e: e.matmul(pv[0:64, cc * 256:(cc + 1) * 256], hT[jb][:, kc, c * 64:(c + 1) * 64],
                                              wig_sb[:, kc, :], start=(kc == 0), stop=(kc == 7)),
                           r=[wig_sb, hT[jb]], w=[pv], sig=(kc == 7 and cc == 1))
              ck(12)
              ck(20 + rnd * 3)
              pv3 = pv[0:64, :].rearrange("p (c n) -> p c n", c=2)
              K.dve(lambda e: e.tensor_copy(out=vi[jb][:, rnd * 2:rnd * 2 + 2, :], in_=pv3[:, :, 0:128]),
                    r=[pv], w=[vi[jb]])
              ck(13)
              ck(21 + rnd * 3)
              K.dve(lambda e: e.tensor_copy(out=gs[jb][:, rnd * 2:rnd * 2 + 2, :], in_=pv3[:, :, 128:256]),
                    r=[pv], w=[gs[jb]])
              ck(14)
              ck(22 + rnd * 3)
          K.act(lambda e: e.activation(out=gs[jb][:], in_=gs[jb][:], func=AF.Silu), r=[gs[jb]], w=[gs[jb]])
          ck(15)
          K.dve(lambda e: e.tensor_tensor(out=gs[jb][:], in0=gs[jb][:],
                                          in1=og_rep[:].unsqueeze(1).to_broadcast([64, 8, 128]), op=ALU.mult),
                r=[gs[jb], og_rep], w=[gs[jb]])
          ck(2)
          K.act(lambda e: e.activation(out=qf[:], in_=pq[:], func=AF.Silu), r=[pq], w=[qf])
          K.act(lambda e: e.activation(out=ff[:], in_=pf[:], func=AF.Sigmoid), r=[pf], w=[ff])
          K.dve(lambda e: e.tensor_scalar(out=ff[:], in0=ff[:], scalar1=OML, scalar2=LB, op0=ALU.mult, op1=ALU.add),
                r=[ff, lb_sb], w=[ff])
          K.act(lambda e: e.activation(out=lf[:], in_=ff[:], func=AF.Ln), r=[ff], w=[lf])
          K.dve(lambda e: e.tensor_scalar(out=kf[:], in0=ff[:], scalar1=-1.0, scalar2=1.0, op0=ALU.mult, op1=ALU.add),
                r=[ff], w=[kf])
          K.dve(lambda e: e.tensor_tensor_scan(out=bc[:], data0=rmask, data1=lf[:], initial=0.0,
                                               op0=ALU.mult, op1=ALU.add), r=[lf, cst_f], w=[bc])
          b3 = bc[:].rearrange("p (c t) -> p c t", c=8)
          bm = b3[:, :, 31:32].to_broadcast([128, 8, 64])
          be = b3[:, :, 63:64].to_broadcast([128, 8, 64])
          v3 = lambda t: t[:].rearrange("p (c t) -> p c t", c=8)
          K.dve(lambda e: e.tensor_tensor(out=v3(e1), in0=b3, in1=bm, op=ALU.subtract), r=[bc], w=[e1])
          K.act(lambda e: e.activation(out=e1[:], in_=e1[:], func=AF.Exp), r=[e1], w=[e1])
          K.dve(lambda e: e.tensor_tensor(out=Qt[jb][:], in0=qf[:], in1=e1[:], op=ALU.mult), r=[qf, e1], w=[Qt[jb]])
          K.dve(lambda e: e.reciprocal(out=e1[:], in_=e1[:]), r=[e1], w=[e1])
          K.dve(lambda e: e.tensor_tensor(out=Kt[jb][:], in0=kf[:], in1=e1[:], op=ALU.mult), r=[kf, e1], w=[Kt[jb]])
          K.act(lambda e: e.activation(out=e2[:], in_=bc[:], func=AF.Exp), r=[bc], w=[e2])
          K.dve(lambda e: e.tensor_tensor(out=Qh[jb][:], in0=qf[:], in1=e2[:], op=ALU.mult), r=[qf, e2], w=[Qh[jb]])
          K.act(lambda e: e.activation(out=ebe[jb][:].unsqueeze(2), in_=b3[:, :, 63:64], func=AF.Exp),
                r=[bc], w=[ebe[jb]])
          K.dve(lambda e: e.tensor_tensor(out=v3(e2), in0=be, in1=b3, op=ALU.subtract), r=[bc], w=[e2])
          K.act(lambda e: e.activation(out=e2[:], in_=e2[:], func=AF.Exp), r=[e2], w=[e2])
          K.dve(lambda e: e.tensor_tensor(out=Kh[jb][:], in0=kf[:], in1=e2[:], op=ALU.mult), r=[kf, e2], w=[Kh[jb]])
          ck(3)
          pT = psT[jb]
          for c in range(8):
              K.pe(lambda e: e.transpose(pT[0:64, c * 128:(c + 1) * 128], Kh[jb][:, c * 64:(c + 1) * 64], ident_b),
                   r=[Kh[jb], cst_b], w=[pT], sig=(c == 7))
          K.act(lambda e: e.copy(out=KhT[jb][:], in_=pT[0:64, :].rearrange("p (c n) -> p c n", c=8)),
                r=[pT], w=[KhT[jb]])
          ck(4)
          pa = psP[0]
          for c in range(8):
              K.pe(lambda e: e.matmul(pa[0:64, c * 64:(c + 1) * 64], Kt[jb][:, c * 64:(c + 1) * 64],
                                      Qt[jb][:, c * 64:(c + 1) * 64], start=True, stop=True),
                   r=[Kt[jb], Qt[jb]], w=[pa], sig=(c == 7))
          K.dve(lambda e: e.tensor_scalar(out=attf[:], in0=pa[0:64, :], scalar1=1e30, scalar2=-1e30,
                                          op0=ALU.min, op1=ALU.max), r=[pa], w=[attf])
          K.dve(lambda e: e.tensor_tensor(out=att[jb][:], in0=attf[:].rearrange("p (c t) -> p c t", c=8),
                                          in1=triT.unsqueeze(1).to_broadcast([64, 8, 64]), op=ALU.mult),
                r=[attf, cst_f], w=[att[jb]])
          ck(5)
          for c in range(8):
              po = psO[c % 2]
              K.pe(lambda e: e.matmul(po[0:64, 0:128], att[jb][:, c, :], vi[jb][:, c, :], start=True, stop=False),
                   r=[att[jb], vi[jb]], w=[po], sig=False)
              K.pe(lambda e: e.matmul(po[0:64, 0:128], Qh[jb][:, c * 64:(c + 1) * 64], state_b[:], start=False, stop=True),
                   r=[Qh[jb], state_b], w=[po])
              pst = psP[1] if c % 2 == 0 else psP[2]
              K.pe(lambda e: e.matmul(pst[:, 0:128], KhT[jb][:, c, :], vi[jb][:, c, :], start=True, stop=True),
                   r=[KhT[jb], vi[jb]], w=[pst])
              K.dve(lambda e: e.scalar_tensor_tensor(out=state[:], in0=state[:], scalar=ebe[jb][:, c:c + 1],
                                                     in1=pst[:, 0:128], op0=ALU.mult, op1=ALU.add),
                    r=[state, ebe[jb], pst], w=[state])
              K.act(lambda e: e.copy(out=state_b[:], in_=state[:]), r=[state], w=[state_b])
              K.act(lambda e: e.copy(out=osb[jb][:, c, :], in_=po[0:64, 0:128]), r=[po], w=[osb[jb]])
          ck(6)
          s_ = so[jb]
          K.pool(lambda e: e.tensor_tensor(out=rsb[jb][:], in0=osb[jb][:], in1=osb[jb][:], op=ALU.mult),
                 r=[osb[jb]], w=[rsb[jb]])
          K.dve(lambda e: e.reduce_sum(out=s_[:], in_=rsb[jb][:], axis=AX.X), r=[rsb[jb]], w=[s_])
          K.dve(lambda e: e.tensor_scalar(out=s_[:], in0=s_[:], scalar1=1.0 / 128, scalar2=EPS, op0=ALU.mult, op1=ALU.add),
                r=[s_], w=[s_])
          K.act(lambda e: e.sqrt(out=s_[:], in_=s_[:]), r=[s_], w=[s_])
          K.dve(lambda e: e.reciprocal(out=s_[:], in_=s_[:]), r=[s_], w=[s_])
          K.dve(lambda e: e.tensor_tensor(out=rsb[jb][:], in0=osb[jb][:], in1=s_[:].unsqueeze(2).to_broadcast([64, 8, 128]),
                                          op=ALU.mult), r=[osb[jb], s_], w=[rsb[jb]])
          K.pool(lambda e: e.tensor_tensor(out=rsb[jb][:], in0=rsb[jb][:], in1=gs[jb][:], op=ALU.mult),
                 r=[rsb[jb], gs[jb]], w=[rsb[jb]])
          K.dma("sp", ro[blk * 512:(blk + 1) * 512, :].rearrange("(c t) e -> t c e", t=64), rsb[jb][:], r=[rsb[jb]])
    except _Stop:
        pass
    K.finish()
    return nc


def launch_hgrn(x, mod_l, nrm_l, w_in, lb_logits, o_gain, ei):
    nc = _prog("hgrn", build_hgrn_program)
    cst = hgrn_consts()
    base = 512 + 6 * 128 + 24
    nrm = _f32(nrm_l).reshape(1, 1024)
    in_maps = []
    for c in range(NCORES):
        b, h = c // 4, c % 4
        cq = w_in[:, base + h * 128: base + (h + 1) * 128]
        cf = w_in[:, base + 512 + h * 128: base + 512 + (h + 1) * 128]
        ci = w_in[:, base + 1024 + h * 128: base + 1024 + (h + 1) * 128]
        cg = w_in[:, base + 1536 + h * 128: base + 1536 + (h + 1) * 128]
        lbl = np.zeros((128, 4), np.float32)
        lbl[:, 0] = lb_logits[0, h * 128:(h + 1) * 128]
        lbl[:, 1] = lb_logits[1, h * 128:(h + 1) * 128]
        lbl[:, 2] = 1.0
        lbl[:, 3] = 1.0 if ei >= 1 else 0.0
        in_maps.append({
            "x": _f32(x[b]), "modv": _f32(mod_l[b].reshape(6, 1024)), "nrm": nrm,
            "wqf": _f32(np.concatenate([cq, cf], axis=1)), "wig": _f32(np.concatenate([ci, cg], axis=1)),
            "lbl": lbl, "og": _f32(o_gain).reshape(1, 128), "cst": cst,
        })
    res = run_bass_kernel_spmd(nc, in_maps, core_ids=list(range(NCORES)))
    r = np.empty((2, 8192, 512), np.float32)
    for c in range(NCORES):
        b, h = c // 4, c % 4
        r[b, :, h * 128:(h + 1) * 128] = res.results[c]["ro"]
    return r


def conv_consts():
    c = np.zeros((128, 256), np.float32)
    c[:, 0:128] = np.eye(128, dtype=np.float32)
    c[:, 128:256] = 1.0
    return c


def build_conv_program():
    nc = bass.Bass("TRN2", target_bir_lowering=False)
    T = 2048
    TH = T + 128
    xh = nc.dram_tensor("xh", [TH, 1024], F32, kind="ExternalInput").ap()
    modv = nc.dram_tensor("modv", [6, 1024], F32, kind="ExternalInput").ap()
    nrm = nc.dram_tensor("nrm", [1, 1024], F32, kind="ExternalInput").ap()
    wpw1 = nc.dram_tensor("wpw1", [1024, 2048], F32, kind="ExternalInput").ap()
    chan = nc.dram_tensor("chan", [128, 8 * 34], F32, kind="ExternalInput").ap()
    flag = nc.dram_tensor("flag", [128, 1], F32, kind="ExternalInput").ap()
    cst = nc.dram_tensor("cst", [128, 256], F32, kind="ExternalInput").ap()
    zT = nc.dram_tensor("zT", [1024, T], F32, kind="ExternalOutput").ap()
    K = KB(nc)
    cst_b = K.sb("cst_b", [128, 256], BF16)
    chan_sb = K.sb("chan_sb", [128, 8, 34], F32)
    flag_sb = K.sb("flag_sb", [128, 1], F32)
    G = K.sb("G", [128, 1024], F32)
    SH = K.sb("SH", [128, 1024], F32)
    tmp = K.sb("tmp", [128, 1024], F32)
    w1_sb = K.sb("w1_sb", [128, 8, 2048], BF16)
    hT_all = K.sb("hT_all", [128, 8, TH], BF16)
    vT = K.sb("vT", [128, 8, T], BF16)
    K.dma("pool", cst_b[:], cst, w=[cst_b])
    K.dma("sp", chan_sb[:], chan.rearrange("p (c n) -> p c n", c=8), w=[chan_sb])
    K.dma("sp", flag_sb[:], flag, w=[flag_sb])
    K.dma("pool", w1_sb[:], wpw1.rearrange("(kc p) n -> p kc n", p=128), w=[w1_sb])
    emit_mod_setup(K, modv, nrm, G, SH, tmp)
    ident_b = cst_b[:, 0:128]
    ones_b = cst_b[:, 128:256]
    psT = [K.ps("psT%d" % j, [128, 1024], BF16) for j in range(2)]
    psP = [K.ps("psP%d" % j, [128, 512], F32) for j in range(4)]
    psV = [K.ps("psV%d" % j, [128, 512], F32) for j in range(2)]

    K.push()
    xt = [K.sb("xt%d" % j, [128, 1024], F32) for j in range(2)]
    junk = K.sb("junk", [128, 1024], BF16)
    sm = [K.sb("sm%d" % j, [128, 8], F32) for j in range(2)]
    hf = [K.sb("hf%d" % j, [128, 1024], F32) for j in range(2)]
    hb = [K.sb("hb%d" % j, [128, 1024], BF16) for j in range(2)]
    hT_tiles = [Tile(hT_all.t, "hT_all_%d" % g) for g in range(TH // 128)]
    for g in range(TH // 128):
        j = g % 2
        K.dma("sp", xt[j][:], xh[g * 128:(g + 1) * 128, :], w=[xt[j]])
        emit_norm_T(K, xt[j], G, SH, junk, sm[j], hf[j], hb[j], psT[j], ident_b, cst_b,
                    hT_all[:, :, g * 128:(g + 1) * 128], hT_tiles[g])
    K.pop()

    K.push()
    uT = [K.sb("uT%d" % j, [128, TH], BF16) for j in range(2)]
    diag = [K.sb("diag%d" % j, [128, 31, 128], BF16) for j in range(2)]
    sgt = [K.sb("sgt%d" % j, [128, 512], F32) for j in range(2)]
    blocks = [(0, 128)] + [(128 + k * 512, 128 + (k + 1) * 512) for k in range(4)]
    for cc in range(8):
        j = cc % 2
        K.dve(lambda e: e.tensor_tensor(out=diag[j][:], in0=ident_b.unsqueeze(1).to_broadcast([128, 31, 128]),
                                        in1=chan_sb[:, cc, 0:31].unsqueeze(2).to_broadcast([128, 31, 128]),
                                        op=ALU.mult), r=[cst_b, chan_sb], w=[diag[j]])
        for tb, (t0, t1) in enumerate(blocks):
            n = t1 - t0
            pa = psP[(2 * tb) % 4]
            pg = psP[(2 * tb + 1) % 4]
            for (pp, c0) in ((pa, cc * 128), (pg, 1024 + cc * 128)):
                for kc in range(8):
                    K.pe(lambda e: e.matmul(pp[:, 0:n], w1_sb[:, kc, c0:c0 + 128], hT_all[:, kc, t0:t1],
                                            start=(kc == 0), stop=(kc == 7)), r=[w1_sb, hT_all], w=[pp], sig=(kc == 7))
            sg = sgt[tb % 2]
            K.act(lambda e: e.activation(out=sg[:, 0:n], in_=pg[:, 0:n], func=AF.Sigmoid), r=[pg], w=[sg])
            if tb == 0:
                K.dve(lambda e: e.scalar_tensor_tensor(out=uT[j][:, t0:t1], in0=pa[:, 0:n], scalar=flag_sb[:, 0:1],
                                                       in1=sg[:, 0:n], op0=ALU.mult, op1=ALU.mult),
                      r=[pa, sg, flag_sb], w=[uT[j]])
            else:
                K.dve(lambda e: e.tensor_tensor(out=uT[j][:, t0:t1], in0=pa[:, 0:n], in1=sg[:, 0:n], op=ALU.mult),
                      r=[pa, sg], w=[uT[j]])
        for tb in range(4):
            pv = psV[tb % 2]
            for jt in range(31):
                o = 128 + tb * 512 - 30 + jt
                K.pe(lambda e: e.matmul(pv[:], diag[j][:, jt, :], uT[j][:, o:o + 512],
                                        start=(jt == 0), stop=(jt == 30)), r=[diag[j], uT[j]], w=[pv], sig=(jt == 30))
            K.act(lambda e: e.activation(out=vT[:, cc, tb * 512:(tb + 1) * 512], in_=pv[:], func=AF.Identity,
                                         bias=chan_sb[:, cc, 31:32]), r=[pv, chan_sb], w=[vT])
    K.pop()

    K.push()
    sqb = [K.sb("sqb%d" % j, [128, 512], BF16) for j in range(2)]
    mu = K.sb("mu", [128, 512], F32)
    ex2 = K.sb("ex2", [128, 512], F32)
    rstd = K.sb("rstd", [128, 512], F32)
    t1b = [K.sb("t1b%d" % j, [128, 512], F32) for j in range(2)]
    t2b = [K.sb("t2b%d" % j, [128, 512], F32) for j in range(2)]
    zo = [K.sb("zo%d" % j, [128, 512], F32) for j in range(2)]
    for tb in range(4):
        S1, S2 = psP[0], psP[1]
        sl = slice(tb * 512, (tb + 1) * 512)
        for cc in range(8):
            sq = sqb[cc % 2]
            K.act(lambda e: e.activation(out=sq[:], in_=vT[:, cc, sl], func=AF.Square), r=[vT], w=[sq])
            K.pe(lambda e: e.matmul(S1[:], ones_b, vT[:, cc, sl], start=(cc == 0), stop=(cc == 7)),
                 r=[cst_b, vT], w=[S1], sig=(cc == 7))
            K.pe(lambda e: e.matmul(S2[:], ones_b, sq[:], start=(cc == 0), stop=(cc == 7)),
                 r=[cst_b, sq], w=[S2])
        K.act(lambda e: e.mul(out=mu[:], in_=S1[:], mul=1.0 / 1024), r=[S1], w=[mu])
        K.dve(lambda e: e.tensor_scalar(out=ex2[:], in0=S2[:], scalar1=1.0 / 1024, scalar2=EPS,
                                        op0=ALU.mult, op1=ALU.add), r=[S2], w=[ex2])
        K.pool(lambda e: e.tensor_tensor(out=rstd[:], in0=mu[:], in1=mu[:], op=ALU.mult), r=[mu], w=[rstd])
        K.dve(lambda e: e.tensor_tensor(out=ex2[:], in0=ex2[:], in1=rstd[:], op=ALU.subtract), r=[ex2, rstd], w=[ex2])
        K.act(lambda e: e.sqrt(out=ex2[:], in_=ex2[:]), r=[ex2], w=[ex2])
        K.dve(lambda e: e.reciprocal(out=rstd[:], in_=ex2[:]), r=[ex2], w=[rstd])
        for cc in range(8):
            j = cc % 2
            K.dve(lambda e: e.tensor_tensor(out=t1b[j][:], in0=vT[:, cc, sl], in1=mu[:], op=ALU.subtract),
                  r=[vT, mu], w=[t1b[j]])
            K.pool(lambda e: e.tensor_tensor(out=t2b[j][:], in0=t1b[j][:], in1=rstd[:], op=ALU.mult),
                   r=[t1b[j], rstd], w=[t2b[j]])
            K.act(lambda e: e.activation(out=zo[j][:], in_=t2b[j][:], func=AF.Silu, scale=chan_sb[:, cc, 32:33],
                                         bias=chan_sb[:, cc, 33:34]), r=[t2b[j], chan_sb], w=[zo[j]])
            K.dma("sp", zT[cc * 128:(cc + 1) * 128, sl], zo[j][:], r=[zo[j]])
    K.pop()
    K.finish()
    return nc


def launch_conv(x, mod_l, nrm_l, w_pw1, dw, dw_b, ln_g, ln_b):
    nc = _prog("conv", build_conv_program)
    cst = conv_consts()
    nrm = _f32(nrm_l).reshape(1, 1024)
    chan = np.zeros((128, 8, 34), np.float32)
    chan[:, :, 0:31] = dw.T.reshape(8, 128, 31).transpose(1, 0, 2)
    chan[:, :, 31] = dw_b.reshape(8, 128).T
    chan[:, :, 32] = ln_g.reshape(8, 128).T
    chan[:, :, 33] = ln_b.reshape(8, 128).T
    chan = _f32(chan.reshape(128, 8 * 34))
    w_pw1 = _f32(w_pw1)
    in_maps = []
    for c in range(NCORES):
        b, q = c // 4, c % 4
        xh = np.zeros((2048 + 128, 1024), np.float32)
        xh[128:] = x[b, q * 2048:(q + 1) * 2048]
        if q > 0:
            xh[:128] = x[b, q * 2048 - 128:q * 2048]
        in_maps.append({
            "xh": xh, "modv": _f32(mod_l[b].reshape(6, 1024)), "nrm": nrm, "wpw1": w_pw1, "chan": chan,
            "flag": np.full((128, 1), 1.0 if q > 0 else 0.0, np.float32), "cst": cst,
        })
    res = run_bass_kernel_spmd(nc, in_maps, core_ids=list(range(NCORES)))
    z = np.empty((2, 8192, 1024), np.float32)
    for c in range(NCORES):
        b, q = c // 4, c % 4
        z[b, q * 2048:(q + 1) * 2048] = res.results[c]["zT"].T
    return z


NSA_NEG = -1e30
_NC_ID, _NC_TLE, _NC_TGT, _NC_A, _NC_V0, _NC_KEEP, _NC_BIAS, _NC_END = 0, 128, 256, 384, 896, 1024, 1279, 1534


def nsa_consts():
    c = np.zeros((128, _NC_END), np.float32)
    p = np.arange(128)
    c[:, _NC_ID:_NC_ID + 128] = np.eye(128, dtype=np.float32)
    c[:, _NC_TLE:_NC_TLE + 128] = (p[:, None] <= p[None, :]).astype(np.float32)
    c[:, _NC_TGT:_NC_TGT + 128] = (p[:, None] > p[None, :]).astype(np.float32)
    A = np.zeros((512, 128), np.float32)
    wts = [1.0, 2.0, 2.0, 2.0, 1.0]
    for m in range(128):
        for o, w in enumerate(wts):
            n = 4 * m + o - 1
            if 0 <= n < 511:
                A[n, m] += w
    c[:, _NC_A:_NC_A + 512] = A.reshape(4, 128, 128).transpose(1, 0, 2).reshape(128, 512)
    c[:, _NC_V0:_NC_V0 + 128] = 16.0 * p[:, None] + 31.0 - p[None, :]
    keep = np.ones((128, 255), np.float32)
    bias = np.zeros((128, 255), np.float32)
    for q in range(128):
        hi = 1 if q >= 64 else 0
        for ui in range(255):
            u = ui - 127
            rel = u - hi
            if rel > 0:
                keep[q, ui], bias[q, ui] = 0.0, NSA_NEG
            elif rel == 0:
                keep[q, ui], bias[q, ui] = 0.0, 2e4
            elif rel == -1:
                keep[q, ui], bias[q, ui] = 0.0, 3e4
    c[:, _NC_KEEP:_NC_KEEP + 255] = keep
    c[:, _NC_BIAS:_NC_BIAS + 255] = bias
    wexp = np.zeros((128, 8192), np.float32)
    for m in range(128):
        wexp[m, m * 64:(m + 1) * 64] = 1.0
    return c, wexp


def build_nsa_program(S=8192, dbg=None):
    nc = bass.Bass("TRN2", target_bir_lowering=False)
    NTL = S // 128
    x = nc.dram_tensor("x", [S, 1024], F32, kind="ExternalInput").ap()
    modv = nc.dram_tensor("modv", [6, 1024], F32, kind="ExternalInput").ap()
    nrm = nc.dram_tensor("nrm", [1, 1024], F32, kind="ExternalInput").ap()
    wtm = nc.dram_tensor("wtm", [1024, 524], F32, kind="ExternalInput").ap()
    wfm = nc.dram_tensor("wfm", [1024, 128], F32, kind="ExternalInput").ap()
    wc = nc.dram_tensor("wc", [64, 2 * 32 * 64], F32, kind="ExternalInput").ap()
    peT = nc.dram_tensor("peT", [64, 32 * 128], F32, kind="ExternalInput").ap()
    gn = nc.dram_tensor("gn", [128, 448], F32, kind="ExternalInput").ap()
    cst = nc.dram_tensor("cst", [128, _NC_END], F32, kind="ExternalInput").ap()
    wexp = nc.dram_tensor("wexp", [128, 8192], F32, kind="ExternalInput").ap()
    ao = nc.dram_tensor("ao", [S, 128], F32, kind="ExternalOutput").ap()
    K = KB(nc)
    cst_f = K.sb("cst_f", [128, _NC_END], F32)
    cst_b = K.sb("cst_b", [128, _NC_V0], BF16)
    wexp_b = K.sb("wexp_b", [128, S], BF16)
    G = K.sb("G", [128, 1024], F32)
    SH = K.sb("SH", [128, 1024], F32)
    tmp = K.sb("tmp", [128, 1024], F32)
    wtm_sb = K.sb("wtm_sb", [128, 8, 524], BF16)
    wfm_sb = K.sb("wfm_sb", [128, 8, 128], BF16)
    wc_sb = K.sb("wc_sb", [64, 2, 32, 64], BF16)
    pe_sb = K.sb("pe_sb", [64, 32, 128], BF16)
    gn_sb = K.sb("gn_sb", [128, 448], F32)
    cpe_rep = K.sb("cpe_rep", [128, 128], F32)
    ksT = K.sb("ksT", [64, S], BF16)
    kwT = K.sb("kwT", [64, S], BF16)
    NCT = (S // 16 - 1 + 127) // 128
    kvcT = K.sb("kvcT", [64, max(S, NCT * 2048) + 32], BF16)
    vvcT = K.sb("vvcT", [64, max(S, NCT * 2048) + 32], BF16)
    kcT = K.sb("kcT", [64, 512], BF16)
    vs_aug = K.sb("vs_aug", [128, NTL, 65], BF16)
    vw_aug = K.sb("vw_aug", [128, NTL, 65], BF16)
    Rc = K.sb("Rc", [128, 4, 193], BF16)
    K.dma("sp", cst_f[:], cst, w=[cst_f])
    K.dma("pool", cst_b[:], cst[:, 0:_NC_V0], w=[cst_b])
    K.dma("pool", wexp_b[:], wexp[:, 0:S], w=[wexp_b])
    with nc.allow_non_contiguous_dma(reason="weight slices"):
        K.dma("pool", wtm_sb[:], wtm.rearrange("(kc p) n -> p kc n", p=128), w=[wtm_sb])
        K.dma("pool", wfm_sb[:], wfm.rearrange("(kc p) n -> p kc n", p=128), w=[wfm_sb])
    K.dma("pool", wc_sb[:], wc.rearrange("p (h l e) -> p h l e", h=2, l=32), w=[wc_sb])
    K.dma("pool", pe_sb[:], peT.rearrange("p (l n) -> p l n", l=32), w=[pe_sb])
    K.dma("sp", gn_sb[:], gn, w=[gn_sb])
    emit_mod_setup(K, modv, nrm, G, SH, tmp)
    ident_b = cst_b[:, _NC_ID:_NC_ID + 128]
    tle_b = cst_b[:, _NC_TLE:_NC_TLE + 128]
    tgt_b = cst_b[:, _NC_TGT:_NC_TGT + 128]
    V0 = cst_f[:, _NC_V0:_NC_V0 + 128]
    K.dve(lambda e: e.memset(kvcT[:], 0.0), w=[kvcT])
    K.dve(lambda e: e.memset(vvcT[:], 0.0), w=[vvcT])
    K.dve(lambda e: e.memset(kcT[:], 0.0), w=[kcT])
    K.dve(lambda e: e.memset(vs_aug[:], 1.0), w=[vs_aug])
    K.dve(lambda e: e.memset(vw_aug[:], 1.0), w=[vw_aug])
    K.dve(lambda e: e.memset(Rc[:], 1.0), w=[Rc])
    K.dve(lambda e: e.tensor_copy(out=Rc[:, :, 65:193], in_=cst_b[:, _NC_A:_NC_A + 512].rearrange("p (c m) -> p c m", c=4)),
          r=[cst_b], w=[Rc])

    psT = [K.ps("psT%d" % j, [128, 1024], BF16) for j in range(2)]
    B = [K.ps("B%d" % j, [128, 512], F32) for j in range(6)]

    for half in range(2):
        for l in range(32):
            K.pe(lambda e: e.matmul(B[1][:, half * 64:(half + 1) * 64], pe_sb[:, l, :], wc_sb[:, half, l, :],
                                    start=(l == 0), stop=(l == 31)), r=[pe_sb, wc_sb], w=[B[1]], sig=(l == 31))
    K.dve(lambda e: e.tensor_copy(out=cpe_rep[:], in_=B[1][:, 0:128]), r=[B[1]], w=[cpe_rep])

    xt = [K.sb("xt%d" % j, [128, 1024], F32) for j in range(2)]
    junk = K.sb("junk", [128, 1024], BF16)
    sm = [K.sb("sm%d" % j, [128, 8], F32) for j in range(2)]
    hf = [K.sb("hf%d" % j, [128, 1024], F32) for j in range(2)]
    hb = [K.sb("hb%d" % j, [128, 1024], BF16) for j in range(2)]
    hT = [K.sb("hT%d" % j, [128, 8, 128], BF16) for j in range(2)]
    sq = K.sb("sq", [128, 384], F32)
    qkn = K.sb("qkn", [128, 384], BF16)
    st = [K.sb("st%d" % j, [128, 80], F32) for j in range(2)]
    qT = [K.sb("qT%d" % j, [64, 512], BF16) for j in range(2)]
    kcn = K.sb("kcn", [128, 64], F32)
    kcb = K.sb("kcb", [128, 64], BF16)
    ec = [K.sb("ec%d" % j, [128, 512], BF16) for j in range(2)]
    es = [K.sb("es%d" % j, [128, 256], BF16) for j in range(3)]
    mk = [K.sb("mk%d" % j, [128, 128], BF16) for j in range(2)]
    imp = K.sb("imp", [128, 128], F32)
    score = K.sb("score", [128, 128], F32)
    sc2 = K.sb("sc2", [128, 128], F32)
    sel = K.sb("sel", [128, 128], BF16)
    selT = K.sb("selT", [128, 128], BF16)
    oacc = [K.sb("oacc%d" % j, [128, 128], F32) for j in range(2)]
    kvT_tiles = [Tile(kvcT.t, "kvcT%d" % g) for g in range(NTL)]
    vvT_tiles = [Tile(vvcT.t, "vvcT%d" % g) for g in range(NTL)]
    ks_tiles = [Tile(ksT.t, "ksT%d" % g) for g in range(NTL)]
    kw_tiles = [Tile(kwT.t, "kwT%d" % g) for g in range(NTL)]
    vs_tiles = [Tile(vs_aug.t, "vs%d" % g) for g in range(NTL)]
    vw_tiles = [Tile(vw_aug.t, "vw%d" % g) for g in range(NTL)]
    kc_tiles = [Tile(kcT.t, "kc%d" % g) for g in range(4)]
    rc_tiles = [Tile(Rc.t, "rc%d" % g) for g in range(4)]
    for t_ in vvT_tiles + kvT_tiles + ks_tiles + kw_tiles + vs_tiles + vw_tiles + kc_tiles + rc_tiles:
        t_.w = (K.engs["dve"].sid, K.engs["dve"].count)
    nslot = 0

    def ck(k, i):
        if dbg is not None and dbg == k:
            raise _Stop()
    try:
     for i in range(NTL):
         j = i % 2
         S_ = st[j]
         sl = slice(i * 128, (i + 1) * 128)
         ck(0, i)
         K.dma("sp", xt[j][:], x[sl, :], w=[xt[j]])
         emit_norm_T(K, xt[j], G, SH, junk, sm[j], hf[j], hb[j], psT[0], ident_b, cst_b, hT[j][:], hT[j])
         ck(7, i)
         pm1, pm2 = B[0], B[1]
         for kc in range(8):
             K.pe(lambda e: e.matmul(pm1[:], hT[j][:, kc, :], wtm_sb[:, kc, 0:512], start=(kc == 0), stop=(kc == 7)),
                  r=[hT[j], wtm_sb], w=[pm1], sig=(kc == 7))
         for kc in range(8):
             K.pe(lambda e: e.matmul(pm2[:, 0:12], hT[j][:, kc, :], wtm_sb[:, kc, 512:524], start=(kc == 0), stop=(kc == 7)),
                  r=[hT[j], wtm_sb], w=[pm2], sig=False)
         for half in range(2):
             for kc in range(8):
                 K.pe(lambda e: e.matmul(pm2[0:64, 128 + half * 128:256 + half * 128], wfm_sb[:, kc, half * 64:(half + 1) * 64],
                                         hT[j][:, kc, :], start=(kc == 0), stop=(kc == 7)),
                      r=[hT[j], wfm_sb], w=[pm2], sig=(kc == 7 and half == 1))
         K.dve(lambda e: e.tensor_copy(out=kvcT[:, sl], in_=pm2[0:64, 128:256]), r=[pm2], w=[kvT_tiles[i]])
         K.dve(lambda e: e.tensor_copy(out=vvcT[:, sl], in_=pm2[0:64, 256:384]), r=[pm2], w=[vvT_tiles[i]])
         K.dve(lambda e: e.tensor_copy(out=S_[:, 0:12], in_=pm2[:, 0:12]), r=[pm2], w=[S_])
         K.act(lambda e: e.activation(out=S_[:, 0:12], in_=S_[:, 0:12], func=AF.Sigmoid), r=[S_], w=[S_])
         K.dve(lambda e: e.tensor_copy(out=vs_aug[:, i, 0:64], in_=pm1[:, 384:448]), r=[pm1], w=[vs_tiles[i]])
         K.dve(lambda e: e.tensor_copy(out=vw_aug[:, i, 0:64], in_=pm1[:, 448:512]), r=[pm1], w=[vw_tiles[i]])
         ck(1, i)
         K.act(lambda e: e.activation(out=sq[:], in_=pm1[:, 0:384], func=AF.Square), r=[pm1], w=[sq])
         K.dve(lambda e: e.reduce_sum(out=S_[:, 16:22], in_=sq[:].rearrange("p (s d) -> p s d", s=6), axis=AX.X),
               r=[sq], w=[S_])
         K.dve(lambda e: e.tensor_scalar(out=S_[:, 16:22], in0=S_[:, 16:22], scalar1=1.0 / 64, scalar2=EPS,
                                         op0=ALU.mult, op1=ALU.add), r=[S_], w=[S_])
         K.act(lambda e: e.sqrt(out=S_[:, 16:22], in_=S_[:, 16:22]), r=[S_], w=[S_])
         K.dve(lambda e: e.reciprocal(out=S_[:, 16:22], in_=S_[:, 16:22]), r=[S_], w=[S_])
         K.dve(lambda e: e.tensor_tensor(out=sq[:].rearrange("p (s d) -> p s d", s=6),
                                         in0=pm1[:, 0:384].rearrange("p (s d) -> p s d", s=6),
                                         in1=S_[:, 16:22].unsqueeze(2).to_broadcast([128, 6, 64]), op=ALU.mult),
               r=[pm1, S_], w=[sq])
         K.dve(lambda e: e.tensor_tensor(out=qkn[:], in0=sq[:], in1=gn_sb[:, 0:384], op=ALU.mult),
               r=[sq, gn_sb], w=[qkn])
         pT = psT[1]
         for s6 in range(6):
             K.pe(lambda e: e.transpose(pT[0:64, s6 * 128:(s6 + 1) * 128], qkn[:, s6 * 64:(s6 + 1) * 64], ident_b),
                  r=[qkn, cst_b], w=[pT], sig=(s6 == 5))
         K.dve(lambda e: e.tensor_copy(out=qT[j][:], in_=pT[0:64, 0:512]), r=[pT], w=[qT[j]])
         K.dve(lambda e: e.tensor_copy(out=ksT[:, sl], in_=pT[0:64, 512:640]), r=[pT], w=[ks_tiles[i]])
         K.dve(lambda e: e.tensor_copy(out=kwT[:, sl], in_=pT[0:64, 640:768]), r=[pT], w=[kw_tiles[i]])
         ck(2, i)
         chi = (8 * i + 6) // 128
         ctiles = [chi] if (i % 16 != 0 or i == 0) else [chi - 1, chi]
         first_tok_tile = lambda c: c * 16
         for c in ctiles:
             rdeps = kvT_tiles[c * 16:min(NTL, c * 16 + 17)] + vvT_tiles[c * 16:min(NTL, c * 16 + 17)]
             pk, pv = B[1], B[2]
             for (pp, srcT, half) in ((pk, kvcT, 0), (pv, vvcT, 1)):
                 for l in range(32):
                     src = srcT[:, c * 2048 + l:c * 2048 + l + 2033:16]
                     K.pe(lambda e: e.matmul(pp[:, 0:64], src, wc_sb[:, half, l, :], start=(l == 0), stop=(l == 31)),
                          r=rdeps + [wc_sb], w=[pp], sig=(l == 31))
             K.dve(lambda e: e.tensor_tensor(out=Rc[:, c, 0:64], in0=pv[:, 0:64], in1=cpe_rep[:, 64:128], op=ALU.add),
                   r=[pv, cpe_rep], w=[rc_tiles[c]])
             K.dve(lambda e: e.tensor_tensor(out=kcn[:], in0=pk[:, 0:64], in1=cpe_rep[:, 0:64], op=ALU.add),
                   r=[pk, cpe_rep], w=[kcn])
             K.act(lambda e: e.activation(out=junk[:, 0:64], in_=kcn[:], func=AF.Square, accum_out=S_[:, 24:25]),
                   r=[kcn], w=[junk, S_])
             K.dve(lambda e: e.tensor_scalar(out=S_[:, 24:25], in0=S_[:, 24:25], scalar1=1.0 / 64, scalar2=EPS,
                                             op0=ALU.mult, op1=ALU.add), r=[S_], w=[S_])
             K.act(lambda e: e.sqrt(out=S_[:, 24:25], in_=S_[:, 24:25]), r=[S_], w=[S_])
             K.dve(lambda e: e.reciprocal(out=S_[:, 24:25], in_=S_[:, 24:25]), r=[S_], w=[S_])
             K.dve(lambda e: e.scalar_tensor_tensor(out=kcb[:], in0=kcn[:], scalar=S_[:, 24:25], in1=gn_sb[:, 384:448],
                                                    op0=ALU.mult, op1=ALU.mult), r=[kcn, S_, gn_sb], w=[kcb])
             K.pe(lambda e: e.transpose(pT[0:64, 768:896], kcb[:], ident_b), r=[kcb, cst_b], w=[pT])
             K.dve(lambda e: e.tensor_copy(out=kcT[:, c * 128:(c + 1) * 128], in_=pT[0:64, 768:896]), r=[pT], w=[kc_tiles[c]])
         ck(3, i)
         pO = (B[3], B[4])
         cvalid = []
         for c in range(chi + 1):
             thr = 128 * i - 2048 * c
             if thr < -96:
                 continue
             cvalid.append((c, thr))
         for ci, (c, thr) in enumerate(cvalid):
             pS = B[2]
             K.pe(lambda e: e.matmul(pS[:], kcT[:, c * 128:(c + 1) * 128], qT[j][:], start=True, stop=True),
                  r=[kc_tiles[c], qT[j]], w=[pS])
             e_ = ec[ci % 2]
             K.act(lambda e: e.activation(out=e_[:], in_=pS[:], func=AF.Exp, scale=0.125), r=[pS], w=[e_])
             if thr < 2063:
                 K.dve(lambda e: e.scalar_tensor_tensor(
                     out=e_[:].rearrange("p (h q) -> p h q", h=4), in0=V0.unsqueeze(1).to_broadcast([128, 4, 128]),
                     scalar=float(thr), in1=e_[:].rearrange("p (h q) -> p h q", h=4), op0=ALU.is_le, op1=ALU.mult),
                     r=[cst_f, e_], w=[e_])
             for h4 in range(4):
                 po = pO[h4 // 2]
                 o0 = (h4 % 2) * 193
                 K.pe(lambda e: e.matmul(po[:, o0:o0 + 193], e_[:, h4 * 128:(h4 + 1) * 128], Rc[:, c, :],
                                         start=(ci == 0 and h4 % 2 == 0), stop=(ci == len(cvalid) - 1 and h4 % 2 == 1)),
                      r=[e_, rc_tiles[c]], w=[po], sig=(h4 % 2 == 1))
         oa = oacc[j]
         if cvalid:
             for h4 in range(4):
                 po = pO[h4 // 2]
                 o0 = (h4 % 2) * 193
                 K.dve(lambda e: e.tensor_scalar(out=S_[:, 32 + h4:33 + h4], in0=po[:, o0 + 64:o0 + 65], scalar1=1e-30,
                                                 scalar2=None, op0=ALU.max), r=[po], w=[S_])
             K.dve(lambda e: e.reciprocal(out=S_[:, 32:36], in_=S_[:, 32:36]), r=[S_], w=[S_])
             for h4 in range(4):
                 po = pO[h4 // 2]
                 o0 = (h4 % 2) * 193
                 if h4 == 0:
                     K.dve(lambda e: e.tensor_scalar(out=imp[:], in0=po[:, o0 + 65:o0 + 193], scalar1=S_[:, 32:33],
                                                     scalar2=None, op0=ALU.mult), r=[po, S_], w=[imp])
                 else:
                     K.dve(lambda e: e.scalar_tensor_tensor(out=imp[:], in0=po[:, o0 + 65:o0 + 193],
                                                            scalar=S_[:, 32 + h4:33 + h4], in1=imp[:],
                                                            op0=ALU.mult, op1=ALU.add), r=[po, S_, imp], w=[imp])
             for h2 in range(2):
                 K.dve(lambda e: e.tensor_tensor(out=S_[:, 36 + h2:37 + h2], in0=S_[:, 32 + h2:33 + h2],
                                                 in1=S_[:, 3 * h2:3 * h2 + 1], op=ALU.mult), r=[S_], w=[S_])
                 K.dve(lambda e: e.tensor_scalar(out=oa[:, h2 * 64:(h2 + 1) * 64], in0=pO[0][:, h2 * 193:h2 * 193 + 64],
                                                 scalar1=S_[:, 36 + h2:37 + h2], scalar2=None, op0=ALU.mult),
                       r=[pO[0], S_], w=[oa])
         else:
             K.dve(lambda e: e.memset(imp[:], 0.0), w=[imp])
             K.dve(lambda e: e.memset(oa[:], 0.0), w=[oa])
         ck(4, i)
         u0 = 127 - 2 * i
         K.dve(lambda e: e.tensor_tensor(out=score[:], in0=imp[:], in1=cst_f[:, _NC_KEEP + u0:_NC_KEEP + u0 + 128],
                                         op=ALU.mult), r=[imp, cst_f], w=[score])
         K.dve(lambda e: e.tensor_tensor(out=score[:], in0=score[:], in1=cst_f[:, _NC_BIAS + u0:_NC_BIAS + u0 + 128],
                                         op=ALU.add), r=[score, cst_f], w=[score])
         K.dve(lambda e: e.memset(score[:, 0:1], 1e4), w=[score])
         K.dve(lambda e: e.max(out=S_[:, 40:48], in_=score[:]), r=[score], w=[S_])
         K.dve(lambda e: e.match_replace(out=sc2[:], in_to_replace=S_[:, 40:48], in_values=score[:], imm_value=-3e38),
               r=[score, S_], w=[sc2])
         K.dve(lambda e: e.max(out=S_[:, 48:56], in_=sc2[:]), r=[sc2], w=[S_])
         K.dve(lambda e: e.tensor_scalar(out=S_[:, 56:57], in0=S_[:, 55:56], scalar1=-1e29, scalar2=None, op0=ALU.max),
               r=[S_], w=[S_])
         K.dve(lambda e: e.tensor_scalar(out=sel[:], in0=score[:], scalar1=S_[:, 56:57], scalar2=None, op0=ALU.is_ge),
               r=[score, S_], w=[sel])
         K.pe(lambda e: e.transpose(pT[:, 896:1024], sel[:], ident_b), r=[sel, cst_b], w=[pT])
         K.act(lambda e: e.copy(out=selT[:], in_=pT[:, 896:1024]), r=[pT], w=[selT])
         ck(5, i)
         pO2 = B[0]
         qown = qT[j][:, 0:256]
         jobs = [("s", c) for c in range(i + 1)] + [("w", c) for c in range(max(0, i - 4), i + 1)]
         ns = i + 1
         nw = i + 1 - max(0, i - 4)
         cnt = {"s": 0, "w": 0}
         for (kind, c) in jobs:
             half = nslot % 2
             pS2 = B[2] if half == 0 else B[5]
             kt = ks_tiles[c] if kind == "s" else kw_tiles[c]
             kk = ksT if kind == "s" else kwT
             K.pe(lambda e: e.matmul(pS2[:, 0:256], kk[:, c * 128:(c + 1) * 128], qown, start=True, stop=True),
                  r=[kt, qT[j]], w=[pS2], sig=(kind == "w"))
             e2 = es[nslot % 3]
             if kind == "s":
                 K.pe(lambda e: e.matmul(pS2[:, 256:384], wexp_b[:, c * 128:(c + 1) * 128], selT[:], start=True, stop=True),
                      r=[wexp_b, selT], w=[pS2])
             ck(8, i)
             K.act(lambda e: e.activation(out=e2[:], in_=pS2[:, 0:256], func=AF.Exp, scale=0.125), r=[pS2], w=[e2])
             ck(9, i)
             e3 = e2[:].rearrange("p (h q) -> p h q", h=2)
             if kind == "s":
                 if c == i:
                     m_ = mk[nslot % 2]
                     K.dve(lambda e: e.tensor_tensor(out=m_[:], in0=pS2[:, 256:384], in1=tle_b, op=ALU.mult),
                           r=[pS2, cst_b], w=[m_])
                     ck(12, i)
                     K.dve(lambda e: e.tensor_tensor(out=e3, in0=e3, in1=m_[:].unsqueeze(1).to_broadcast([128, 2, 128]),
                                                     op=ALU.mult), r=[e2, m_], w=[e2])
                 else:
                     K.dve(lambda e: e.tensor_tensor(out=e3, in0=e3,
                                                     in1=pS2[:, 256:384].unsqueeze(1).to_broadcast([128, 2, 128]),
                                                     op=ALU.mult), r=[e2, pS2], w=[e2])
             else:
                 mm = None
                 if c == i:
                     mm = tle_b
                 elif c == i - 4:
                     mm = tgt_b
                 if mm is not None:
                     K.dve(lambda e: e.tensor_tensor(out=e3, in0=e3, in1=mm.unsqueeze(1).to_broadcast([128, 2, 128]),
                                                     op=ALU.mult), r=[e2, cst_b], w=[e2])
             ck(10, i)
             va = vs_aug if kind == "s" else vw_aug
             vt = vs_tiles[c] if kind == "s" else vw_tiles[c]
             ob = 0 if kind == "s" else 256
             n_ = ns if kind == "s" else nw
             for h2 in range(2):
                 K.pe(lambda e: e.matmul(pO2[:, ob + h2 * 65:ob + h2 * 65 + 65], e2[:, h2 * 128:(h2 + 1) * 128], va[:, c, :],
                                         start=(kind == "s" and cnt[kind] == 0 and h2 == 0),
                                         stop=(kind == "w" and cnt[kind] == n_ - 1 and h2 == 1)),
                      r=[e2, vt], w=[pO2], sig=(h2 == 1))
             ck(11, i)
             cnt[kind] += 1
             nslot += 1
         ck(6, i)
         for bi, ob in ((1, 0), (2, 256)):
             for h2 in range(2):
                 cidx = 60 + bi * 2 + h2
                 K.dve(lambda e: e.tensor_scalar(out=S_[:, cidx:cidx + 1], in0=pO2[:, ob + h2 * 65 + 64:ob + h2 * 65 + 65],
                                                 scalar1=1e-30, scalar2=None, op0=ALU.max), r=[pO2], w=[S_])
                 K.dve(lambda e: e.reciprocal(out=S_[:, cidx:cidx + 1], in_=S_[:, cidx:cidx + 1]), r=[S_], w=[S_])
                 K.dve(lambda e: e.tensor_tensor(out=S_[:, cidx:cidx + 1], in0=S_[:, cidx:cidx + 1],
                                                 in1=S_[:, 3 * h2 + bi:3 * h2 + bi + 1], op=ALU.mult), r=[S_], w=[S_])
                 K.dve(lambda e: e.scalar_tensor_tensor(out=oa[:, h2 * 64:(h2 + 1) * 64],
                                                        in0=pO2[:, ob + h2 * 65:ob + h2 * 65 + 64],
                                                        scalar=S_[:, cidx:cidx + 1], in1=oa[:, h2 * 64:(h2 + 1) * 64],
                                                        op0=ALU.mult, op1=ALU.add), r=[pO2, S_, oa], w=[oa])
         K.dma("sp", ao[sl, :], oa[:], r=[oa])
    except _Stop:
        pass
    K.finish()
    return nc


def launch_nsa(x, mod_l, nrm_l, w_in, w_ck, w_cv, pe, q_gain, k_gain, S=8192, dbg=None):
    nc = _prog("nsa%d_%s" % (S, dbg), lambda: build_nsa_program(S, dbg))
    cst, wexp = nsa_consts()
    nrm = _f32(nrm_l).reshape(1, 1024)
    wc = np.zeros((64, 2, 32, 64), np.float32)
    wc[:, 0] = w_ck.reshape(32, 64, 64).transpose(1, 0, 2)
    wc[:, 1] = w_cv.reshape(32, 64, 64).transpose(1, 0, 2)
    wc = _f32(wc.reshape(64, 4096))
    peT = _f32(np.repeat(pe.T[:, :, None], 128, axis=2).reshape(64, 4096))
    gn = np.zeros((128, 448), np.float32)
    gn[:, 0:256] = np.tile(q_gain, 4)[None, :]
    gn[:, 256:320] = k_gain[1][None, :]
    gn[:, 320:384] = k_gain[2][None, :]
    gn[:, 384:448] = k_gain[0][None, :]
    in_maps = []
    for c in range(NCORES):
        b, g, hh = c // 4, (c // 2) % 2, c % 2
        heads = [g * 4 + 2 * hh, g * 4 + 2 * hh + 1, g * 4 + 2 * (1 - hh), g * 4 + 2 * (1 - hh) + 1]
        qcols = np.concatenate([w_in[:, h * 64:(h + 1) * 64] for h in heads], axis=1)
        kv = lambda slot: w_in[:, 512 + slot * 128 + g * 64: 512 + slot * 128 + (g + 1) * 64]
        gcols = np.concatenate([w_in[:, 1280 + h * 3:1280 + h * 3 + 3] for h in heads[:2]] +
                               [w_in[:, 1280 + h * 3:1280 + h * 3 + 3] for h in heads[2:]], axis=1)
        wtm = np.concatenate([qcols, kv(2), kv(4), kv(3), kv(5), gcols], axis=1)
        wfm = np.concatenate([kv(0), kv(1)], axis=1)
        in_maps.append({"x": _f32(x[b, :S]), "modv": _f32(mod_l[b].reshape(6, 1024)), "nrm": nrm,
                        "wtm": _f32(wtm), "wfm": _f32(wfm), "wc": wc, "peT": peT, "gn": gn, "cst": cst, "wexp": wexp})
    res = run_bass_kernel_spmd(nc, in_maps, core_ids=list(range(NCORES)))
    a = np.empty((2, S, 512), np.float32)
    for c in range(NCORES):
        b, g, hh = c // 4, (c // 2) % 2, c % 2
        h0 = g * 4 + 2 * hh
        a[b, :, h0 * 64:(h0 + 2) * 64] = res.results[c]["ao"]
    return a


def kernel(x, c, ada_w, ada_b, norm_mix, norm_ffn, mix_w_in, mix_w_out, nsa_cmp_wk, nsa_cmp_wv, nsa_cmp_pe,
           nsa_q_gain, nsa_k_gain, hgrn_lb_logits, hgrn_o_gain, conv_w_pw1, conv_dw, conv_dw_b, conv_ln_g,
           conv_ln_b, conv_w_pw2, moe_w_group, moe_w_expert, moe_w1, moe_w3, moe_w2):
    x = np.asarray(x, dtype=np.float32)
    mod = launch_mod(np.asarray(c), np.asarray(ada_w), np.asarray(ada_b))
    for layer in range(4):
        i = layer // 2
        if layer % 2 == 0:
            a = launch_nsa(x, mod[layer], norm_mix[layer], np.asarray(mix_w_in[i]), np.asarray(nsa_cmp_wk[i]),
                           np.asarray(nsa_cmp_wv[i]), np.asarray(nsa_cmp_pe[i]), np.asarray(nsa_q_gain[i]),
                           np.asarray(nsa_k_gain[i]))
            r = launch_hgrn(x, mod[layer], norm_mix[layer], np.asarray(mix_w_in[i]), np.asarray(hgrn_lb_logits),
                            np.asarray(hgrn_o_gain[i]), i)
            mix = np.concatenate([a, r], axis=-1)
            w_o = mix_w_out[i]
        else:
            mix = launch_conv(x, mod[layer], norm_mix[layer], np.asarray(conv_w_pw1[i]), np.asarray(conv_dw[i]),
                              np.asarray(conv_dw_b[i]), np.asarray(conv_ln_g[i]), np.asarray(conv_ln_b[i]))
            w_o = conv_w_pw2[i]
        x = launch_moe(x, mix, np.asarray(w_o), mod[layer], norm_ffn[layer], np.asarray(moe_w_group[layer]),
                       np.asarray(moe_w_expert[layer]), np.asarray(moe_w1[layer]), np.asarray(moe_w3[layer]),
                       np.asarray(moe_w2[layer]))
    return x
```

```python
import numpy as np
from contextlib import ExitStack
import concourse.bass as bass
import concourse.mybir as mybir
from concourse.bass_utils import run_bass_kernel_spmd

F32 = mybir.dt.float32
BF16 = mybir.dt.bfloat16
I32 = mybir.dt.int32
AF = mybir.ActivationFunctionType
ALU = mybir.AluOpType
AX = mybir.AxisListType

EPS = 1e-6
NCORES = 8


class Tile:
    __slots__ = ("t", "w", "r", "name", "psum")

    def __init__(self, t, name="", psum=False):
        self.t = t
        self.w = None
        self.r = {}
        self.name = name
        self.psum = psum

    def __getitem__(self, k):
        return self.t[k]


class _Eng:
    def __init__(self, name, eng):
        self.name = name
        self.eng = eng
        self.sem = None
        self.sid = None
        self.count = 0
        self.seen = {}
        self.pending = False


class KB:
    NDMA = 24
    ROT = 3500

    def __init__(self, nc):
        self.nc = nc
        self.es = ExitStack()
        self.nuniq = 0
        self.sems = []
        self.engs = {}
        for name, eng in (("pe", nc.tensor), ("act", nc.scalar), ("dve", nc.vector),
                          ("pool", nc.gpsimd), ("sp", nc.sync)):
            e = _Eng(name, eng)
            self.engs[name] = e
            self._rot(e)
        self.slots = []
        self.qslots = {}
        self.qrr = {}
        for q in ("sp", "pool"):
            self.qslots[q] = []
            self.qrr[q] = 0
            for i in range(self.NDMA // 2):
                sid = self._newsem("dq%s%d" % (q, i))
                sl = [sid, 0]
                self.slots.append(sl)
                self.qslots[q].append(sl)
        self.scopes = []

    def _newsem(self, name):
        self.nuniq += 1
        s = self.es.enter_context(self.nc.semaphore("%s_%d" % (name, self.nuniq)))
        self.sems.append(s)
        return len(self.sems) - 1

    def _rot(self, e):
        e.sid = self._newsem("e_" + e.name)
        e.sem = self.sems[e.sid]
        e.count = 0

    def _stack(self):
        return self.scopes[-1] if self.scopes else self.es

    def sb(self, name, shape, dt):
        self.nuniq += 1
        t = self._stack().enter_context(self.nc.sbuf_tensor("%s_%d" % (name, self.nuniq), list(shape), dt))
        return Tile(t, name)

    def ps(self, name, shape, dt):
        self.nuniq += 1
        t = self._stack().enter_context(self.nc.psum_tensor("%s_%d" % (name, self.nuniq), list(shape), dt))
        return Tile(t, name, psum=True)

    def push(self):
        self.scopes.append(ExitStack())

    def pop(self):
        self.barrier()
        self.scopes.pop().close()

    def _waits(self, E, r, w):
        need = {}

        def add(ev):
            if ev is None:
                return
            s, v = ev
            if need.get(s, 0) < v:
                need[s] = v

        for t in r:
            add(t.w)
            if t.psum:
                for s, v in t.r.items():
                    if s != E.sid:
                        add((s, v))
        for t in w:
            add(t.w)
            for s, v in t.r.items():
                add((s, v))
        for s, v in need.items():
            if s == E.sid and E.name == "pe":
                continue
            if E.seen.get(s, 0) >= v:
                continue
            for F in self.engs.values():
                if F.sid == s:
                    assert v <= F.count, "wait on unsignaled event (%s waits %s)" % (E.name, F.name)
            E.eng.wait_ge(self.sems[s], v)
            E.seen[s] = v

    def _mark(self, ev, r, w):
        s, v = ev
        for t in r:
            if t.r.get(s, 0) < v:
                t.r[s] = v
        for t in w:
            t.w = ev
            t.r = {}

    def op(self, en, fn, r=(), w=(), sig=True):
        E = self.engs[en]
        if E.count >= self.ROT and not E.pending:
            self._rot(E)
        self._waits(E, r, w)
        ins = fn(E.eng)
        if sig:
            E.count += 1
            ins.then_inc(E.sem, 1)
            ev = (E.sid, E.count)
            E.pending = False
        else:
            ev = (E.sid, E.count + 1)
            E.pending = True
        self._mark(ev, r, w)
        return ins

    def pe(self, fn, r=(), w=(), sig=True):
        return self.op("pe", fn, r, w, sig)

    def act(self, fn, r=(), w=(), sig=True):
        return self.op("act", fn, r, w, sig)

    def dve(self, fn, r=(), w=(), sig=True):
        return self.op("dve", fn, r, w, sig)

    def pool(self, fn, r=(), w=(), sig=True):
        return self.op("pool", fn, r, w, sig)

    def dmaf(self, qn, fn, r=(), w=()):
        Q = self.engs[qn]
        self._waits(Q, r, w)
        slot = self.qslots[qn][self.qrr[qn]]
        self.qrr[qn] = (self.qrr[qn] + 1) % len(self.qslots[qn])
        if slot[1] >= self.ROT:
            slot[0] = self._newsem("dq")
            slot[1] = 0
        sid, val = slot
        if val > 0 and Q.seen.get(sid, 0) < val:
            Q.eng.wait_ge(self.sems[sid], val)
            Q.seen[sid] = val
        ins = fn(Q.eng)
        ins.then_inc(self.sems[sid], 16)
        slot[1] = val + 16
        self._mark((sid, slot[1]), r, w)
        return ins

    def dma(self, qn, out, in_, r=(), w=()):
        return self.dmaf(qn, lambda e: e.dma_start(out=out, in_=in_), r, w)

    def barrier(self):
        evs = []
        for F in self.engs.values():
            if F.count > 0:
                evs.append((F.sid, F.count))
        for sid, val in self.slots:
            if val > 0:
                evs.append((sid, val))
        for E in self.engs.values():
            for s, v in evs:
                if s == E.sid:
                    continue
                if E.seen.get(s, 0) >= v:
                    continue
                E.eng.wait_ge(self.sems[s], v)
                E.seen[s] = v

    def finish(self):
        self.barrier()
        while self.scopes:
            self.scopes.pop().close()
        self.es.close()


def _bcast_rows(ap_row, n=128):
    return ap_row.partition_broadcast(n)


C_CAP = 512
NS = C_CAP // 128
NSLOT = 32 * C_CAP
DUMMY = NSLOT


def moe_consts():
    c = np.zeros((128, 416), np.float32)
    c[:, 0:128] = np.eye(128, dtype=np.float32)
    tp = np.arange(128)
    c[:, 128:256] = (tp[:, None] < tp[None, :]).astype(np.float32)
    c[:, 256:384] = 1.0
    c[:, 384:416] = (np.arange(32) * C_CAP)[None, :].astype(np.float32)
    return c


def build_moe_program():
    nc = bass.Bass("TRN2", target_bir_lowering=False)
    T = 2048
    NT = T // 128
    xin = nc.dram_tensor("xin", [T, 1024], F32, kind="ExternalInput").ap()
    mixT = nc.dram_tensor("mixT", [1024, T], F32, kind="ExternalInput").ap()
    wo = nc.dram_tensor("wo", [1024, 1024], F32, kind="ExternalInput").ap()
    modv = nc.dram_tensor("modv", [2, 6, 1024], F32, kind="ExternalInput").ap()
    nrm = nc.dram_tensor("nrm", [1, 1024], F32, kind="ExternalInput").ap()
    wr = nc.dram_tensor("wr", [1024, 36], F32, kind="ExternalInput").ap()
    w1 = nc.dram_tensor("w1", [32, 1024, 512], F32, kind="ExternalInput").ap()
    w3 = nc.dram_tensor("w3", [32, 1024, 512], F32, kind="ExternalInput").ap()
    w2 = nc.dram_tensor("w2", [32, 512, 1024], F32, kind="ExternalInput").ap()
    cst = nc.dram_tensor("cst", [128, 416], F32, kind="ExternalInput").ap()
    xout = nc.dram_tensor("xout", [T, 1024], F32, kind="ExternalOutput").ap()
    Xg = nc.dram_tensor("Xg", [NSLOT + 128, 1024], BF16).ap()
    Yd = nc.dram_tensor("Yd", [NSLOT + 128, 1024], F32).ap()

    K = KB(nc)
    xg_t = Tile(Xg, "Xg")
    yd_t = Tile(Yd, "Yd")
    dram_in = Tile(None, "dram_in")

    cst_f = K.sb("cst_f", [128, 416], F32)
    cst_b = K.sb("cst_b", [128, 384], BF16)
    gf_rep = [K.sb("gf_rep%d" % b, [128, 1024], F32) for b in range(2)]
    xmid = [K.sb("xmid%d" % i, [128, 1024], F32) for i in range(NT)]
    dest_i = K.sb("dest_i", [128, 2 * NT], I32)
    wts = K.sb("wts", [128, 2 * NT], F32)
    wbufs = {}

    def load_expert(e):
        w1b, w3b, w2b = wbufs["w1b"], wbufs["w3b"], wbufs["w2b"]
        j = e % 2
        K.dma("pool", w1b[j][:], w1[e].rearrange("(kc p) f -> p kc f", p=128), w=[w1b[j]])
        K.dma("pool", w3b[j][:], w3[e].rearrange("(kc p) f -> p kc f", p=128), w=[w3b[j]])
        K.dma("pool", w2b[j][:], w2[e].rearrange("(fc p) d -> p fc d", p=128), w=[w2b[j]])

    K.dma("sp", cst_f[:], cst, w=[cst_f])
    K.dma("pool", cst_b[:], cst[:, 0:384], w=[cst_b])
    for b in range(2):
        K.dma("sp", gf_rep[b][:], _bcast_rows(modv[b, 5:6, :]), w=[gf_rep[b]])
    ident_f = cst_f[:, 0:128]
    slotbase = cst_f[:, 384:416]
    ident_b = cst_b[:, 0:128]
    ltri_b = cst_b[:, 128:256]
    ones_b = cst_b[:, 256:384]

    psA = [K.ps("psA%d" % j, [128, 512], F32) for j in range(4)]
    psB = [K.ps("psB%d" % j, [128, 512], F32) for j in range(2)]
    psT = [K.ps("psT%d" % j, [128, 1024], BF16) for j in range(2)]

    K.push()
    mix_sb = K.sb("mix_sb", [128, 8, T], BF16)
    wo_sb = K.sb("wo_sb", [128, 8, 1024], BF16)
    gm_rep2 = [K.sb("gm_rep%d" % b, [128, 1024], F32) for b in range(2)]
    Gf2 = [K.sb("Gf%d" % b, [128, 1024], F32) for b in range(2)]
    shf_rep2 = [K.sb("shf_rep%d" % b, [128, 1024], F32) for b in range(2)]
    tmp_rep = K.sb("tmp_rep", [128, 1024], F32)
    wr_sb = K.sb("wr_sb", [128, 8, 36], F32)
    masks_b = [K.sb("masks_b%d" % i, [128, 32], BF16) for i in range(NT)]
    zero_t = K.sb("zero_t", [128, 1024], F32)
    xt = [K.sb("xt%d" % j, [128, 1024], F32) for j in range(2)]
    ytmp = [K.sb("ytmp%d" % j, [128, 1024], F32) for j in range(2)]
    junk = K.sb("junk", [128, 1024], BF16)
    hf = [K.sb("hf%d" % j, [128, 1024], F32) for j in range(2)]
    hb = [K.sb("hb%d" % j, [128, 1024], BF16) for j in range(2)]
    hT = [K.sb("hT%d" % j, [128, 8, 128], F32) for j in range(2)]
    sm = [K.sb("sm%d" % j, [128, 256], F32) for j in range(2)]

    K.dma("pool", mix_sb[:], mixT.rearrange("(kc p) t -> p kc t", p=128), w=[mix_sb])
    K.dma("pool", wo_sb[:], wo.rearrange("(kc p) n -> p kc n", p=128), w=[wo_sb])
    for b in range(2):
        K.dma("sp", gm_rep2[b][:], _bcast_rows(modv[b, 2:3, :]), w=[gm_rep2[b]])
        K.dma("sp", shf_rep2[b][:], _bcast_rows(modv[b, 3:4, :]), w=[shf_rep2[b]])
        K.dma("sp", tmp_rep[:], _bcast_rows(modv[b, 4:5, :]), w=[tmp_rep])
        K.dma("sp", Gf2[b][:], _bcast_rows(nrm[0:1, :]), w=[Gf2[b]])
        K.dve(lambda e: e.scalar_tensor_tensor(out=Gf2[b][:], in0=tmp_rep[:], scalar=1.0, in1=Gf2[b][:],
                                               op0=ALU.add, op1=ALU.mult), r=[tmp_rep, Gf2[b]], w=[Gf2[b]])
    with nc.allow_non_contiguous_dma(reason="small router weight load"):
        K.dma("sp", wr_sb[:], wr.rearrange("(kc p) n -> p kc n", p=128), w=[wr_sb])
    K.pool(lambda e: e.memset(zero_t[:], 0.0), w=[zero_t])
    K.dma("sp", Yd[NSLOT:NSLOT + 128, :], zero_t[:], r=[zero_t])

    for i in range(NT):
        j = i % 2
        x_i = xmid[i]
        bsel = 0 if i < NT // 2 else 1
        gm_rep, Gf, shf_rep = gm_rep2[bsel], Gf2[bsel], shf_rep2[bsel]
        K.dma("sp", xt[j][:], xin[i * 128:(i + 1) * 128, :], w=[xt[j]])
        for half in range(2):
            pt = psA[half]
            for kc in range(8):
                K.pe(lambda e, kc=kc, half=half, pt=pt: e.matmul(
                    pt[:], mix_sb[:, kc, i * 128:(i + 1) * 128], wo_sb[:, kc, half * 512:(half + 1) * 512],
                    start=(kc == 0), stop=(kc == 7)), r=[mix_sb, wo_sb], w=[pt], sig=(kc == 7))
            K.dve(lambda e, half=half, pt=pt: e.tensor_tensor(
                out=ytmp[j][:, half * 512:(half + 1) * 512], in0=pt[:], in1=gm_rep[:, half * 512:(half + 1) * 512],
                op=ALU.mult), r=[pt, gm_rep], w=[ytmp[j]])
        K.pool(lambda e: e.tensor_tensor(out=x_i[:], in0=xt[j][:], in1=ytmp[j][:], op=ALU.add),
               r=[xt[j], ytmp[j]], w=[x_i])
        s = sm[j]
        K.act(lambda e: e.activation(out=junk[:], in_=x_i[:], func=AF.Square, accum_out=s[:, 0:1]),
              r=[x_i], w=[junk, s])
        K.dve(lambda e: e.tensor_scalar(out=s[:, 1:2], in0=s[:, 0:1], scalar1=1.0 / 1024, scalar2=EPS,
                                        op0=ALU.mult, op1=ALU.add), r=[s], w=[s])
        K.act(lambda e: e.sqrt(out=s[:, 2:3], in_=s[:, 1:2]), r=[s], w=[s])
        K.dve(lambda e: e.reciprocal(out=s[:, 3:4], in_=s[:, 2:3]), r=[s], w=[s])
        K.dve(lambda e: e.scalar_tensor_tensor(out=hf[j][:], in0=x_i[:], scalar=s[:, 3:4], in1=Gf[:],
                                               op0=ALU.mult, op1=ALU.mult), r=[x_i, s, Gf], w=[hf[j]])
        K.pool(lambda e: e.tensor_tensor(out=hf[j][:], in0=hf[j][:], in1=shf_rep[:], op=ALU.add),
               r=[hf[j], shf_rep], w=[hf[j]])
        K.act(lambda e: e.copy(out=hb[j][:], in_=hf[j][:]), r=[hf[j]], w=[hb[j]])
        for g in range(2):
            pt = psB[g]
            for q in range(4):
                kc = g * 4 + q
                K.pe(lambda e, kc=kc, q=q, pt=pt: e.transpose(
                    pt[:, q * 128:(q + 1) * 128], hf[j][:, kc * 128:(kc + 1) * 128], ident_f),
                    r=[hf[j], cst_f], w=[pt], sig=(q == 3))
            K.act(lambda e, g=g, pt=pt: e.copy(out=hT[j][:, g * 4:(g + 1) * 4, :],
                                               in_=pt[:].rearrange("p (a b) -> p a b", a=4)),
                  r=[pt], w=[hT[j]])
        pl = psA[2]
        for kc in range(8):
            K.pe(lambda e, kc=kc: e.matmul(pl[:, 0:36], hT[j][:, kc, :], wr_sb[:, kc, :],
                                           start=(kc == 0), stop=(kc == 7)),
                 r=[hT[j], wr_sb], w=[pl], sig=(kc == 7))
        lg = s[:, 16:52]
        gl = s[:, 16:20]
        el = s[:, 20:52].rearrange("p (g e) -> p g e", g=4)
        K.dve(lambda e: e.tensor_copy(out=lg, in_=pl[:, 0:36]), r=[pl], w=[s])
        c = lambda a, b=None: s[:, a:(a + 1 if b is None else b)]
        GMAX, GSUM, GGATE, L1, L2, DD, ED, DEN, W1, W2 = 4, 5, 6, 7, 8, 9, 10, 11, 12, 13
        OHG = (56, 60)
        GSH = (60, 64)
        ES = (64, 72)
        M1 = (72, 80)
        ES2 = (80, 88)
        M2 = (88, 96)
        MK1 = (96, 128)
        MK2 = (128, 160)
        MKU = (160, 192)
        POSB = (192, 224)
        OK = (224, 256)
        sw = [s]
        D = lambda fn: K.dve(fn, r=sw, w=sw)
        D(lambda e: e.reduce_max(out=c(GMAX), in_=gl, axis=AX.X))
        D(lambda e: e.tensor_scalar(out=c(*OHG), in0=gl, scalar1=c(GMAX), scalar2=None, op0=ALU.is_ge))
        D(lambda e: e.tensor_scalar(out=c(*GSH), in0=gl, scalar1=c(GMAX), scalar2=None, op0=ALU.subtract))
        K.act(lambda e: e.activation(out=c(*GSH), in_=c(*GSH), func=AF.Exp, accum_out=c(GSUM)), r=sw, w=sw)
        D(lambda e: e.reciprocal(out=c(GGATE), in_=c(GSUM)))
        D(lambda e: e.tensor_scalar(out=c(*ES), in0=el[:, 0, :], scalar1=c(OHG[0]), scalar2=None, op0=ALU.mult))
        for g in range(1, 4):
            D(lambda e, g=g: e.scalar_tensor_tensor(out=c(*ES), in0=el[:, g, :], scalar=c(OHG[0] + g),
                                                    in1=c(*ES), op0=ALU.mult, op1=ALU.add))
        D(lambda e: e.reduce_max(out=c(L1), in_=c(*ES), axis=AX.X))
        D(lambda e: e.tensor_scalar(out=c(*M1), in0=c(*ES), scalar1=c(L1), scalar2=None, op0=ALU.is_ge))
        D(lambda e: e.scalar_tensor_tensor(out=c(*ES2), in0=c(*M1), scalar=-1e30, in1=c(*ES),
                                           op0=ALU.mult, op1=ALU.add))
        D(lambda e: e.reduce_max(out=c(L2), in_=c(*ES2), axis=AX.X))
        D(lambda e: e.tensor_scalar(out=c(*M2), in0=c(*ES2), scalar1=c(L2), scalar2=None, op0=ALU.is_ge))
        D(lambda e: e.tensor_tensor(out=c(DD), in0=c(L2), in1=c(L1), op=ALU.subtract))
        K.act(lambda e: e.activation(out=c(ED), in_=c(DD), func=AF.Exp), r=sw, w=sw)
        D(lambda e: e.tensor_scalar(out=c(DEN), in0=c(ED), scalar1=1.0, scalar2=None, op0=ALU.add))
        D(lambda e: e.reciprocal(out=c(DEN), in_=c(DEN)))
        D(lambda e: e.tensor_tensor(out=c(W1), in0=c(GGATE), in1=c(DEN), op=ALU.mult))
        D(lambda e: e.tensor_tensor(out=c(W2), in0=c(W1), in1=c(ED), op=ALU.mult))
        ohg3 = c(*OHG).unsqueeze(2).to_broadcast([128, 4, 8])
        for (MK, MM) in ((MK1, M1), (MK2, M2)):
            D(lambda e, MK=MK, MM=MM: e.tensor_tensor(
                out=c(*MK).rearrange("p (g e) -> p g e", g=4), in0=ohg3,
                in1=c(*MM).unsqueeze(1).to_broadcast([128, 4, 8]), op=ALU.mult))
        D(lambda e: e.tensor_tensor(out=c(*MKU), in0=c(*MK1), in1=c(*MK2), op=ALU.add))
        K.dve(lambda e: e.tensor_copy(out=masks_b[i][:], in_=c(*MKU)), r=sw, w=[masks_b[i]])
        pp = psA[3]
        for i2 in range(i + 1):
            K.pe(lambda e, i2=i2: e.matmul(pp[:, 0:32], (ones_b if i2 < i else ltri_b), masks_b[i2][:],
                                           start=(i2 == 0), stop=(i2 == i)),
                 r=[cst_b, masks_b[i2]], w=[pp], sig=(i2 == i))
        K.dve(lambda e: e.tensor_tensor(out=c(*POSB), in0=pp[:, 0:32], in1=slotbase, op=ALU.add),
              r=[pp, cst_f], w=sw)
        K.dve(lambda e: e.tensor_single_scalar(out=c(*OK), in_=pp[:, 0:32], scalar=C_CAP - 0.5, op=ALU.is_lt),
              r=[pp], w=sw)
        for k, (MK, WW) in enumerate(((MK1, W1), (MK2, W2))):
            D(lambda e, MK=MK: e.tensor_tensor(out=c(*MK), in0=c(*MK), in1=c(*OK), op=ALU.mult))
            D(lambda e, MK=MK: e.reduce_sum(out=c(14), in_=c(*MK), axis=AX.X))
            D(lambda e, MK=MK: e.tensor_tensor(out=c(*MK), in0=c(*MK), in1=c(*POSB), op=ALU.mult))
            D(lambda e, MK=MK: e.reduce_sum(out=c(15), in_=c(*MK), axis=AX.X))
            D(lambda e: e.scalar_tensor_tensor(out=c(15), in0=c(14), scalar=-float(DUMMY), in1=c(15),
                                               op0=ALU.mult, op1=ALU.add))
            D(lambda e: e.tensor_scalar(out=c(15), in0=c(15), scalar1=float(DUMMY), scalar2=None, op0=ALU.add))
            K.dve(lambda e, k=k: e.tensor_copy(out=dest_i[:, 2 * i + k:2 * i + k + 1], in_=c(15)),
                  r=sw, w=[dest_i])
            K.dve(lambda e, k=k, WW=WW: e.tensor_tensor(out=wts[:, 2 * i + k:2 * i + k + 1], in0=c(WW), in1=c(14),
                                                        op=ALU.mult), r=sw, w=[wts])
            K.dmaf("pool", lambda e, k=k: e.indirect_dma_start(
                out=Xg, out_offset=bass.IndirectOffsetOnAxis(ap=dest_i[:, 2 * i + k:2 * i + k + 1], axis=0),
                in_=hb[j][:], in_offset=None), r=[dest_i, hb[j]])
    K.pop()

    K.push()
    w1b = [K.sb("w1b%d" % j, [128, 8, 512], BF16) for j in range(2)]
    w3b = [K.sb("w3b%d" % j, [128, 8, 512], BF16) for j in range(2)]
    w2b = [K.sb("w2b%d" % j, [128, 4, 1024], BF16) for j in range(2)]
    wbufs.update(w1b=w1b, w3b=w3b, w2b=w2b)
    load_expert(0)
    load_expert(1)
    xe = [K.sb("xe%d" % j, [128, NS, 1024], BF16) for j in range(2)]
    xeT = [K.sb("xeT%d" % j, [128, 8, C_CAP], BF16) for j in range(2)]
    gact = [K.sb("gact%d" % j, [128, C_CAP], F32) for j in range(2)]
    actT = [K.sb("actT%d" % j, [128, 4, C_CAP], BF16) for j in range(2)]
    ye = [K.sb("ye%d" % j, [128, 1024], F32) for j in range(3)]
    yec = 0
    for ex in range(32):
        j = ex % 2
        K.dma("sp", xe[j][:], Xg[ex * C_CAP:(ex + 1) * C_CAP, :].rearrange("(s p) d -> p s d", p=128),
              w=[xe[j]])
        for s_ in range(NS):
            pt = psT[s_ % 2]
            for kc in range(8):
                K.pe(lambda e: e.transpose(
                    pt[:, kc * 128:(kc + 1) * 128], xe[j][:, s_, kc * 128:(kc + 1) * 128], ident_b),
                    r=[xe[j], cst_b], w=[pt], sig=(kc == 7))
            K.act(lambda e: e.copy(out=xeT[j][:, :, s_ * 128:(s_ + 1) * 128],
                                   in_=pt[:].rearrange("p (a b) -> p a b", a=8)),
                  r=[pt], w=[xeT[j]])
        for fc in range(4):
            ph1 = psA[fc % 2]
            ph3 = psA[2 + fc % 2]
            for (ph, wb) in ((ph1, w1b[j]), (ph3, w3b[j])):
                for kc in range(8):
                    K.pe(lambda e: e.matmul(
                        ph[:, 0:C_CAP], wb[:, kc, fc * 128:(fc + 1) * 128], xeT[j][:, kc, :],
                        start=(kc == 0), stop=(kc == 7)), r=[wb, xeT[j]], w=[ph], sig=(kc == 7))
            ga = gact[fc % 2]
            K.act(lambda e: e.activation(out=ga[:], in_=ph1[:, 0:C_CAP], func=AF.Silu), r=[ph1], w=[ga])
            K.dve(lambda e: e.tensor_tensor(out=actT[j][:, fc, :], in0=ga[:], in1=ph3[:, 0:C_CAP],
                                            op=ALU.mult), r=[ph3, ga], w=[actT[j]])
        for s_ in range(NS):
            yt = ye[yec % 3]
            yec += 1
            for dh in range(2):
                py = psB[dh]
                for fc in range(4):
                    K.pe(lambda e: e.matmul(
                        py[:], actT[j][:, fc, s_ * 128:(s_ + 1) * 128], w2b[j][:, fc, dh * 512:(dh + 1) * 512],
                        start=(fc == 0), stop=(fc == 3)), r=[actT[j], w2b[j]], w=[py], sig=(fc == 3))
                if dh == 0:
                    K.act(lambda e: e.copy(out=yt[:, 0:512], in_=py[:]), r=[py], w=[yt])
                else:
                    K.dve(lambda e: e.tensor_copy(out=yt[:, 512:1024], in_=py[:]), r=[py], w=[yt])
            r0 = ex * C_CAP + s_ * 128
            K.dma("sp", Yd[r0:r0 + 128, :], yt[:], r=[yt])
        if ex + 2 < 32:
            load_expert(ex + 2)
    K.pop()

    K.push()
    y1 = [K.sb("y1_%d" % j, [128, 1024], F32) for j in range(2)]
    y2 = [K.sb("y2_%d" % j, [128, 1024], F32) for j in range(2)]
    xo = [K.sb("xo_%d" % j, [128, 1024], F32) for j in range(2)]
    for i in range(NT):
        j = i % 2
        for k, yy in enumerate((y1[j], y2[j])):
            K.dmaf("pool", lambda e, k=k, yy=yy: e.indirect_dma_start(
                out=yy[:], out_offset=None, in_=Yd,
                in_offset=bass.IndirectOffsetOnAxis(ap=dest_i[:, 2 * i + k:2 * i + k + 1], axis=0)),
                r=[dest_i], w=[yy])
        K.dve(lambda e: e.tensor_scalar(out=y1[j][:], in0=y1[j][:], scalar1=wts[:, 2 * i:2 * i + 1], scalar2=None,
                                        op0=ALU.mult), r=[y1[j], wts], w=[y1[j]])
        K.dve(lambda e: e.scalar_tensor_tensor(out=y2[j][:], in0=y2[j][:], scalar=wts[:, 2 * i + 1:2 * i + 2],
                                               in1=y1[j][:], op0=ALU.mult, op1=ALU.add),
              r=[y1[j], y2[j], wts], w=[y2[j]])
        K.pool(lambda e: e.tensor_tensor(out=y2[j][:], in0=y2[j][:], in1=gf_rep[0 if i < NT // 2 else 1][:], op=ALU.mult),
               r=[y2[j], gf_rep[0 if i < NT // 2 else 1]], w=[y2[j]])
        K.pool(lambda e: e.tensor_tensor(out=xo[j][:], in0=y2[j][:], in1=xmid[i][:], op=ALU.add),
               r=[y2[j], xmid[i]], w=[xo[j]])
        K.dma("sp", xout[i * 128:(i + 1) * 128, :], xo[j][:], r=[xo[j]])
    K.pop()
    K.finish()
    return nc


_PROGS = {}


def _prog(name, builder):
    if name not in _PROGS:
        _PROGS[name] = builder()
    return _PROGS[name]


def _f32(a):
    return np.ascontiguousarray(a, dtype=np.float32)


def launch_moe(x, mix, w_o, mod_l, nrm_l, w_group, w_expert, w1, w3, w2):
    nc = _prog("moe", build_moe_program)
    wr = _f32(np.concatenate([w_group, w_expert], axis=1))
    cst = moe_consts()
    w_o = _f32(w_o)
    w1 = _f32(w1)
    w3 = _f32(w3)
    w2 = _f32(w2)
    nrm = _f32(nrm_l).reshape(1, 1024)
    xf = x.reshape(128, 128, 1024)
    mf = mix.reshape(128, 128, 1024)
    modv = _f32(mod_l.reshape(2, 6, 1024))
    in_maps = []
    for c in range(NCORES):
        in_maps.append({
            "xin": _f32(xf[c::8].reshape(2048, 1024)),
            "mixT": _f32(mf[c::8].reshape(2048, 1024).T),
            "wo": w_o, "modv": modv,
            "nrm": nrm, "wr": wr, "w1": w1, "w3": w3, "w2": w2, "cst": cst,
        })
    res = run_bass_kernel_spmd(nc, in_maps, core_ids=list(range(NCORES)))
    out = np.empty((128, 128, 1024), np.float32)
    for c in range(NCORES):
        out[c::8] = res.results[c]["xout"].reshape(16, 128, 1024)
    out = out.reshape(2, 8192, 1024)
    return out


def build_mod_program():
    nc = bass.Bass("TRN2", target_bir_lowering=False)
    cT = nc.dram_tensor("cT", [128, 16], F32, kind="ExternalInput").ap()
    aw = nc.dram_tensor("aw", [4, 1024, 768], F32, kind="ExternalInput").ap()
    ab = nc.dram_tensor("ab", [1, 3072], F32, kind="ExternalInput").ap()
    mo = nc.dram_tensor("mo", [2, 3072], F32, kind="ExternalOutput").ap()
    K = KB(nc)
    c_sb = K.sb("c_sb", [128, 16], F32)
    ca = K.sb("ca", [128, 16], F32)
    bias = K.sb("bias", [2, 3072], F32)
    osb = K.sb("osb", [2, 3072], F32)
    wsb = [K.sb("wsb%d" % j, [128, 8, 768], F32) for j in range(2)]
    ps = [K.ps("ps%d" % j, [128, 512], F32) for j in range(4)]
    K.dma("sp", c_sb[:], cT, w=[c_sb])
    K.dma("sp", bias[:], ab.partition_broadcast(2), w=[bias])
    K.act(lambda e: e.activation(out=ca[:], in_=c_sb[:], func=AF.Silu), r=[c_sb], w=[ca])
    for l in range(4):
        w = wsb[l % 2]
        K.dma("sp", w[:], aw[l].rearrange("(kc p) n -> p kc n", p=128), w=[w])
        for h_, (n0, n1) in enumerate(((0, 512), (512, 768))):
            p = ps[(2 * l + h_) % 4]
            for kc in range(8):
                K.pe(lambda e: e.matmul(p[0:2, 0:n1 - n0], ca[:, 2 * kc:2 * kc + 2], w[:, kc, n0:n1],
                                        start=(kc == 0), stop=(kc == 7)), r=[ca, w], w=[p], sig=(kc == 7))
            K.dve(lambda e: e.tensor_tensor(out=osb[:, l * 768 + n0:l * 768 + n1], in0=p[0:2, 0:n1 - n0],
                                            in1=bias[:, l * 768 + n0:l * 768 + n1], op=ALU.add),
                  r=[p, bias], w=[osb])
    K.dma("sp", mo, osb[:], r=[osb])
    K.finish()
    return nc


def launch_mod(c, ada_w, ada_b):
    nc = _prog("mod", build_mod_program)
    cT = _f32(c.T.reshape(8, 128, 2).transpose(1, 0, 2).reshape(128, 16))
    in_maps = []
    for k in range(NCORES):
        sl = slice(k * 768, (k + 1) * 768)
        in_maps.append({"cT": cT, "aw": _f32(ada_w[:, :, sl]), "ab": _f32(ada_b[:, sl].reshape(1, 3072))})
    res = run_bass_kernel_spmd(nc, in_maps, core_ids=list(range(NCORES)))
    mod = np.empty((4, 2, 6144), np.float32)
    for k in range(NCORES):
        o = res.results[k]["mo"].reshape(2, 4, 768)
        mod[:, :, k * 768:(k + 1) * 768] = o.transpose(1, 0, 2)
    return mod


def emit_norm_T(K, x_t, G, SH, junk, sm, hf, hb, psT, ident_b, cst_t, hT_out_ap, hT_tile):
    K.act(lambda e: e.activation(out=junk[:], in_=x_t[:], func=AF.Square, accum_out=sm[:, 0:1]),
          r=[x_t], w=[junk, sm])
    K.dve(lambda e: e.tensor_scalar(out=sm[:, 1:2], in0=sm[:, 0:1], scalar1=1.0 / 1024, scalar2=EPS,
                                    op0=ALU.mult, op1=ALU.add), r=[sm], w=[sm])
    K.act(lambda e: e.sqrt(out=sm[:, 2:3], in_=sm[:, 1:2]), r=[sm], w=[sm])
    K.dve(lambda e: e.reciprocal(out=sm[:, 3:4], in_=sm[:, 2:3]), r=[sm], w=[sm])
    K.dve(lambda e: e.scalar_tensor_tensor(out=hf[:], in0=x_t[:], scalar=sm[:, 3:4], in1=G[:],
                                           op0=ALU.mult, op1=ALU.mult), r=[x_t, sm, G], w=[hf])
    K.pool(lambda e: e.tensor_tensor(out=hb[:], in0=hf[:], in1=SH[:], op=ALU.add), r=[hf, SH], w=[hb])
    for kc in range(8):
        K.pe(lambda e: e.transpose(psT[:, kc * 128:(kc + 1) * 128], hb[:, kc * 128:(kc + 1) * 128], ident_b),
             r=[hb, cst_t], w=[psT], sig=(kc == 7))
    K.act(lambda e: e.copy(out=hT_out_ap, in_=psT[:].rearrange("p (a b) -> p a b", a=8)),
          r=[psT], w=[hT_tile])


def emit_mod_setup(K, modv, nrm, G, SH, tmp):
    K.dma("sp", SH[:], _bcast_rows(modv[0:1, :]), w=[SH])
    K.dma("sp", tmp[:], _bcast_rows(modv[1:2, :]), w=[tmp])
    K.dma("sp", G[:], _bcast_rows(nrm[0:1, :]), w=[G])
    K.dve(lambda e: e.scalar_tensor_tensor(out=G[:], in0=tmp[:], scalar=1.0, in1=G[:],
                                           op0=ALU.add, op1=ALU.mult), r=[tmp, G], w=[G])


def hgrn_consts():
    c = np.zeros((128, 128 + 512 + 64), np.float32)
    c[:, 0:128] = np.eye(128, dtype=np.float32)
    m = np.ones(512, np.float32)
    m[0::64] = 0.0
    c[:, 128:640] = m[None, :]
    s = np.arange(64)
    c[0:64, 640:704] = (s[:, None] <= s[None, :]).astype(np.float32)
    return c


class _Stop(Exception):
    pass


def build_hgrn_program(S=8192, dbg=None):
    nc = bass.Bass("TRN2", target_bir_lowering=False)
    x = nc.dram_tensor("x", [S, 1024], F32, kind="ExternalInput").ap()
    modv = nc.dram_tensor("modv", [6, 1024], F32, kind="ExternalInput").ap()
    nrm = nc.dram_tensor("nrm", [1, 1024], F32, kind="ExternalInput").ap()
    wqf = nc.dram_tensor("wqf", [1024, 256], F32, kind="ExternalInput").ap()
    wig = nc.dram_tensor("wig", [1024, 256], F32, kind="ExternalInput").ap()
    lbl = nc.dram_tensor("lbl", [128, 4], F32, kind="ExternalInput").ap()
    og = nc.dram_tensor("og", [1, 128], F32, kind="ExternalInput").ap()
    cst = nc.dram_tensor("cst", [128, 704], F32, kind="ExternalInput").ap()
    ro = nc.dram_tensor("ro", [S, 128], F32, kind="ExternalOutput").ap()
    K = KB(nc)

    def ck(k):
        if dbg == k:
            raise _Stop()
    cst_f = K.sb("cst_f", [128, 704], F32)
    cst_b = K.sb("cst_b", [128, 128], BF16)
    G = K.sb("G", [128, 1024], F32)
    SH = K.sb("SH", [128, 1024], F32)
    tmp = K.sb("tmp", [128, 1024], F32)
    wqf_sb = K.sb("wqf_sb", [128, 8, 256], BF16)
    wig_sb = K.sb("wig_sb", [128, 8, 256], BF16)
    lb_sb = K.sb("lb_sb", [128, 16], F32)
    og_rep = K.sb("og_rep", [64, 128], F32)
    state = K.sb("state", [128, 128], F32)
    state_b = K.sb("state_b", [128, 128], BF16)
    def early(k):
        if dbg == k:
            K.finish()
            return True
        return False
    K.dma("sp", cst_f[:], cst, w=[cst_f])
    K.dma("pool", cst_b[:], cst[:, 0:128], w=[cst_b])
    if early(-1):
        return nc
    with nc.allow_non_contiguous_dma(reason="weight slices"):
        K.dma("pool", wqf_sb[:], wqf.rearrange("(kc p) n -> p kc n", p=128), w=[wqf_sb])
        K.dma("pool", wig_sb[:], wig.rearrange("(kc p) n -> p kc n", p=128), w=[wig_sb])
    if early(-2):
        return nc
    K.dma("sp", lb_sb[:, 0:4], lbl, w=[lb_sb])
    if early(-3):
        return nc
    K.dma("sp", og_rep[:], og.partition_broadcast(64), w=[og_rep])
    if early(-4):
        return nc
    emit_mod_setup(K, modv, nrm, G, SH, tmp)
    if early(-5):
        return nc
    ident_b = cst_b[:, 0:128]
    rmask = cst_f[:, 128:640]
    triT = cst_f[0:64, 640:704]
    L = lambda a, b=None: lb_sb[:, a:(a + 1 if b is None else b)]
    lw = [lb_sb]
    K.dve(lambda e: e.tensor_tensor(out=L(4), in0=L(1), in1=L(0), op=ALU.subtract), r=lw, w=lw)
    K.act(lambda e: e.activation(out=L(5), in_=L(4), func=AF.Exp), r=lw, w=lw)
    K.dve(lambda e: e.tensor_scalar(out=L(5), in0=L(5), scalar1=1.0, scalar2=None, op0=ALU.add), r=lw, w=lw)
    K.dve(lambda e: e.reciprocal(out=L(6), in_=L(5)), r=lw, w=lw)
    K.dve(lambda e: e.tensor_scalar(out=L(7), in0=L(6), scalar1=-1.0, scalar2=1.0, op0=ALU.mult, op1=ALU.add),
          r=lw, w=lw)
    K.dve(lambda e: e.tensor_tensor(out=L(8), in0=L(2), in1=L(6), op=ALU.mult), r=lw, w=lw)
    K.dve(lambda e: e.tensor_tensor(out=L(9), in0=L(3), in1=L(7), op=ALU.mult), r=lw, w=lw)
    K.dve(lambda e: e.tensor_tensor(out=L(8), in0=L(8), in1=L(9), op=ALU.add), r=lw, w=lw)
    K.dve(lambda e: e.tensor_tensor(out=L(10), in0=L(8), in1=L(6), op=ALU.subtract), r=lw, w=lw)
    K.dve(lambda e: e.tensor_scalar(out=L(11), in0=L(10), scalar1=-1.0, scalar2=1.0, op0=ALU.mult, op1=ALU.add),
          r=lw, w=lw)
    LB, OML = L(10), L(11)
    if early(-6):
        return nc
    K.dve(lambda e: e.memset(state[:], 0.0), w=[state])
    K.dve(lambda e: e.memset(state_b[:], 0.0), w=[state_b])

    psT = [K.ps("psT%d" % j, [128, 1024], BF16) for j in range(2)]
    psP = [K.ps("psP%d" % j, [128, 512], F32) for j in range(4)]
    psO = [K.ps("psO%d" % j, [128, 512], F32) for j in range(2)]

    xt = [K.sb("xt%d" % j, [128, 1024], F32) for j in range(2)]
    junk = K.sb("junk", [128, 1024], BF16)
    sm = [K.sb("sm%d" % j, [128, 8], F32) for j in range(2)]
    hf = [K.sb("hf%d" % j, [128, 1024], F32) for j in range(2)]
    hb = [K.sb("hb%d" % j, [128, 1024], BF16) for j in range(2)]
    hT = [K.sb("hT%d" % j, [128, 8, 512], BF16) for j in range(2)]
    qf = K.sb("qf", [128, 512], F32)
    ff = K.sb("ff", [128, 512], F32)
    lf = K.sb("lf", [128, 512], F32)
    kf = K.sb("kf", [128, 512], F32)
    bc = K.sb("bc", [128, 512], F32)
    e1 = K.sb("e1", [128, 512], F32)
    e2 = K.sb("e2", [128, 512], F32)
    Qt = [K.sb("Qt%d" % j, [128, 512], BF16) for j in range(2)]
    Kt = [K.sb("Kt%d" % j, [128, 512], BF16) for j in range(2)]
    Qh = [K.sb("Qh%d" % j, [128, 512], BF16) for j in range(2)]
    Kh = [K.sb("Kh%d" % j, [128, 512], BF16) for j in range(2)]
    ebe = [K.sb("ebe%d" % j, [128, 8], F32) for j in range(2)]
    vi = [K.sb("vi%d" % j, [64, 8, 128], BF16) for j in range(2)]
    gs = [K.sb("gs%d" % j, [64, 8, 128], F32) for j in range(2)]
    KhT = [K.sb("KhT%d" % j, [64, 8, 128], BF16) for j in range(2)]
    att = [K.sb("att%d" % j, [64, 8, 64], BF16) for j in range(2)]
    rsb = [K.sb("rsb%d" % j, [64, 8, 128], F32) for j in range(2)]
    so = [K.sb("so%d" % j, [64, 8], F32) for j in range(2)]
    osb = [K.sb("osb%d" % j, [64, 8, 128], F32) for j in range(2)]
    attf = K.sb("attf", [64, 512], F32)

    NB = S // 512
    try:
      ck(0)
      for blk in range(NB):
          jb = blk % 2
          for ti in range(4):
              g = blk * 4 + ti
              j = g % 2
              K.dma("sp", xt[j][:], x[g * 128:(g + 1) * 128, :], w=[xt[j]])
              emit_norm_T(K, xt[j], G, SH, junk, sm[j], hf[j], hb[j], psT[j], ident_b, cst_b,
                          hT[jb][:, :, ti * 128:(ti + 1) * 128], hT[jb])
          ck(1)
          pq, pf = psP[0], psP[1]
          for (pp, c0) in ((pq, 0), (pf, 128)):
              for kc in range(8):
                  K.pe(lambda e: e.matmul(pp[:], wqf_sb[:, kc, c0:c0 + 128], hT[jb][:, kc, :],
                                          start=(kc == 0), stop=(kc == 7)), r=[wqf_sb, hT[jb]], w=[pp], sig=(kc == 7))
          ck(11)
          for rnd in range(4):
              pv = psP[2 + rnd % 2]
              for cc in range(2):
                  c = rnd * 2 + cc
                  for kc in range(8):
                      K.pe(lambda e: e.matmul(pv[0:64, cc * 256:(cc + 1) * 256], hT[jb][:, kc, c * 64:(c + 1) * 64],
                                              wig_sb[:, kc, :], start=(kc == 0), stop=(kc == 7)),
                           r=[wig_sb, hT[jb]], w=[pv], sig=(kc == 7 and cc == 1))
              ck(12)
              ck(20 + rnd * 3)
              pv3 = pv[0:64, :].rearrange("p (c n) -> p c n", c=2)
              K.dve(lambda e: e.tensor_copy(out=vi[jb][:, rnd * 2:rnd * 2 + 2, :], in_=pv3[:, :, 0:128]),
                    r=[pv], w=[vi[jb]])
              ck(13)
              ck(21 + rnd * 3)
              K.dve(lambda e: e.tensor_copy(out=gs[jb][:, rnd * 2:rnd * 2 + 2, :], in_=pv3[:, :, 128:256]),
                    r=[pv], w=[gs[jb]])
              ck(14)
              ck(22 + rnd * 3)
          K.act(lambda e: e.activation(out=gs[jb][:], in_=gs[jb][:], func=AF.Silu), r=[gs[jb]], w=[gs[jb]])
          ck(15)
          K.dve(lambda e: e.tensor_tensor(out=gs[jb][:], in0=gs[jb][:],
                                          in1=og_rep[:].unsqueeze(1).to_broadcast([64, 8, 128]), op=ALU.mult),
                r=[gs[jb], og_rep], w=[gs[jb]])
          ck(2)
          K.act(lambda e: e.activation(out=qf[:], in_=pq[:], func=AF.Silu), r=[pq], w=[qf])
          K.act(lambda e: e.activation(out=ff[:], in_=pf[:], func=AF.Sigmoid), r=[pf], w=[ff])
          K.dve(lambda e: e.tensor_scalar(out=ff[:], in0=ff[:], scalar1=OML, scalar2=LB, op0=ALU.mult, op1=ALU.add),
                r=[ff, lb_sb], w=[ff])
          K.act(lambda e: e.activation(out=lf[:], in_=ff[:], func=AF.Ln), r=[ff], w=[lf])
          K.dve(lambda e: e.tensor_scalar(out=kf[:], in0=ff[:], scalar1=-1.0, scalar2=1.0, op0=ALU.mult, op1=ALU.add),
                r=[ff], w=[kf])
          K.dve(lambda e: e.tensor_tensor_scan(out=bc[:], data0=rmask, data1=lf[:], initial=0.0,
                                               op0=ALU.mult, op1=ALU.add), r=[lf, cst_f], w=[bc])
          b3 = bc[:].rearrange("p (c t) -> p c t", c=8)
          bm = b3[:, :, 31:32].to_broadcast([128, 8, 64])
          be = b3[:, :, 63:64].to_broadcast([128, 8, 64])
          v3 = lambda t: t[:].rearrange("p (c t) -> p c t", c=8)
          K.dve(lambda e: e.tensor_tensor(out=v3(e1), in0=b3, in1=bm, op=ALU.subtract), r=[bc], w=[e1])
          K.act(lambda e: e.activation(out=e1[:], in_=e1[:], func=AF.Exp), r=[e1], w=[e1])
          K.dve(lambda e: e.tensor_tensor(out=Qt[jb][:], in0=qf[:], in1=e1[:], op=ALU.mult), r=[qf, e1], w=[Qt[jb]])
          K.dve(lambda e: e.reciprocal(out=e1[:], in_=e1[:]), r=[e1], w=[e1])
          K.dve(lambda e: e.tensor_tensor(out=Kt[jb][:], in0=kf[:], in1=e1[:], op=ALU.mult), r=[kf, e1], w=[Kt[jb]])
          K.act(lambda e: e.activation(out=e2[:], in_=bc[:], func=AF.Exp), r=[bc], w=[e2])
          K.dve(lambda e: e.tensor_tensor(out=Qh[jb][:], in0=qf[:], in1=e2[:], op=ALU.mult), r=[qf, e2], w=[Qh[jb]])
          K.act(lambda e: e.activation(out=ebe[jb][:].unsqueeze(2), in_=b3[:, :, 63:64], func=AF.Exp),
                r=[bc], w=[ebe[jb]])
          K.dve(lambda e: e.tensor_tensor(out=v3(e2), in0=be, in1=b3, op=ALU.subtract), r=[bc], w=[e2])
          K.act(lambda e: e.activation(out=e2[:], in_=e2[:], func=AF.Exp), r=[e2], w=[e2])
          K.dve(lambda e: e.tensor_tensor(out=Kh[jb][:], in0=kf[:], in1=e2[:], op=ALU.mult), r=[kf, e2], w=[Kh[jb]])
          ck(3)
          pT = psT[jb]
          for c in range(8):
              K.pe(lambda e: e.transpose(pT[0:64, c * 128:(c + 1) * 128], Kh[jb][:, c * 64:(c + 1) * 64], ident_b),
                   r=[Kh[jb], cst_b], w=[pT], sig=(c == 7))
          K.act(lambda e: e.copy(out=KhT[jb][:], in_=pT[0:64, :].rearrange("p (c n) -> p c n", c=8)),
                r=[pT], w=[KhT[jb]])
          ck(4)
          pa = psP[0]
          for c in range(8):
              K.pe(lambda e: e.matmul(pa[0:64, c * 64:(c + 1) * 64], Kt[jb][:, c * 64:(c + 1) * 64],
                                      Qt[jb][:, c * 64:(c + 1) * 64], start=True, stop=True),
                   r=[Kt[jb], Qt[jb]], w=[pa], sig=(c == 7))
          K.dve(lambda e: e.tensor_scalar(out=attf[:], in0=pa[0:64, :], scalar1=1e30, scalar2=-1e30,
                                          op0=ALU.min, op1=ALU.max), r=[pa], w=[attf])
          K.dve(lambda e: e.tensor_tensor(out=att[jb][:], in0=attf[:].rearrange("p (c t) -> p c t", c=8),
                                          in1=triT.unsqueeze(1).to_broadcast([64, 8, 64]), op=ALU.mult),
                r=[attf, cst_f], w=[att[jb]])
          ck(5)
          for c in range(8):
              po = psO[c % 2]
              K.pe(lambda e: e.matmul(po[0:64, 0:128], att[jb][:, c, :], vi[jb][:, c, :], start=True, stop=False),
                   r=[att[jb], vi[jb]], w=[po], sig=False)
              K.pe(lambda e: e.matmul(po[0:64, 0:128], Qh[jb][:, c * 64:(c + 1) * 64], state_b[:], start=False, stop=True),
                   r=[Qh[jb], state_b], w=[po])
              pst = psP[1] if c % 2 == 0 else psP[2]
              K.pe(lambda e: e.matmul(pst[:, 0:128], KhT[jb][:, c, :], vi[jb][:, c, :], start=True, stop=True),
                   r=[KhT[jb], vi[jb]], w=[pst])
              K.dve(lambda e: e.scalar_tensor_tensor(out=state[:], in0=state[:], scalar=ebe[jb][:, c:c + 1],
                                                     in1=pst[:, 0:128], op0=ALU.mult, op1=ALU.add),
                    r=[state, ebe[jb], pst], w=[state])
              K.act(lambda e: e.copy(out=state_b[:], in_=state[:]), r=[state], w=[state_b])
              K.act(lambda e: e.copy(out=osb[jb][:, c, :], in_=po[0:64, 0:128]), r=[po], w=[osb[jb]])
          ck(6)
          s_ = so[jb]
          K.pool(lambda e: e.tensor_tensor(out=rsb[jb][:], in0=osb[jb][:], in1=osb[jb][:], op=ALU.mult),
                 r=[osb[jb]], w=[rsb[jb]])
          K.dve(lambda e: e.reduce_sum(out=s_[:], in_=rsb[jb][:], axis=AX.X), r=[rsb[jb]], w=[s_])
          K.dve(lambda e: e.tensor_scalar(out=s_[:], in0=s_[:], scalar1=1.0 / 128, scalar2=EPS, op0=ALU.mult, op1=ALU.add),
                r=[s_], w=[s_])
          K.act(lambda e: e.sqrt(out=s_[:], in_=s_[:]), r=[s_], w=[s_])
          K.dve(lambda e: e.reciprocal(out=s_[:], in_=s_[:]), r=[s_], w=[s_])
          K.dve(lambda e: e.tensor_tensor(out=rsb[jb][:], in0=osb[jb][:], in1=s_[:].unsqueeze(2).to_broadcast([64, 8, 128]),
                                          op=ALU.mult), r=[osb[jb], s_], w=[rsb[jb]])
          K.pool(lambda e: e.tensor_tensor(out=rsb[jb][:], in0=rsb[jb][:], in1=gs[jb][:], op=ALU.mult),
                 r=[rsb[jb], gs[jb]], w=[rsb[jb]])
          K.dma("sp", ro[blk * 512:(blk + 1) * 512, :].rearrange("(c t) e -> t c e", t=64), rsb[jb][:], r=[rsb[jb]])
    except _Stop:
        pass
    K.finish()
    return nc


def launch_hgrn(x, mod_l, nrm_l, w_in, lb_logits, o_gain, ei):
    nc = _prog("hgrn", build_hgrn_program)
    cst = hgrn_consts()
    base = 512 + 6 * 128 + 24
    nrm = _f32(nrm_l).reshape(1, 1024)
    in_maps = []
    for c in range(NCORES):
        b, h = c // 4, c % 4
        cq = w_in[:, base + h * 128: base + (h + 1) * 128]
        cf = w_in[:, base + 512 + h * 128: base + 512 + (h + 1) * 128]
        ci = w_in[:, base + 1024 + h * 128: base + 1024 + (h + 1) * 128]
        cg = w_in[:, base + 1536 + h * 128: base + 1536 + (h + 1) * 128]
        lbl = np.zeros((128, 4), np.float32)
        lbl[:, 0] = lb_logits[0, h * 128:(h + 1) * 128]
        lbl[:, 1] = lb_logits[1, h * 128:(h + 1) * 128]
        lbl[:, 2] = 1.0
        lbl[:, 3] = 1.0 if ei >= 1 else 0.0
        in_maps.append({
            "x": _f32(x[b]), "modv": _f32(mod_l[b].reshape(6, 1024)), "nrm": nrm,
            "wqf": _f32(np.concatenate([cq, cf], axis=1)), "wig": _f32(np.concatenate([ci, cg], axis=1)),
            "lbl": lbl, "og": _f32(o_gain).reshape(1, 128), "cst": cst,
        })
    res = run_bass_kernel_spmd(nc, in_maps, core_ids=list(range(NCORES)))
    r = np.empty((2, 8192, 512), np.float32)
    for c in range(NCORES):
        b, h = c // 4, c % 4
        r[b, :, h * 128:(h + 1) * 128] = res.results[c]["ro"]
    return r


def conv_consts():
    c = np.zeros((128, 256), np.float32)
    c[:, 0:128] = np.eye(128, dtype=np.float32)
    c[:, 128:256] = 1.0
    return c


def build_conv_program():
    nc = bass.Bass("TRN2", target_bir_lowering=False)
    T = 2048
    TH = T + 128
    xh = nc.dram_tensor("xh", [TH, 1024], F32, kind="ExternalInput").ap()
    modv = nc.dram_tensor("modv", [6, 1024], F32, kind="ExternalInput").ap()
    nrm = nc.dram_tensor("nrm", [1, 1024], F32, kind="ExternalInput").ap()
    wpw1 = nc.dram_tensor("wpw1", [1024, 2048], F32, kind="ExternalInput").ap()
    chan = nc.dram_tensor("chan", [128, 8 * 34], F32, kind="ExternalInput").ap()
    flag = nc.dram_tensor("flag", [128, 1], F32, kind="ExternalInput").ap()
    cst = nc.dram_tensor("cst", [128, 256], F32, kind="ExternalInput").ap()
    zT = nc.dram_tensor("zT", [1024, T], F32, kind="ExternalOutput").ap()
    K = KB(nc)
    cst_b = K.sb("cst_b", [128, 256], BF16)
    chan_sb = K.sb("chan_sb", [128, 8, 34], F32)
    flag_sb = K.sb("flag_sb", [128, 1], F32)
    G = K.sb("G", [128, 1024], F32)
    SH = K.sb("SH", [128, 1024], F32)
    tmp = K.sb("tmp", [128, 1024], F32)
    w1_sb = K.sb("w1_sb", [128, 8, 2048], BF16)
    hT_all = K.sb("hT_all", [128, 8, TH], BF16)
    vT = K.sb("vT", [128, 8, T], BF16)
    K.dma("pool", cst_b[:], cst, w=[cst_b])
    K.dma("sp", chan_sb[:], chan.rearrange("p (c n) -> p c n", c=8), w=[chan_sb])
    K.dma("sp", flag_sb[:], flag, w=[flag_sb])
    K.dma("pool", w1_sb[:], wpw1.rearrange("(kc p) n -> p kc n", p=128), w=[w1_sb])
    emit_mod_setup(K, modv, nrm, G, SH, tmp)
    ident_b = cst_b[:, 0:128]
    ones_b = cst_b[:, 128:256]
    psT = [K.ps("psT%d" % j, [128, 1024], BF16) for j in range(2)]
    psP = [K.ps("psP%d" % j, [128, 512], F32) for j in range(4)]
    psV = [K.ps("psV%d" % j, [128, 512], F32) for j in range(2)]

    K.push()
    xt = [K.sb("xt%d" % j, [128, 1024], F32) for j in range(2)]
    junk = K.sb("junk", [128, 1024], BF16)
    sm = [K.sb("sm%d" % j, [128, 8], F32) for j in range(2)]
    hf = [K.sb("hf%d" % j, [128, 1024], F32) for j in range(2)]
    hb = [K.sb("hb%d" % j, [128, 1024], BF16) for j in range(2)]
    hT_tiles = [Tile(hT_all.t, "hT_all_%d" % g) for g in range(TH // 128)]
    for g in range(TH // 128):
        j = g % 2
        K.dma("sp", xt[j][:], xh[g * 128:(g + 1) * 128, :], w=[xt[j]])
        emit_norm_T(K, xt[j], G, SH, junk, sm[j], hf[j], hb[j], psT[j], ident_b, cst_b,
                    hT_all[:, :, g * 128:(g + 1) * 128], hT_tiles[g])
    K.pop()

    K.push()
    uT = [K.sb("uT%d" % j, [128, TH], BF16) for j in range(2)]
    diag = [K.sb("diag%d" % j, [128, 31, 128], BF16) for j in range(2)]
    sgt = [K.sb("sgt%d" % j, [128, 512], F32) for j in range(2)]
    blocks = [(0, 128)] + [(128 + k * 512, 128 + (k + 1) * 512) for k in range(4)]
    for cc in range(8):
        j = cc % 2
        K.dve(lambda e: e.tensor_tensor(out=diag[j][:], in0=ident_b.unsqueeze(1).to_broadcast([128, 31, 128]),
                                        in1=chan_sb[:, cc, 0:31].unsqueeze(2).to_broadcast([128, 31, 128]),
                                        op=ALU.mult), r=[cst_b, chan_sb], w=[diag[j]])
        for tb, (t0, t1) in enumerate(blocks):
            n = t1 - t0
            pa = psP[(2 * tb) % 4]
            pg = psP[(2 * tb + 1) % 4]
            for (pp, c0) in ((pa, cc * 128), (pg, 1024 + cc * 128)):
                for kc in range(8):
                    K.pe(lambda e: e.matmul(pp[:, 0:n], w1_sb[:, kc, c0:c0 + 128], hT_all[:, kc, t0:t1],
                                            start=(kc == 0), stop=(kc == 7)), r=[w1_sb, hT_all], w=[pp], sig=(kc == 7))
            sg = sgt[tb % 2]
            K.act(lambda e: e.activation(out=sg[:, 0:n], in_=pg[:, 0:n], func=AF.Sigmoid), r=[pg], w=[sg])
            if tb == 0:
                K.dve(lambda e: e.scalar_tensor_tensor(out=uT[j][:, t0:t1], in0=pa[:, 0:n], scalar=flag_sb[:, 0:1],
                                                       in1=sg[:, 0:n], op0=ALU.mult, op1=ALU.mult),
                      r=[pa, sg, flag_sb], w=[uT[j]])
            else:
                K.dve(lambda e: e.tensor_tensor(out=uT[j][:, t0:t1], in0=pa[:, 0:n], in1=sg[:, 0:n], op=ALU.mult),
                      r=[pa, sg], w=[uT[j]])
        for tb in range(4):
            pv = psV[tb % 2]
            for jt in range(31):
                o = 128 + tb * 512 - 30 + jt
                K.pe(lambda e: e.matmul(pv[:], diag[j][:, jt, :], uT[j][:, o:o + 512],
                                        start=(jt == 0), stop=(jt == 30)), r=[diag[j], uT[j]], w=[pv], sig=(jt == 30))
            K.act(lambda e: e.activation(out=vT[:, cc, tb * 512:(tb + 1) * 512], in_=pv[:], func=AF.Identity,
                                         bias=chan_sb[:, cc, 31:32]), r=[pv, chan_sb], w=[vT])
    K.pop()

    K.push()
    sqb = [K.sb("sqb%d" % j, [128, 512], BF16) for j in range(2)]
    mu = K.sb("mu", [128, 512], F32)
    ex2 = K.sb("ex2", [128, 512], F32)
    rstd = K.sb("rstd", [128, 512], F32)
    t1b = [K.sb("t1b%d" % j, [128, 512], F32) for j in range(2)]
    t2b = [K.sb("t2b%d" % j, [128, 512], F32) for j in range(2)]
    zo = [K.sb("zo%d" % j, [128, 512], F32) for j in range(2)]
    for tb in range(4):
        S1, S2 = psP[0], psP[1]
        sl = slice(tb * 512, (tb + 1) * 512)
        for cc in range(8):
            sq = sqb[cc % 2]
            K.act(lambda e: e.activation(out=sq[:], in_=vT[:, cc, sl], func=AF.Square), r=[vT], w=[sq])
            K.pe(lambda e: e.matmul(S1[:], ones_b, vT[:, cc, sl], start=(cc == 0), stop=(cc == 7)),
                 r=[cst_b, vT], w=[S1], sig=(cc == 7))
            K.pe(lambda e: e.matmul(S2[:], ones_b, sq[:], start=(cc == 0), stop=(cc == 7)),
                 r=[cst_b, sq], w=[S2])
        K.act(lambda e: e.mul(out=mu[:], in_=S1[:], mul=1.0 / 1024), r=[S1], w=[mu])
        K.dve(lambda e: e.tensor_scalar(out=ex2[:], in0=S2[:], scalar1=1.0 / 1024, scalar2=EPS,
                                        op0=ALU.mult, op1=ALU.add), r=[S2], w=[ex2])
        K.pool(lambda e: e.tensor_tensor(out=rstd[:], in0=mu[:], in1=mu[:], op=ALU.mult), r=[mu], w=[rstd])
        K.dve(lambda e: e.tensor_tensor(out=ex2[:], in0=ex2[:], in1=rstd[:], op=ALU.subtract), r=[ex2, rstd], w=[ex2])
        K.act(lambda e: e.sqrt(out=ex2[:], in_=ex2[:]), r=[ex2], w=[ex2])
        K.dve(lambda e: e.reciprocal(out=rstd[:], in_=ex2[:]), r=[ex2], w=[rstd])
        for cc in range(8):
            j = cc % 2
            K.dve(lambda e: e.tensor_tensor(out=t1b[j][:], in0=vT[:, cc, sl], in1=mu[:], op=ALU.subtract),
                  r=[vT, mu], w=[t1b[j]])
            K.pool(lambda e: e.tensor_tensor(out=t2b[j][:], in0=t1b[j][:], in1=rstd[:], op=ALU.mult),
                   r=[t1b[j], rstd], w=[t2b[j]])
            K.act(lambda e: e.activation(out=zo[j][:], in_=t2b[j][:], func=AF.Silu, scale=chan_sb[:, cc, 32:33],
                                         bias=chan_sb[:, cc, 33:34]), r=[t2b[j], chan_sb], w=[zo[j]])
            K.dma("sp", zT[cc * 128:(cc + 1) * 128, sl], zo[j][:], r=[zo[j]])
    K.pop()
    K.finish()
    return nc


def launch_conv(x, mod_l, nrm_l, w_pw1, dw, dw_b, ln_g, ln_b):
    nc = _prog("conv", build_conv_program)
    cst = conv_consts()
    nrm = _f32(nrm_l).reshape(1, 1024)
    chan = np.zeros((128, 8, 34), np.float32)
    chan[:, :, 0:31] = dw.T.reshape(8, 128, 31).transpose(1, 0, 2)
    chan[:, :, 31] = dw_b.reshape(8, 128).T
    chan[:, :, 32] = ln_g.reshape(8, 128).T
    chan[:, :, 33] = ln_b.reshape(8, 128).T
    chan = _f32(chan.reshape(128, 8 * 34))
    w_pw1 = _f32(w_pw1)
    in_maps = []
    for c in range(NCORES):
        b, q = c // 4, c % 4
        xh = np.zeros((2048 + 128, 1024), np.float32)
        xh[128:] = x[b, q * 2048:(q + 1) * 2048]
        if q > 0:
            xh[:128] = x[b, q * 2048 - 128:q * 2048]
        in_maps.append({
            "xh": xh, "modv": _f32(mod_l[b].reshape(6, 1024)), "nrm": nrm, "wpw1": w_pw1, "chan": chan,
            "flag": np.full((128, 1), 1.0 if q > 0 else 0.0, np.float32), "cst": cst,
        })
    res = run_bass_kernel_spmd(nc, in_maps, core_ids=list(range(NCORES)))
    z = np.empty((2, 8192, 1024), np.float32)
    for c in range(NCORES):
        b, q = c // 4, c % 4
        z[b, q * 2048:(q + 1) * 2048] = res.results[c]["zT"].T
    return z


NSA_NEG = -1e30
_NC_ID, _NC_TLE, _NC_TGT, _NC_A, _NC_V0, _NC_KEEP, _NC_BIAS, _NC_END = 0, 128, 256, 384, 896, 1024, 1279, 1534


def nsa_consts():
    c = np.zeros((128, _NC_END), np.float32)
    p = np.arange(128)
    c[:, _NC_ID:_NC_ID + 128] = np.eye(128, dtype=np.float32)
    c[:, _NC_TLE:_NC_TLE + 128] = (p[:, None] <= p[None, :]).astype(np.float32)
    c[:, _NC_TGT:_NC_TGT + 128] = (p[:, None] > p[None, :]).astype(np.float32)
    A = np.zeros((512, 128), np.float32)
    wts = [1.0, 2.0, 2.0, 2.0, 1.0]
    for m in range(128):
        for o, w in enumerate(wts):
            n = 4 * m + o - 1
            if 0 <= n < 511:
                A[n, m] += w
    c[:, _NC_A:_NC_A + 512] = A.reshape(4, 128, 128).transpose(1, 0, 2).reshape(128, 512)
    c[:, _NC_V0:_NC_V0 + 128] = 16.0 * p[:, None] + 31.0 - p[None, :]
    keep = np.ones((128, 255), np.float32)
    bias = np.zeros((128, 255), np.float32)
    for q in range(128):
        hi = 1 if q >= 64 else 0
        for ui in range(255):
            u = ui - 127
            rel = u - hi
            if rel > 0:
                keep[q, ui], bias[q, ui] = 0.0, NSA_NEG
            elif rel == 0:
                keep[q, ui], bias[q, ui] = 0.0, 2e4
            elif rel == -1:
                keep[q, ui], bias[q, ui] = 0.0, 3e4
    c[:, _NC_KEEP:_NC_KEEP + 255] = keep
    c[:, _NC_BIAS:_NC_BIAS + 255] = bias
    wexp = np.zeros((128, 8192), np.float32)
    for m in range(128):
        wexp[m, m * 64:(m + 1) * 64] = 1.0
    return c, wexp


def build_nsa_program(S=8192, dbg=None):
    nc = bass.Bass("TRN2", target_bir_lowering=False)
    NTL = S // 128
    x = nc.dram_tensor("x", [S, 1024], F32, kind="ExternalInput").ap()
    modv = nc.dram_tensor("modv", [6, 1024], F32, kind="ExternalInput").ap()
    nrm = nc.dram_tensor("nrm", [1, 1024], F32, kind="ExternalInput").ap()
    wtm = nc.dram_tensor("wtm", [1024, 524], F32, kind="ExternalInput").ap()
    wfm = nc.dram_tensor("wfm", [1024, 128], F32, kind="ExternalInput").ap()
    wc = nc.dram_tensor("wc", [64, 2 * 32 * 64], F32, kind="ExternalInput").ap()
    peT = nc.dram_tensor("peT", [64, 32 * 128], F32, kind="ExternalInput").ap()
    gn = nc.dram_tensor("gn", [128, 448], F32, kind="ExternalInput").ap()
    cst = nc.dram_tensor("cst", [128, _NC_END], F32, kind="ExternalInput").ap()
    wexp = nc.dram_tensor("wexp", [128, 8192], F32, kind="ExternalInput").ap()
    ao = nc.dram_tensor("ao", [S, 128], F32, kind="ExternalOutput").ap()
    K = KB(nc)
    cst_f = K.sb("cst_f", [128, _NC_END], F32)
    cst_b = K.sb("cst_b", [128, _NC_V0], BF16)
    wexp_b = K.sb("wexp_b", [128, S], BF16)
    G = K.sb("G", [128, 1024], F32)
    SH = K.sb("SH", [128, 1024], F32)
    tmp = K.sb("tmp", [128, 1024], F32)
    wtm_sb = K.sb("wtm_sb", [128, 8, 524], BF16)
    wfm_sb = K.sb("wfm_sb", [128, 8, 128], BF16)
    wc_sb = K.sb("wc_sb", [64, 2, 32, 64], BF16)
    pe_sb = K.sb("pe_sb", [64, 32, 128], BF16)
    gn_sb = K.sb("gn_sb", [128, 448], F32)
    cpe_rep = K.sb("cpe_rep", [128, 128], F32)
    ksT = K.sb("ksT", [64, S], BF16)
    kwT = K.sb("kwT", [64, S], BF16)
    NCT = (S // 16 - 1 + 127) // 128
    kvcT = K.sb("kvcT", [64, max(S, NCT * 2048) + 32], BF16)
    vvcT = K.sb("vvcT", [64, max(S, NCT * 2048) + 32], BF16)
    kcT = K.sb("kcT", [64, 512], BF16)
    vs_aug = K.sb("vs_aug", [128, NTL, 65], BF16)
    vw_aug = K.sb("vw_aug", [128, NTL, 65], BF16)
    Rc = K.sb("Rc", [128, 4, 193], BF16)
    K.dma("sp", cst_f[:], cst, w=[cst_f])
    K.dma("pool", cst_b[:], cst[:, 0:_NC_V0], w=[cst_b])
    K.dma("pool", wexp_b[:], wexp[:, 0:S], w=[wexp_b])
    with nc.allow_non_contiguous_dma(reason="weight slices"):
        K.dma("pool", wtm_sb[:], wtm.rearrange("(kc p) n -> p kc n", p=128), w=[wtm_sb])
        K.dma("pool", wfm_sb[:], wfm.rearrange("(kc p) n -> p kc n", p=128), w=[wfm_sb])
    K.dma("pool", wc_sb[:], wc.rearrange("p (h l e) -> p h l e", h=2, l=32), w=[wc_sb])
    K.dma("pool", pe_sb[:], peT.rearrange("p (l n) -> p l n", l=32), w=[pe_sb])
    K.dma("sp", gn_sb[:], gn, w=[gn_sb])
    emit_mod_setup(K, modv, nrm, G, SH, tmp)
    ident_b = cst_b[:, _NC_ID:_NC_ID + 128]
    tle_b = cst_b[:, _NC_TLE:_NC_TLE + 128]
    tgt_b = cst_b[:, _NC_TGT:_NC_TGT + 128]
    V0 = cst_f[:, _NC_V0:_NC_V0 + 128]
    K.dve(lambda e: e.memset(kvcT[:], 0.0), w=[kvcT])
    K.dve(lambda e: e.memset(vvcT[:], 0.0), w=[vvcT])
    K.dve(lambda e: e.memset(kcT[:], 0.0), w=[kcT])
    K.dve(lambda e: e.memset(vs_aug[:], 1.0), w=[vs_aug])
    K.dve(lambda e: e.memset(vw_aug[:], 1.0), w=[vw_aug])
    K.dve(lambda e: e.memset(Rc[:], 1.0), w=[Rc])
    K.dve(lambda e: e.tensor_copy(out=Rc[:, :, 65:193], in_=cst_b[:, _NC_A:_NC_A + 512].rearrange("p (c m) -> p c m", c=4)),
          r=[cst_b], w=[Rc])

    psT = [K.ps("psT%d" % j, [128, 1024], BF16) for j in range(2)]
    B = [K.ps("B%d" % j, [128, 512], F32) for j in range(6)]

    for half in range(2):
        for l in range(32):
            K.pe(lambda e: e.matmul(B[1][:, half * 64:(half + 1) * 64], pe_sb[:, l, :], wc_sb[:, half, l, :],
                                    start=(l == 0), stop=(l == 31)), r=[pe_sb, wc_sb], w=[B[1]], sig=(l == 31))
    K.dve(lambda e: e.tensor_copy(out=cpe_rep[:], in_=B[1][:, 0:128]), r=[B[1]], w=[cpe_rep])

    xt = [K.sb("xt%d" % j, [128, 1024], F32) for j in range(2)]
    junk = K.sb("junk", [128, 1024], BF16)
    sm = [K.sb("sm%d" % j, [128, 8], F32) for j in range(2)]
    hf = [K.sb("hf%d" % j, [128, 1024], F32) for j in range(2)]
    hb = [K.sb("hb%d" % j, [128, 1024], BF16) for j in range(2)]
    hT = [K.sb("hT%d" % j, [128, 8, 128], BF16) for j in range(2)]
    sq = K.sb("sq", [128, 384], F32)
    qkn = K.sb("qkn", [128, 384], BF16)
    st = [K.sb("st%d" % j, [128, 80], F32) for j in range(2)]
    qT = [K.sb("qT%d" % j, [64, 512], BF16) for j in range(2)]
    kcn = K.sb("kcn", [128, 64], F32)
    kcb = K.sb("kcb", [128, 64], BF16)
    ec = [K.sb("ec%d" % j, [128, 512], BF16) for j in range(2)]
    es = [K.sb("es%d" % j, [128, 256], BF16) for j in range(6)]
    mk = [K.sb("mk%d" % j, [128, 128], BF16) for j in range(2)]
    imp = K.sb("imp", [128, 128], F32)
    score = K.sb("score", [128, 128], F32)
    sc2 = K.sb("sc2", [128, 128], F32)
    sel = K.sb("sel", [128, 128], BF16)
    selT = K.sb("selT", [128, 128], BF16)
    oacc = [K.sb("oacc%d" % j, [128, 128], F32) for j in range(2)]
    kvT_tiles = [Tile(kvcT.t, "kvcT%d" % g) for g in range(NTL)]
    vvT_tiles = [Tile(vvcT.t, "vvcT%d" % g) for g in range(NTL)]
    ks_tiles = [Tile(ksT.t, "ksT%d" % g) for g in range(NTL)]
    kw_tiles = [Tile(kwT.t, "kwT%d" % g) for g in range(NTL)]
    vs_tiles = [Tile(vs_aug.t, "vs%d" % g) for g in range(NTL)]
    vw_tiles = [Tile(vw_aug.t, "vw%d" % g) for g in range(NTL)]
    kc_tiles = [Tile(kcT.t, "kc%d" % g) for g in range(4)]
    rc_tiles = [Tile(Rc.t, "rc%d" % g) for g in range(4)]
    for t_ in vvT_tiles + kvT_tiles + ks_tiles + kw_tiles + vs_tiles + vw_tiles + kc_tiles + rc_tiles:
        t_.w = (K.engs["dve"].sid, K.engs["dve"].count)
    nslot = 0

    def ck(k, i):
        if dbg is not None and dbg == k:
            raise _Stop()
    try:
     for i in range(NTL):
         j = i % 2
         S_ = st[j]
         sl = slice(i * 128, (i + 1) * 128)
         ck(0, i)
         K.dma("sp", xt[j][:], x[sl, :], w=[xt[j]])
         emit_norm_T(K, xt[j], G, SH, junk, sm[j], hf[j], hb[j], psT[0], ident_b, cst_b, hT[j][:], hT[j])
         ck(7, i)
         pm1, pm2 = B[0], B[1]
         for kc in range(8):
             K.pe(lambda e: e.matmul(pm1[:], hT[j][:, kc, :], wtm_sb[:, kc, 0:512], start=(kc == 0), stop=(kc == 7)),
                  r=[hT[j], wtm_sb], w=[pm1], sig=(kc == 7))
         for kc in range(8):
             K.pe(lambda e: e.matmul(pm2[:, 0:12], hT[j][:, kc, :], wtm_sb[:, kc, 512:524], start=(kc == 0), stop=(kc == 7)),
                  r=[hT[j], wtm_sb], w=[pm2], sig=False)
         for half in range(2):
             for kc in range(8):
                 K.pe(lambda e: e.matmul(pm2[0:64, 128 + half * 128:256 + half * 128], wfm_sb[:, kc, half * 64:(half + 1) * 64],
                                         hT[j][:, kc, :], start=(kc == 0), stop=(kc == 7)),
                      r=[hT[j], wfm_sb], w=[pm2], sig=(kc == 7 and half == 1))
         K.dve(lambda e: e.tensor_copy(out=kvcT[:, sl], in_=pm2[0:64, 128:256]), r=[pm2], w=[kvT_tiles[i]])
         K.dve(lambda e: e.tensor_copy(out=vvcT[:, sl], in_=pm2[0:64, 256:384]), r=[pm2], w=[vvT_tiles[i]])
         K.dve(lambda e: e.tensor_copy(out=S_[:, 0:12], in_=pm2[:, 0:12]), r=[pm2], w=[S_])
         K.act(lambda e: e.activation(out=S_[:, 0:12], in_=S_[:, 0:12], func=AF.Sigmoid), r=[S_], w=[S_])
         K.dve(lambda e: e.tensor_copy(out=vs_aug[:, i, 0:64], in_=pm1[:, 384:448]), r=[pm1], w=[vs_tiles[i]])
         K.dve(lambda e: e.tensor_copy(out=vw_aug[:, i, 0:64], in_=pm1[:, 448:512]), r=[pm1], w=[vw_tiles[i]])
         ck(1, i)
         K.act(lambda e: e.activation(out=sq[:], in_=pm1[:, 0:384], func=AF.Square), r=[pm1], w=[sq])
         K.dve(lambda e: e.reduce_sum(out=S_[:, 16:22], in_=sq[:].rearrange("p (s d) -> p s d", s=6), axis=AX.X),
               r=[sq], w=[S_])
         K.dve(lambda e: e.tensor_scalar(out=S_[:, 16:22], in0=S_[:, 16:22], scalar1=1.0 / 64, scalar2=EPS,
                                         op0=ALU.mult, op1=ALU.add), r=[S_], w=[S_])
         K.act(lambda e: e.sqrt(out=S_[:, 16:22], in_=S_[:, 16:22]), r=[S_], w=[S_])
         K.dve(lambda e: e.reciprocal(out=S_[:, 16:22], in_=S_[:, 16:22]), r=[S_], w=[S_])
         K.dve(lambda e: e.tensor_tensor(out=sq[:].rearrange("p (s d) -> p s d", s=6),
                                         in0=pm1[:, 0:384].rearrange("p (s d) -> p s d", s=6),
                                         in1=S_[:, 16:22].unsqueeze(2).to_broadcast([128, 6, 64]), op=ALU.mult),
               r=[pm1, S_], w=[sq])
         K.dve(lambda e: e.tensor_tensor(out=qkn[:], in0=sq[:], in1=gn_sb[:, 0:384], op=ALU.mult),
               r=[sq, gn_sb], w=[qkn])
         pT = psT[1]
         for s6 in range(6):
             K.pe(lambda e: e.transpose(pT[0:64, s6 * 128:(s6 + 1) * 128], qkn[:, s6 * 64:(s6 + 1) * 64], ident_b),
                  r=[qkn, cst_b], w=[pT], sig=(s6 == 5))
         K.dve(lambda e: e.tensor_copy(out=qT[j][:], in_=pT[0:64, 0:512]), r=[pT], w=[qT[j]])
         K.dve(lambda e: e.tensor_copy(out=ksT[:, sl], in_=pT[0:64, 512:640]), r=[pT], w=[ks_tiles[i]])
         K.dve(lambda e: e.tensor_copy(out=kwT[:, sl], in_=pT[0:64, 640:768]), r=[pT], w=[kw_tiles[i]])
         ck(2, i)
         chi = (8 * i + 6) // 128
         ctiles = [chi] if (i % 16 != 0 or i == 0) else [chi - 1, chi]
         first_tok_tile = lambda c: c * 16
         for c in ctiles:
             rdeps = kvT_tiles[c * 16:min(NTL, c * 16 + 17)] + vvT_tiles[c * 16:min(NTL, c * 16 + 17)]
             pk, pv = B[1], B[2]
             for (pp, srcT, half) in ((pk, kvcT, 0), (pv, vvcT, 1)):
                 for l in range(32):
                     src = srcT[:, c * 2048 + l:c * 2048 + l + 2033:16]
                     K.pe(lambda e: e.matmul(pp[:, 0:64], src, wc_sb[:, half, l, :], start=(l == 0), stop=(l == 31)),
                          r=rdeps + [wc_sb], w=[pp], sig=(l == 31))
             K.dve(lambda e: e.tensor_tensor(out=Rc[:, c, 0:64], in0=pv[:, 0:64], in1=cpe_rep[:, 64:128], op=ALU.add),
                   r=[pv, cpe_rep], w=[rc_tiles[c]])
             K.dve(lambda e: e.tensor_tensor(out=kcn[:], in0=pk[:, 0:64], in1=cpe_rep[:, 0:64], op=ALU.add),
                   r=[pk, cpe_rep], w=[kcn])
             K.act(lambda e: e.activation(out=junk[:, 0:64], in_=kcn[:], func=AF.Square, accum_out=S_[:, 24:25]),
                   r=[kcn], w=[junk, S_])
             K.dve(lambda e: e.tensor_scalar(out=S_[:, 24:25], in0=S_[:, 24:25], scalar1=1.0 / 64, scalar2=EPS,
                                             op0=ALU.mult, op1=ALU.add), r=[S_], w=[S_])
             K.act(lambda e: e.sqrt(out=S_[:, 24:25], in_=S_[:, 24:25]), r=[S_], w=[S_])
             K.dve(lambda e: e.reciprocal(out=S_[:, 24:25], in_=S_[:, 24:25]), r=[S_], w=[S_])
             K.dve(lambda e: e.scalar_tensor_tensor(out=kcb[:], in0=kcn[:], scalar=S_[:, 24:25], in1=gn_sb[:, 384:448],
                                                    op0=ALU.mult, op1=ALU.mult), r=[kcn, S_, gn_sb], w=[kcb])
             K.pe(lambda e: e.transpose(pT[0:64, 768:896], kcb[:], ident_b), r=[kcb, cst_b], w=[pT])
             K.dve(lambda e: e.tensor_copy(out=kcT[:, c * 128:(c + 1) * 128], in_=pT[0:64, 768:896]), r=[pT], w=[kc_tiles[c]])
         ck(3, i)
         pO = (B[3], B[4])
         cvalid = []
         for c in range(chi + 1):
             thr = 128 * i - 2048 * c
             if thr < -96:
                 continue
             cvalid.append((c, thr))
         for ci, (c, thr) in enumerate(cvalid):
             pS = B[2]
             K.pe(lambda e: e.matmul(pS[:], kcT[:, c * 128:(c + 1) * 128], qT[j][:], start=True, stop=True),
                  r=[kc_tiles[c], qT[j]], w=[pS])
             e_ = ec[ci % 2]
             K.act(lambda e: e.activation(out=e_[:], in_=pS[:], func=AF.Exp, scale=0.125), r=[pS], w=[e_])
             if thr < 2063:
                 K.dve(lambda e: e.scalar_tensor_tensor(
                     out=e_[:].rearrange("p (h q) -> p h q", h=4), in0=V0.unsqueeze(1).to_broadcast([128, 4, 128]),
                     scalar=float(thr), in1=e_[:].rearrange("p (h q) -> p h q", h=4), op0=ALU.is_le, op1=ALU.mult),
                     r=[cst_f, e_], w=[e_])
             for h4 in range(4):
                 po = pO[h4 // 2]
                 o0 = (h4 % 2) * 193
                 K.pe(lambda e: e.matmul(po[:, o0:o0 + 193], e_[:, h4 * 128:(h4 + 1) * 128], Rc[:, c, :],
                                         start=(ci == 0 and h4 % 2 == 0), stop=(ci == len(cvalid) - 1 and h4 % 2 == 1)),
                      r=[e_, rc_tiles[c]], w=[po], sig=(h4 % 2 == 1))
         oa = oacc[j]
         if cvalid:
             for h4 in range(4):
                 po = pO[h4 // 2]
                 o0 = (h4 % 2) * 193
                 K.dve(lambda e: e.tensor_scalar(out=S_[:, 32 + h4:33 + h4], in0=po[:, o0 + 64:o0 + 65], scalar1=1e-30,
                                                 scalar2=None, op0=ALU.max), r=[po], w=[S_])
             K.dve(lambda e: e.reciprocal(out=S_[:, 32:36], in_=S_[:, 32:36]), r=[S_], w=[S_])
             for h4 in range(4):
                 po = pO[h4 // 2]
                 o0 = (h4 % 2) * 193
                 if h4 == 0:
                     K.dve(lambda e: e.tensor_scalar(out=imp[:], in0=po[:, o0 + 65:o0 + 193], scalar1=S_[:, 32:33],
                                                     scalar2=None, op0=ALU.mult), r=[po, S_], w=[imp])
                 else:
                     K.dve(lambda e: e.scalar_tensor_tensor(out=imp[:], in0=po[:, o0 + 65:o0 + 193],
                                                            scalar=S_[:, 32 + h4:33 + h4], in1=imp[:],
                                                            op0=ALU.mult, op1=ALU.add), r=[po, S_, imp], w=[imp])
             for h2 in range(2):
                 K.dve(lambda e: e.tensor_tensor(out=S_[:, 36 + h2:37 + h2], in0=S_[:, 32 + h2:33 + h2],
                                                 in1=S_[:, 3 * h2:3 * h2 + 1], op=ALU.mult), r=[S_], w=[S_])
                 K.dve(lambda e: e.tensor_scalar(out=oa[:, h2 * 64:(h2 + 1) * 64], in0=pO[0][:, h2 * 193:h2 * 193 + 64],
                                                 scalar1=S_[:, 36 + h2:37 + h2], scalar2=None, op0=ALU.mult),
                       r=[pO[0], S_], w=[oa])
         else:
             K.dve(lambda e: e.memset(imp[:], 0.0), w=[imp])
             K.dve(lambda e: e.memset(oa[:], 0.0), w=[oa])
         ck(4, i)
         u0 = 127 - 2 * i
         K.dve(lambda e: e.tensor_tensor(out=score[:], in0=imp[:], in1=cst_f[:, _NC_KEEP + u0:_NC_KEEP + u0 + 128],
                                         op=ALU.mult), r=[imp, cst_f], w=[score])
         K.dve(lambda e: e.tensor_tensor(out=score[:], in0=score[:], in1=cst_f[:, _NC_BIAS + u0:_NC_BIAS + u0 + 128],
                                         op=ALU.add), r=[score, cst_f], w=[score])
         K.dve(lambda e: e.memset(score[:, 0:1], 1e4), w=[score])
         K.dve(lambda e: e.max(out=S_[:, 40:48], in_=score[:]), r=[score], w=[S_])
         K.dve(lambda e: e.match_replace(out=sc2[:], in_to_replace=S_[:, 40:48], in_values=score[:], imm_value=-3e38),
               r=[score, S_], w=[sc2])
         K.dve(lambda e: e.max(out=S_[:, 48:56], in_=sc2[:]), r=[sc2], w=[S_])
         K.dve(lambda e: e.tensor_scalar(out=S_[:, 56:57], in0=S_[:, 55:56], scalar1=-1e29, scalar2=None, op0=ALU.max),
               r=[S_], w=[S_])
         K.dve(lambda e: e.tensor_scalar(out=sel[:], in0=score[:], scalar1=S_[:, 56:57], scalar2=None, op0=ALU.is_ge),
               r=[score, S_], w=[sel])
         K.pe(lambda e: e.transpose(pT[:, 896:1024], sel[:], ident_b), r=[sel, cst_b], w=[pT])
         K.act(lambda e: e.copy(out=selT[:], in_=pT[:, 896:1024]), r=[pT], w=[selT])
         ck(5, i)
         pO2 = B[0]
         qown = qT[j][:, 0:256]
         jobs = [("s", c) for c in range(i + 1)] + [("w", c) for c in range(max(0, i - 4), i + 1)]
         ns = i + 1
         nw = i + 1 - max(0, i - 4)
         cnt = {"s": 0, "w": 0}
         sbanks = (B[2], B[5], B[3], B[4])
         LOOK = 3

         def emit_A(kind, c, slot):
             pS2 = sbanks[slot % 4]
             kt = ks_tiles[c] if kind == "s" else kw_tiles[c]
             kk = ksT if kind == "s" else kwT
             K.pe(lambda e: e.matmul(pS2[:, 0:256], kk[:, c * 128:(c + 1) * 128], qown, start=True, stop=True),
                  r=[kt, qT[j]], w=[pS2], sig=(kind == "w"))
             e2 = es[slot % 6]
             if kind == "s":
                 K.pe(lambda e: e.matmul(pS2[:, 256:384], wexp_b[:, c * 128:(c + 1) * 128], selT[:], start=True, stop=True),
                      r=[wexp_b, selT], w=[pS2])
             K.act(lambda e: e.activation(out=e2[:], in_=pS2[:, 0:256], func=AF.Exp, scale=0.125), r=[pS2], w=[e2])
             e3 = e2[:].rearrange("p (h q) -> p h q", h=2)
             if kind == "s":
                 if c == i:
                     m_ = mk[slot % 2]
                     K.dve(lambda e: e.tensor_tensor(out=m_[:], in0=pS2[:, 256:384], in1=tle_b, op=ALU.mult),
                           r=[pS2, cst_b], w=[m_])
                     K.dve(lambda e: e.tensor_tensor(out=e3, in0=e3, in1=m_[:].unsqueeze(1).to_broadcast([128, 2, 128]),
                                                     op=ALU.mult), r=[e2, m_], w=[e2])
                 else:
                     K.dve(lambda e: e.tensor_tensor(out=e3, in0=e3,
                                                     in1=pS2[:, 256:384].unsqueeze(1).to_broadcast([128, 2, 128]),
                                                     op=ALU.mult), r=[e2, pS2], w=[e2])
             else:
                 mm = None
                 if c == i:
                     mm = tle_b
                 elif c == i - 4:
                     mm = tgt_b
                 if mm is not None:
                     K.dve(lambda e: e.tensor_tensor(out=e3, in0=e3, in1=mm.unsqueeze(1).to_broadcast([128, 2, 128]),
                                                     op=ALU.mult), r=[e2, cst_b], w=[e2])

         def emit_B(kind, c, slot):
             e2 = es[slot % 6]
             va = vs_aug if kind == "s" else vw_aug
             vt = vs_tiles[c] if kind == "s" else vw_tiles[c]
             ob = 0 if kind == "s" else 256
             n_ = ns if kind == "s" else nw
             for h2 in range(2):
                 K.pe(lambda e: e.matmul(pO2[:, ob + h2 * 65:ob + h2 * 65 + 65], e2[:, h2 * 128:(h2 + 1) * 128], va[:, c, :],
                                         start=(kind == "s" and cnt[kind] == 0 and h2 == 0),
                                         stop=(kind == "w" and cnt[kind] == n_ - 1 and h2 == 1)),
                      r=[e2, vt], w=[pO2], sig=(h2 == 1))
             cnt[kind] += 1

         pend = []
         for (kind, c) in jobs:
             emit_A(kind, c, nslot)
             pend.append((kind, c, nslot))
             nslot += 1
             if len(pend) > LOOK:
                 emit_B(*pend.pop(0))
         while pend:
             emit_B(*pend.pop(0))
         ck(6, i)
         for bi, ob in ((1, 0), (2, 256)):
             for h2 in range(2):
                 cidx = 60 + bi * 2 + h2
                 K.dve(lambda e: e.tensor_scalar(out=S_[:, cidx:cidx + 1], in0=pO2[:, ob + h2 * 65 + 64:ob + h2 * 65 + 65],
                                                 scalar1=1e-30, scalar2=None, op0=ALU.max), r=[pO2], w=[S_])
                 K.dve(lambda e: e.reciprocal(out=S_[:, cidx:cidx + 1], in_=S_[:, cidx:cidx + 1]), r=[S_], w=[S_])
                 K.dve(lambda e: e.tensor_tensor(out=S_[:, cidx:cidx + 1], in0=S_[:, cidx:cidx + 1],
                                                 in1=S_[:, 3 * h2 + bi:3 * h2 + bi + 1], op=ALU.mult), r=[S_], w=[S_])
                 K.dve(lambda e: e.scalar_tensor_tensor(out=oa[:, h2 * 64:(h2 + 1) * 64],
                                                        in0=pO2[:, ob + h2 * 65:ob + h2 * 65 + 64],
                                                        scalar=S_[:, cidx:cidx + 1], in1=oa[:, h2 * 64:(h2 + 1) * 64],
                                                        op0=ALU.mult, op1=ALU.add), r=[pO2, S_, oa], w=[oa])
         K.dma("sp", ao[sl, :], oa[:], r=[oa])
    except _Stop:
        pass
    K.finish()
    return nc


def launch_nsa(x, mod_l, nrm_l, w_in, w_ck, w_cv, pe, q_gain, k_gain, S=8192, dbg=None):
    nc = _prog("nsa%d_%s" % (S, dbg), lambda: build_nsa_program(S, dbg))
    cst, wexp = nsa_consts()
    nrm = _f32(nrm_l).reshape(1, 1024)
    wc = np.zeros((64, 2, 32, 64), np.float32)
    wc[:, 0] = w_ck.reshape(32, 64, 64).transpose(1, 0, 2)
    wc[:, 1] = w_cv.reshape(32, 64, 64).transpose(1, 0, 2)
    wc = _f32(wc.reshape(64, 4096))
    peT = _f32(np.repeat(pe.T[:, :, None], 128, axis=2).reshape(64, 4096))
    gn = np.zeros((128, 448), np.float32)
    gn[:, 0:256] = np.tile(q_gain, 4)[None, :]
    gn[:, 256:320] = k_gain[1][None, :]
    gn[:, 320:384] = k_gain[2][None, :]
    gn[:, 384:448] = k_gain[0][None, :]
    in_maps = []
    for c in range(NCORES):
        b, g, hh = c // 4, (c // 2) % 2, c % 2
        heads = [g * 4 + 2 * hh, g * 4 + 2 * hh + 1, g * 4 + 2 * (1 - hh), g * 4 + 2 * (1 - hh) + 1]
        qcols = np.concatenate([w_in[:, h * 64:(h + 1) * 64] for h in heads], axis=1)
        kv = lambda slot: w_in[:, 512 + slot * 128 + g * 64: 512 + slot * 128 + (g + 1) * 64]
        gcols = np.concatenate([w_in[:, 1280 + h * 3:1280 + h * 3 + 3] for h in heads[:2]] +
                               [w_in[:, 1280 + h * 3:1280 + h * 3 + 3] for h in heads[2:]], axis=1)
        wtm = np.concatenate([qcols, kv(2), kv(4), kv(3), kv(5), gcols], axis=1)
        wfm = np.concatenate([kv(0), kv(1)], axis=1)
        in_maps.append({"x": _f32(x[b, :S]), "modv": _f32(mod_l[b].reshape(6, 1024)), "nrm": nrm,
                        "wtm": _f32(wtm), "wfm": _f32(wfm), "wc": wc, "peT": peT, "gn": gn, "cst": cst, "wexp": wexp})
    res = run_bass_kernel_spmd(nc, in_maps, core_ids=list(range(NCORES)))
    a = np.empty((2, S, 512), np.float32)
    for c in range(NCORES):
        b, g, hh = c // 4, (c // 2) % 2, c % 2
        h0 = g * 4 + 2 * hh
        a[b, :, h0 * 64:(h0 + 2) * 64] = res.results[c]["ao"]
    return a


def kernel(x, c, ada_w, ada_b, norm_mix, norm_ffn, mix_w_in, mix_w_out, nsa_cmp_wk, nsa_cmp_wv, nsa_cmp_pe,
           nsa_q_gain, nsa_k_gain, hgrn_lb_logits, hgrn_o_gain, conv_w_pw1, conv_dw, conv_dw_b, conv_ln_g,
           conv_ln_b, conv_w_pw2, moe_w_group, moe_w_expert, moe_w1, moe_w3, moe_w2):
    x = np.asarray(x, dtype=np.float32)
    mod = launch_mod(np.asarray(c), np.asarray(ada_w), np.asarray(ada_b))
    for layer in range(4):
        i = layer // 2
        if layer % 2 == 0:
            a = launch_nsa(x, mod[layer], norm_mix[layer], np.asarray(mix_w_in[i]), np.asarray(nsa_cmp_wk[i]),
                           np.asarray(nsa_cmp_wv[i]), np.asarray(nsa_cmp_pe[i]), np.asarray(nsa_q_gain[i]),
                           np.asarray(nsa_k_gain[i]))
            r = launch_hgrn(x, mod[layer], norm_mix[layer], np.asarray(mix_w_in[i]), np.asarray(hgrn_lb_logits),
                            np.asarray(hgrn_o_gain[i]), i)
            mix = np.concatenate([a, r], axis=-1)
            w_o = mix_w_out[i]
        else:
            mix = launch_conv(x, mod[layer], norm_mix[layer], np.asarray(conv_w_pw1[i]), np.asarray(conv_dw[i]),
                              np.asarray(conv_dw_b[i]), np.asarray(conv_ln_g[i]), np.asarray(conv_ln_b[i]))
            w_o = conv_w_pw2[i]
        x = launch_moe(x, mix, np.asarray(w_o), mod[layer], norm_ffn[layer], np.asarray(moe_w_group[layer]),
                       np.asarray(moe_w_expert[layer]), np.asarray(moe_w1[layer]), np.asarray(moe_w3[layer]),
                       np.asarray(moe_w2[layer]))
    return x
```

```python
import numpy as np
from contextlib import ExitStack
import concourse.bass as bass
import concourse.mybir as mybir
from concourse.bass_utils import run_bass_kernel_spmd

F32 = mybir.dt.float32
BF16 = mybir.dt.bfloat16
I32 = mybir.dt.int32
AF = mybir.ActivationFunctionType
ALU = mybir.AluOpType
AX = mybir.AxisListType

EPS = 1e-6
NCORES = 8


class Tile:
    __slots__ = ("t", "w", "r", "name", "psum")

    def __init__(self, t, name="", psum=False):
        self.t = t
        self.w = None
        self.r = {}
        self.name = name
        self.psum = psum

    def __getitem__(self, k):
        return self.t[k]


class _Eng:
    def __init__(self, name, eng):
        self.name = name
        self.eng = eng
        self.sem = None
        self.sid = None
        self.count = 0
        self.seen = {}
        self.pending = False


class KB:
    NDMA = 24
    ROT = 3500

    def __init__(self, nc):
        self.nc = nc
        self.es = ExitStack()
        self.nuniq = 0
        self.sems = []
        self.engs = {}
        for name, eng in (("pe", nc.tensor), ("act", nc.scalar), ("dve", nc.vector),
                          ("pool", nc.gpsimd), ("sp", nc.sync)):
            e = _Eng(name, eng)
            self.engs[name] = e
            self._rot(e)
        self.slots = []
        self.qslots = {}
        self.qrr = {}
        for q in ("sp", "pool"):
            self.qslots[q] = []
            self.qrr[q] = 0
            for i in range(self.NDMA // 2):
                sid = self._newsem("dq%s%d" % (q, i))
                sl = [sid, 0]
                self.slots.append(sl)
                self.qslots[q].append(sl)
        self.scopes = []

    def _newsem(self, name):
        self.nuniq += 1
        s = self.es.enter_context(self.nc.semaphore("%s_%d" % (name, self.nuniq)))
        self.sems.append(s)
        return len(self.sems) - 1

    def _rot(self, e):
        e.sid = self._newsem("e_" + e.name)
        e.sem = self.sems[e.sid]
        e.count = 0

    def _stack(self):
        return self.scopes[-1] if self.scopes else self.es

    def sb(self, name, shape, dt):
        self.nuniq += 1
        t = self._stack().enter_context(self.nc.sbuf_tensor("%s_%d" % (name, self.nuniq), list(shape), dt))
        return Tile(t, name)

    def ps(self, name, shape, dt):
        self.nuniq += 1
        t = self._stack().enter_context(self.nc.psum_tensor("%s_%d" % (name, self.nuniq), list(shape), dt))
        return Tile(t, name, psum=True)

    def push(self):
        self.scopes.append(ExitStack())

    def pop(self):
        self.barrier()
        self.scopes.pop().close()

    def _waits(self, E, r, w):
        need = {}

        def add(ev):
            if ev is None:
                return
            s, v = ev
            if need.get(s, 0) < v:
                need[s] = v

        for t in r:
            add(t.w)
            if t.psum:
                for s, v in t.r.items():
                    if s != E.sid:
                        add((s, v))
        for t in w:
            add(t.w)
            for s, v in t.r.items():
                add((s, v))
        for s, v in need.items():
            if s == E.sid and E.name == "pe":
                continue
            if E.seen.get(s, 0) >= v:
                continue
            for F in self.engs.values():
                if F.sid == s:
                    assert v <= F.count, "wait on unsignaled event (%s waits %s)" % (E.name, F.name)
            E.eng.wait_ge(self.sems[s], v)
            E.seen[s] = v

    def _mark(self, ev, r, w):
        s, v = ev
        for t in r:
            if t.r.get(s, 0) < v:
                t.r[s] = v
        for t in w:
            t.w = ev
            t.r = {}

    def op(self, en, fn, r=(), w=(), sig=True):
        E = self.engs[en]
        if E.count >= self.ROT and not E.pending:
            self._rot(E)
        self._waits(E, r, w)
        ins = fn(E.eng)
        if sig:
            E.count += 1
            ins.then_inc(E.sem, 1)
            ev = (E.sid, E.count)
            E.pending = False
        else:
            ev = (E.sid, E.count + 1)
            E.pending = True
        self._mark(ev, r, w)
        return ins

    def pe(self, fn, r=(), w=(), sig=True):
        return self.op("pe", fn, r, w, sig)

    def act(self, fn, r=(), w=(), sig=True):
        return self.op("act", fn, r, w, sig)

    def dve(self, fn, r=(), w=(), sig=True):
        return self.op("dve", fn, r, w, sig)

    def pool(self, fn, r=(), w=(), sig=True):
        return self.op("pool", fn, r, w, sig)

    def dmaf(self, qn, fn, r=(), w=()):
        Q = self.engs[qn]
        self._waits(Q, r, w)
        slot = self.qslots[qn][self.qrr[qn]]
        self.qrr[qn] = (self.qrr[qn] + 1) % len(self.qslots[qn])
        if slot[1] >= self.ROT:
            slot[0] = self._newsem("dq")
            slot[1] = 0
        sid, val = slot
        if val > 0 and Q.seen.get(sid, 0) < val:
            Q.eng.wait_ge(self.sems[sid], val)
            Q.seen[sid] = val
        ins = fn(Q.eng)
        ins.then_inc(self.sems[sid], 16)
        slot[1] = val + 16
        self._mark((sid, slot[1]), r, w)
        return ins

    def dma(self, qn, out, in_, r=(), w=()):
        return self.dmaf(qn, lambda e: e.dma_start(out=out, in_=in_), r, w)

    def barrier(self):
        evs = []
        for F in self.engs.values():
            if F.count > 0:
                evs.append((F.sid, F.count))
        for sid, val in self.slots:
            if val > 0:
                evs.append((sid, val))
        for E in self.engs.values():
            for s, v in evs:
                if s == E.sid:
                    continue
                if E.seen.get(s, 0) >= v:
                    continue
                E.eng.wait_ge(self.sems[s], v)
                E.seen[s] = v

    def finish(self):
        self.barrier()
        while self.scopes:
            self.scopes.pop().close()
        self.es.close()


def _bcast_rows(ap_row, n=128):
    return ap_row.partition_broadcast(n)


C_CAP = 512
NS = C_CAP // 128
NSLOT = 32 * C_CAP
DUMMY = NSLOT


def moe_consts():
    c = np.zeros((128, 416), np.float32)
    c[:, 0:128] = np.eye(128, dtype=np.float32)
    tp = np.arange(128)
    c[:, 128:256] = (tp[:, None] < tp[None, :]).astype(np.float32)
    c[:, 256:384] = 1.0
    c[:, 384:416] = (np.arange(32) * C_CAP)[None, :].astype(np.float32)
    return c


def build_moe_program():
    nc = bass.Bass("TRN2", target_bir_lowering=False)
    T = 2048
    NT = T // 128
    xin = nc.dram_tensor("xin", [T, 1024], F32, kind="ExternalInput").ap()
    mixT = nc.dram_tensor("mixT", [1024, T], F32, kind="ExternalInput").ap()
    wo = nc.dram_tensor("wo", [1024, 1024], F32, kind="ExternalInput").ap()
    modv = nc.dram_tensor("modv", [2, 6, 1024], F32, kind="ExternalInput").ap()
    nrm = nc.dram_tensor("nrm", [1, 1024], F32, kind="ExternalInput").ap()
    wr = nc.dram_tensor("wr", [1024, 36], F32, kind="ExternalInput").ap()
    w1 = nc.dram_tensor("w1", [32, 1024, 512], F32, kind="ExternalInput").ap()
    w3 = nc.dram_tensor("w3", [32, 1024, 512], F32, kind="ExternalInput").ap()
    w2 = nc.dram_tensor("w2", [32, 512, 1024], F32, kind="ExternalInput").ap()
    cst = nc.dram_tensor("cst", [128, 416], F32, kind="ExternalInput").ap()
    xout = nc.dram_tensor("xout", [T, 1024], F32, kind="ExternalOutput").ap()
    Xg = nc.dram_tensor("Xg", [NSLOT + 128, 1024], BF16).ap()
    Yd = nc.dram_tensor("Yd", [NSLOT + 128, 1024], F32).ap()

    K = KB(nc)
    xg_t = Tile(Xg, "Xg")
    yd_t = Tile(Yd, "Yd")
    dram_in = Tile(None, "dram_in")

    cst_f = K.sb("cst_f", [128, 416], F32)
    cst_b = K.sb("cst_b", [128, 384], BF16)
    gf_rep = [K.sb("gf_rep%d" % b, [128, 1024], F32) for b in range(2)]
    xmid = [K.sb("xmid%d" % i, [128, 1024], F32) for i in range(NT)]
    dest_i = K.sb("dest_i", [128, 2 * NT], I32)
    wts = K.sb("wts", [128, 2 * NT], F32)
    wbufs = {}

    def load_expert(e):
        w1b, w3b, w2b = wbufs["w1b"], wbufs["w3b"], wbufs["w2b"]
        j = e % 2
        K.dma("pool", w1b[j][:], w1[e].rearrange("(kc p) f -> p kc f", p=128), w=[w1b[j]])
        K.dma("pool", w3b[j][:], w3[e].rearrange("(kc p) f -> p kc f", p=128), w=[w3b[j]])
        K.dma("pool", w2b[j][:], w2[e].rearrange("(fc p) d -> p fc d", p=128), w=[w2b[j]])

    K.dma("sp", cst_f[:], cst, w=[cst_f])
    K.dma("pool", cst_b[:], cst[:, 0:384], w=[cst_b])
    for b in range(2):
        K.dma("sp", gf_rep[b][:], _bcast_rows(modv[b, 5:6, :]), w=[gf_rep[b]])
    ident_f = cst_f[:, 0:128]
    slotbase = cst_f[:, 384:416]
    ident_b = cst_b[:, 0:128]
    ltri_b = cst_b[:, 128:256]
    ones_b = cst_b[:, 256:384]

    psA = [K.ps("psA%d" % j, [128, 512], F32) for j in range(4)]
    psB = [K.ps("psB%d" % j, [128, 512], F32) for j in range(2)]
    psT = [K.ps("psT%d" % j, [128, 1024], BF16) for j in range(2)]

    K.push()
    mix_sb = K.sb("mix_sb", [128, 8, T], BF16)
    wo_sb = K.sb("wo_sb", [128, 8, 1024], BF16)
    gm_rep2 = [K.sb("gm_rep%d" % b, [128, 1024], F32) for b in range(2)]
    Gf2 = [K.sb("Gf%d" % b, [128, 1024], F32) for b in range(2)]
    shf_rep2 = [K.sb("shf_rep%d" % b, [128, 1024], F32) for b in range(2)]
    tmp_rep = K.sb("tmp_rep", [128, 1024], F32)
    wr_sb = K.sb("wr_sb", [128, 8, 36], F32)
    masks_b = [K.sb("masks_b%d" % i, [128, 32], BF16) for i in range(NT)]
    zero_t = K.sb("zero_t", [128, 1024], F32)
    xt = [K.sb("xt%d" % j, [128, 1024], F32) for j in range(2)]
    ytmp = [K.sb("ytmp%d" % j, [128, 1024], F32) for j in range(2)]
    junk = K.sb("junk", [128, 1024], BF16)
    hf = [K.sb("hf%d" % j, [128, 1024], F32) for j in range(2)]
    hb = [K.sb("hb%d" % j, [128, 1024], BF16) for j in range(2)]
    hT = [K.sb("hT%d" % j, [128, 8, 128], F32) for j in range(2)]
    sm = [K.sb("sm%d" % j, [128, 256], F32) for j in range(2)]

    K.dma("pool", mix_sb[:], mixT.rearrange("(kc p) t -> p kc t", p=128), w=[mix_sb])
    K.dma("pool", wo_sb[:], wo.rearrange("(kc p) n -> p kc n", p=128), w=[wo_sb])
    for b in range(2):
        K.dma("sp", gm_rep2[b][:], _bcast_rows(modv[b, 2:3, :]), w=[gm_rep2[b]])
        K.dma("sp", shf_rep2[b][:], _bcast_rows(modv[b, 3:4, :]), w=[shf_rep2[b]])
        K.dma("sp", tmp_rep[:], _bcast_rows(modv[b, 4:5, :]), w=[tmp_rep])
        K.dma("sp", Gf2[b][:], _bcast_rows(nrm[0:1, :]), w=[Gf2[b]])
        K.dve(lambda e: e.scalar_tensor_tensor(out=Gf2[b][:], in0=tmp_rep[:], scalar=1.0, in1=Gf2[b][:],
                                               op0=ALU.add, op1=ALU.mult), r=[tmp_rep, Gf2[b]], w=[Gf2[b]])
    with nc.allow_non_contiguous_dma(reason="small router weight load"):
        K.dma("sp", wr_sb[:], wr.rearrange("(kc p) n -> p kc n", p=128), w=[wr_sb])
    K.pool(lambda e: e.memset(zero_t[:], 0.0), w=[zero_t])
    K.dma("sp", Yd[NSLOT:NSLOT + 128, :], zero_t[:], r=[zero_t])

    for i in range(NT):
        j = i % 2
        x_i = xmid[i]
        bsel = 0 if i < NT // 2 else 1
        gm_rep, Gf, shf_rep = gm_rep2[bsel], Gf2[bsel], shf_rep2[bsel]
        K.dma("sp", xt[j][:], xin[i * 128:(i + 1) * 128, :], w=[xt[j]])
        for half in range(2):
            pt = psA[half]
            for kc in range(8):
                K.pe(lambda e, kc=kc, half=half, pt=pt: e.matmul(
                    pt[:], mix_sb[:, kc, i * 128:(i + 1) * 128], wo_sb[:, kc, half * 512:(half + 1) * 512],
                    start=(kc == 0), stop=(kc == 7)), r=[mix_sb, wo_sb], w=[pt], sig=(kc == 7))
            K.dve(lambda e, half=half, pt=pt: e.tensor_tensor(
                out=ytmp[j][:, half * 512:(half + 1) * 512], in0=pt[:], in1=gm_rep[:, half * 512:(half + 1) * 512],
                op=ALU.mult), r=[pt, gm_rep], w=[ytmp[j]])
        K.pool(lambda e: e.tensor_tensor(out=x_i[:], in0=xt[j][:], in1=ytmp[j][:], op=ALU.add),
               r=[xt[j], ytmp[j]], w=[x_i])
        s = sm[j]
        K.act(lambda e: e.activation(out=junk[:], in_=x_i[:], func=AF.Square, accum_out=s[:, 0:1]),
              r=[x_i], w=[junk, s])
        K.dve(lambda e: e.tensor_scalar(out=s[:, 1:2], in0=s[:, 0:1], scalar1=1.0 / 1024, scalar2=EPS,
                                        op0=ALU.mult, op1=ALU.add), r=[s], w=[s])
        K.act(lambda e: e.sqrt(out=s[:, 2:3], in_=s[:, 1:2]), r=[s], w=[s])
        K.dve(lambda e: e.reciprocal(out=s[:, 3:4], in_=s[:, 2:3]), r=[s], w=[s])
        K.dve(lambda e: e.scalar_tensor_tensor(out=hf[j][:], in0=x_i[:], scalar=s[:, 3:4], in1=Gf[:],
                                               op0=ALU.mult, op1=ALU.mult), r=[x_i, s, Gf], w=[hf[j]])
        K.pool(lambda e: e.tensor_tensor(out=hf[j][:], in0=hf[j][:], in1=shf_rep[:], op=ALU.add),
               r=[hf[j], shf_rep], w=[hf[j]])
        K.act(lambda e: e.copy(out=hb[j][:], in_=hf[j][:]), r=[hf[j]], w=[hb[j]])
        for g in range(2):
            pt = psB[g]
            for q in range(4):
                kc = g * 4 + q
                K.pe(lambda e, kc=kc, q=q, pt=pt: e.transpose(
                    pt[:, q * 128:(q + 1) * 128], hf[j][:, kc * 128:(kc + 1) * 128], ident_f),
                    r=[hf[j], cst_f], w=[pt], sig=(q == 3))
            K.act(lambda e, g=g, pt=pt: e.copy(out=hT[j][:, g * 4:(g + 1) * 4, :],
                                               in_=pt[:].rearrange("p (a b) -> p a b", a=4)),
                  r=[pt], w=[hT[j]])
        pl = psA[2]
        for kc in range(8):
            K.pe(lambda e, kc=kc: e.matmul(pl[:, 0:36], hT[j][:, kc, :], wr_sb[:, kc, :],
                                           start=(kc == 0), stop=(kc == 7)),
                 r=[hT[j], wr_sb], w=[pl], sig=(kc == 7))
        lg = s[:, 16:52]
        gl = s[:, 16:20]
        el = s[:, 20:52].rearrange("p (g e) -> p g e", g=4)
        K.dve(lambda e: e.tensor_copy(out=lg, in_=pl[:, 0:36]), r=[pl], w=[s])
        c = lambda a, b=None: s[:, a:(a + 1 if b is None else b)]
        GMAX, GSUM, GGATE, L1, L2, DD, ED, DEN, W1, W2 = 4, 5, 6, 7, 8, 9, 10, 11, 12, 13
        OHG = (56, 60)
        GSH = (60, 64)
        ES = (64, 72)
        M1 = (72, 80)
        ES2 = (80, 88)
        M2 = (88, 96)
        MK1 = (96, 128)
        MK2 = (128, 160)
        MKU = (160, 192)
        POSB = (192, 224)
        OK = (224, 256)
        sw = [s]
        D = lambda fn: K.dve(fn, r=sw, w=sw)
        D(lambda e: e.reduce_max(out=c(GMAX), in_=gl, axis=AX.X))
        D(lambda e: e.tensor_scalar(out=c(*OHG), in0=gl, scalar1=c(GMAX), scalar2=None, op0=ALU.is_ge))
        D(lambda e: e.tensor_scalar(out=c(*GSH), in0=gl, scalar1=c(GMAX), scalar2=None, op0=ALU.subtract))
        K.act(lambda e: e.activation(out=c(*GSH), in_=c(*GSH), func=AF.Exp, accum_out=c(GSUM)), r=sw, w=sw)
        D(lambda e: e.reciprocal(out=c(GGATE), in_=c(GSUM)))
        D(lambda e: e.tensor_scalar(out=c(*ES), in0=el[:, 0, :], scalar1=c(OHG[0]), scalar2=None, op0=ALU.mult))
        for g in range(1, 4):
            D(lambda e, g=g: e.scalar_tensor_tensor(out=c(*ES), in0=el[:, g, :], scalar=c(OHG[0] + g),
                                                    in1=c(*ES), op0=ALU.mult, op1=ALU.add))
        D(lambda e: e.reduce_max(out=c(L1), in_=c(*ES), axis=AX.X))
        D(lambda e: e.tensor_scalar(out=c(*M1), in0=c(*ES), scalar1=c(L1), scalar2=None, op0=ALU.is_ge))
        D(lambda e: e.scalar_tensor_tensor(out=c(*ES2), in0=c(*M1), scalar=-1e30, in1=c(*ES),
                                           op0=ALU.mult, op1=ALU.add))
        D(lambda e: e.reduce_max(out=c(L2), in_=c(*ES2), axis=AX.X))
        D(lambda e: e.tensor_scalar(out=c(*M2), in0=c(*ES2), scalar1=c(L2), scalar2=None, op0=ALU.is_ge))
        D(lambda e: e.tensor_tensor(out=c(DD), in0=c(L2), in1=c(L1), op=ALU.subtract))
        K.act(lambda e: e.activation(out=c(ED), in_=c(DD), func=AF.Exp), r=sw, w=sw)
        D(lambda e: e.tensor_scalar(out=c(DEN), in0=c(ED), scalar1=1.0, scalar2=None, op0=ALU.add))
        D(lambda e: e.reciprocal(out=c(DEN), in_=c(DEN)))
        D(lambda e: e.tensor_tensor(out=c(W1), in0=c(GGATE), in1=c(DEN), op=ALU.mult))
        D(lambda e: e.tensor_tensor(out=c(W2), in0=c(W1), in1=c(ED), op=ALU.mult))
        ohg3 = c(*OHG).unsqueeze(2).to_broadcast([128, 4, 8])
        for (MK, MM) in ((MK1, M1), (MK2, M2)):
            D(lambda e, MK=MK, MM=MM: e.tensor_tensor(
                out=c(*MK).rearrange("p (g e) -> p g e", g=4), in0=ohg3,
                in1=c(*MM).unsqueeze(1).to_broadcast([128, 4, 8]), op=ALU.mult))
        D(lambda e: e.tensor_tensor(out=c(*MKU), in0=c(*MK1), in1=c(*MK2), op=ALU.add))
        K.dve(lambda e: e.tensor_copy(out=masks_b[i][:], in_=c(*MKU)), r=sw, w=[masks_b[i]])
        pp = psA[3]
        for i2 in range(i + 1):
            K.pe(lambda e, i2=i2: e.matmul(pp[:, 0:32], (ones_b if i2 < i else ltri_b), masks_b[i2][:],
                                           start=(i2 == 0), stop=(i2 == i)),
                 r=[cst_b, masks_b[i2]], w=[pp], sig=(i2 == i))
        K.dve(lambda e: e.tensor_tensor(out=c(*POSB), in0=pp[:, 0:32], in1=slotbase, op=ALU.add),
              r=[pp, cst_f], w=sw)
        K.dve(lambda e: e.tensor_single_scalar(out=c(*OK), in_=pp[:, 0:32], scalar=C_CAP - 0.5, op=ALU.is_lt),
              r=[pp], w=sw)
        for k, (MK, WW) in enumerate(((MK1, W1), (MK2, W2))):
            D(lambda e, MK=MK: e.tensor_tensor(out=c(*MK), in0=c(*MK), in1=c(*OK), op=ALU.mult))
            D(lambda e, MK=MK: e.reduce_sum(out=c(14), in_=c(*MK), axis=AX.X))
            D(lambda e, MK=MK: e.tensor_tensor(out=c(*MK), in0=c(*MK), in1=c(*POSB), op=ALU.mult))
            D(lambda e, MK=MK: e.reduce_sum(out=c(15), in_=c(*MK), axis=AX.X))
            D(lambda e: e.scalar_tensor_tensor(out=c(15), in0=c(14), scalar=-float(DUMMY), in1=c(15),
                                               op0=ALU.mult, op1=ALU.add))
            D(lambda e: e.tensor_scalar(out=c(15), in0=c(15), scalar1=float(DUMMY), scalar2=None, op0=ALU.add))
            K.dve(lambda e, k=k: e.tensor_copy(out=dest_i[:, 2 * i + k:2 * i + k + 1], in_=c(15)),
                  r=sw, w=[dest_i])
            K.dve(lambda e, k=k, WW=WW: e.tensor_tensor(out=wts[:, 2 * i + k:2 * i + k + 1], in0=c(WW), in1=c(14),
                                                        op=ALU.mult), r=sw, w=[wts])
            K.dmaf("pool", lambda e, k=k: e.indirect_dma_start(
                out=Xg, out_offset=bass.IndirectOffsetOnAxis(ap=dest_i[:, 2 * i + k:2 * i + k + 1], axis=0),
                in_=hb[j][:], in_offset=None), r=[dest_i, hb[j]])
    K.pop()

    K.push()
    w1b = [K.sb("w1b%d" % j, [128, 8, 512], BF16) for j in range(2)]
    w3b = [K.sb("w3b%d" % j, [128, 8, 512], BF16) for j in range(2)]
    w2b = [K.sb("w2b%d" % j, [128, 4, 1024], BF16) for j in range(2)]
    wbufs.update(w1b=w1b, w3b=w3b, w2b=w2b)
    load_expert(0)
    load_expert(1)
    xe = [K.sb("xe%d" % j, [128, NS, 1024], BF16) for j in range(2)]
    xeT = [K.sb("xeT%d" % j, [128, 8, C_CAP], BF16) for j in range(2)]
    gact = [K.sb("gact%d" % j, [128, C_CAP], F32) for j in range(2)]
    actT = [K.sb("actT%d" % j, [128, 4, C_CAP], BF16) for j in range(2)]
    ye = [K.sb("ye%d" % j, [128, 1024], F32) for j in range(3)]
    yec = [0]

    def stage_T(ex):
        j = ex % 2
        K.dma("sp", xe[j][:], Xg[ex * C_CAP:(ex + 1) * C_CAP, :].rearrange("(s p) d -> p s d", p=128),
              w=[xe[j]])
        for s_ in range(NS):
            pt = psT[s_ % 2]
            for kc in range(8):
                K.pe(lambda e: e.transpose(
                    pt[:, kc * 128:(kc + 1) * 128], xe[j][:, s_, kc * 128:(kc + 1) * 128], ident_b),
                    r=[xe[j], cst_b], w=[pt], sig=(kc == 7))
            if s_ % 2 == 0:
                K.act(lambda e: e.copy(out=xeT[j][:, :, s_ * 128:(s_ + 1) * 128],
                                       in_=pt[:].rearrange("p (a b) -> p a b", a=8)),
                      r=[pt], w=[xeT[j]])
            else:
                K.dve(lambda e: e.tensor_copy(out=xeT[j][:, :, s_ * 128:(s_ + 1) * 128],
                                              in_=pt[:].rearrange("p (a b) -> p a b", a=8)),
                      r=[pt], w=[xeT[j]])

    def stage_U(ex):
        j = ex % 2
        for fc in range(4):
            ph1 = psA[fc % 2]
            ph3 = psA[2 + fc % 2]
            for (ph, wb) in ((ph1, w1b[j]), (ph3, w3b[j])):
                for kc in range(8):
                    K.pe(lambda e: e.matmul(
                        ph[:, 0:C_CAP], wb[:, kc, fc * 128:(fc + 1) * 128], xeT[j][:, kc, :],
                        start=(kc == 0), stop=(kc == 7)), r=[wb, xeT[j]], w=[ph], sig=(kc == 7))
            ga = gact[fc % 2]
            K.act(lambda e: e.activation(out=ga[:], in_=ph1[:, 0:C_CAP], func=AF.Silu), r=[ph1], w=[ga])
            K.dve(lambda e: e.tensor_tensor(out=actT[j][:, fc, :], in0=ga[:], in1=ph3[:, 0:C_CAP],
                                            op=ALU.mult), r=[ph3, ga], w=[actT[j]])

    def stage_D(ex):
        j = ex % 2
        for s_ in range(NS):
            yt = ye[yec[0] % 3]
            yec[0] += 1
            for dh in range(2):
                py = psB[dh]
                for fc in range(4):
                    K.pe(lambda e: e.matmul(
                        py[:], actT[j][:, fc, s_ * 128:(s_ + 1) * 128], w2b[j][:, fc, dh * 512:(dh + 1) * 512],
                        start=(fc == 0), stop=(fc == 3)), r=[actT[j], w2b[j]], w=[py], sig=(fc == 3))
                if dh == 0:
                    K.act(lambda e: e.copy(out=yt[:, 0:512], in_=py[:]), r=[py], w=[yt])
                else:
                    K.dve(lambda e: e.tensor_copy(out=yt[:, 512:1024], in_=py[:]), r=[py], w=[yt])
            r0 = ex * C_CAP + s_ * 128
            K.dma("sp", Yd[r0:r0 + 128, :], yt[:], r=[yt])
        if ex + 2 < 32:
            load_expert(ex + 2)

    stage_T(0)
    stage_U(0)
    for ex in range(32):
        if ex + 1 < 32:
            stage_T(ex + 1)
        stage_D(ex)
        if ex + 1 < 32:
            stage_U(ex + 1)
    K.pop()

    K.push()
    y1 = [K.sb("y1_%d" % j, [128, 1024], F32) for j in range(2)]
    y2 = [K.sb("y2_%d" % j, [128, 1024], F32) for j in range(2)]
    xo = [K.sb("xo_%d" % j, [128, 1024], F32) for j in range(2)]
    for i in range(NT):
        j = i % 2
        for k, yy in enumerate((y1[j], y2[j])):
            K.dmaf("pool", lambda e, k=k, yy=yy: e.indirect_dma_start(
                out=yy[:], out_offset=None, in_=Yd,
                in_offset=bass.IndirectOffsetOnAxis(ap=dest_i[:, 2 * i + k:2 * i + k + 1], axis=0)),
                r=[dest_i], w=[yy])
        K.dve(lambda e: e.tensor_scalar(out=y1[j][:], in0=y1[j][:], scalar1=wts[:, 2 * i:2 * i + 1], scalar2=None,
                                        op0=ALU.mult), r=[y1[j], wts], w=[y1[j]])
        K.dve(lambda e: e.scalar_tensor_tensor(out=y2[j][:], in0=y2[j][:], scalar=wts[:, 2 * i + 1:2 * i + 2],
                                               in1=y1[j][:], op0=ALU.mult, op1=ALU.add),
              r=[y1[j], y2[j], wts], w=[y2[j]])
        K.pool(lambda e: e.tensor_tensor(out=y2[j][:], in0=y2[j][:], in1=gf_rep[0 if i < NT // 2 else 1][:], op=ALU.mult),
               r=[y2[j], gf_rep[0 if i < NT // 2 else 1]], w=[y2[j]])
        K.pool(lambda e: e.tensor_tensor(out=xo[j][:], in0=y2[j][:], in1=xmid[i][:], op=ALU.add),
               r=[y2[j], xmid[i]], w=[xo[j]])
        K.dma("sp", xout[i * 128:(i + 1) * 128, :], xo[j][:], r=[xo[j]])
    K.pop()
    K.finish()
    return nc


_PROGS = {}


def _prog(name, builder):
    if name not in _PROGS:
        _PROGS[name] = builder()
    return _PROGS[name]


def _f32(a):
    return np.ascontiguousarray(a, dtype=np.float32)


def launch_moe(x, mix, w_o, mod_l, nrm_l, w_group, w_expert, w1, w3, w2):
    nc = _prog("moe", build_moe_program)
    wr = _f32(np.concatenate([w_group, w_expert], axis=1))
    cst = moe_consts()
    w_o = _f32(w_o)
    w1 = _f32(w1)
    w3 = _f32(w3)
    w2 = _f32(w2)
    nrm = _f32(nrm_l).reshape(1, 1024)
    xf = x.reshape(128, 128, 1024)
    mf = mix.reshape(128, 128, 1024)
    modv = _f32(mod_l.reshape(2, 6, 1024))
    in_maps = []
    for c in range(NCORES):
        in_maps.append({
            "xin": _f32(xf[c::8].reshape(2048, 1024)),
            "mixT": _f32(mf[c::8].reshape(2048, 1024).T),
            "wo": w_o, "modv": modv,
            "nrm": nrm, "wr": wr, "w1": w1, "w3": w3, "w2": w2, "cst": cst,
        })
    res = run_bass_kernel_spmd(nc, in_maps, core_ids=list(range(NCORES)))
    out = np.empty((128, 128, 1024), np.float32)
    for c in range(NCORES):
        out[c::8] = res.results[c]["xout"].reshape(16, 128, 1024)
    out = out.reshape(2, 8192, 1024)
    return out


def build_mod_program():
    nc = bass.Bass("TRN2", target_bir_lowering=False)
    cT = nc.dram_tensor("cT", [128, 16], F32, kind="ExternalInput").ap()
    aw = nc.dram_tensor("aw", [4, 1024, 768], F32, kind="ExternalInput").ap()
    ab = nc.dram_tensor("ab", [1, 3072], F32, kind="ExternalInput").ap()
    mo = nc.dram_tensor("mo", [2, 3072], F32, kind="ExternalOutput").ap()
    K = KB(nc)
    c_sb = K.sb("c_sb", [128, 16], F32)
    ca = K.sb("ca", [128, 16], F32)
    bias = K.sb("bias", [2, 3072], F32)
    osb = K.sb("osb", [2, 3072], F32)
    wsb = [K.sb("wsb%d" % j, [128, 8, 768], F32) for j in range(2)]
    ps = [K.ps("ps%d" % j, [128, 512], F32) for j in range(4)]
    K.dma("sp", c_sb[:], cT, w=[c_sb])
    K.dma("sp", bias[:], ab.partition_broadcast(2), w=[bias])
    K.act(lambda e: e.activation(out=ca[:], in_=c_sb[:], func=AF.Silu), r=[c_sb], w=[ca])
    for l in range(4):
        w = wsb[l % 2]
        K.dma("sp", w[:], aw[l].rearrange("(kc p) n -> p kc n", p=128), w=[w])
        for h_, (n0, n1) in enumerate(((0, 512), (512, 768))):
            p = ps[(2 * l + h_) % 4]
            for kc in range(8):
                K.pe(lambda e: e.matmul(p[0:2, 0:n1 - n0], ca[:, 2 * kc:2 * kc + 2], w[:, kc, n0:n1],
                                        start=(kc == 0), stop=(kc == 7)), r=[ca, w], w=[p], sig=(kc == 7))
            K.dve(lambda e: e.tensor_tensor(out=osb[:, l * 768 + n0:l * 768 + n1], in0=p[0:2, 0:n1 - n0],
                                            in1=bias[:, l * 768 + n0:l * 768 + n1], op=ALU.add),
                  r=[p, bias], w=[osb])
    K.dma("sp", mo, osb[:], r=[osb])
    K.finish()
    return nc


def launch_mod(c, ada_w, ada_b):
    nc = _prog("mod", build_mod_program)
    cT = _f32(c.T.reshape(8, 128, 2).transpose(1, 0, 2).reshape(128, 16))
    in_maps = []
    for k in range(NCORES):
        sl = slice(k * 768, (k + 1) * 768)
        in_maps.append({"cT": cT, "aw": _f32(ada_w[:, :, sl]), "ab": _f32(ada_b[:, sl].reshape(1, 3072))})
    res = run_bass_kernel_spmd(nc, in_maps, core_ids=list(range(NCORES)))
    mod = np.empty((4, 2, 6144), np.float32)
    for k in range(NCORES):
        o = res.results[k]["mo"].reshape(2, 4, 768)
        mod[:, :, k * 768:(k + 1) * 768] = o.transpose(1, 0, 2)
    return mod


def emit_norm_T(K, x_t, G, SH, junk, sm, hf, hb, psT, ident_b, cst_t, hT_out_ap, hT_tile):
    K.act(lambda e: e.activation(out=junk[:], in_=x_t[:], func=AF.Square, accum_out=sm[:, 0:1]),
          r=[x_t], w=[junk, sm])
    K.dve(lambda e: e.tensor_scalar(out=sm[:, 1:2], in0=sm[:, 0:1], scalar1=1.0 / 1024, scalar2=EPS,
                                    op0=ALU.mult, op1=ALU.add), r=[sm], w=[sm])
    K.act(lambda e: e.sqrt(out=sm[:, 2:3], in_=sm[:, 1:2]), r=[sm], w=[sm])
    K.dve(lambda e: e.reciprocal(out=sm[:, 3:4], in_=sm[:, 2:3]), r=[sm], w=[sm])
    K.dve(lambda e: e.scalar_tensor_tensor(out=hf[:], in0=x_t[:], scalar=sm[:, 3:4], in1=G[:],
                                           op0=ALU.mult, op1=ALU.mult), r=[x_t, sm, G], w=[hf])
    K.pool(lambda e: e.tensor_tensor(out=hb[:], in0=hf[:], in1=SH[:], op=ALU.add), r=[hf, SH], w=[hb])
    for kc in range(8):
        K.pe(lambda e: e.transpose(psT[:, kc * 128:(kc + 1) * 128], hb[:, kc * 128:(kc + 1) * 128], ident_b),
             r=[hb, cst_t], w=[psT], sig=(kc == 7))
    K.act(lambda e: e.copy(out=hT_out_ap, in_=psT[:].rearrange("p (a b) -> p a b", a=8)),
          r=[psT], w=[hT_tile])


def emit_mod_setup(K, modv, nrm, G, SH, tmp):
    K.dma("sp", SH[:], _bcast_rows(modv[0:1, :]), w=[SH])
    K.dma("sp", tmp[:], _bcast_rows(modv[1:2, :]), w=[tmp])
    K.dma("sp", G[:], _bcast_rows(nrm[0:1, :]), w=[G])
    K.dve(lambda e: e.scalar_tensor_tensor(out=G[:], in0=tmp[:], scalar=1.0, in1=G[:],
                                           op0=ALU.add, op1=ALU.mult), r=[tmp, G], w=[G])


def hgrn_consts():
    c = np.zeros((128, 128 + 512 + 64), np.float32)
    c[:, 0:128] = np.eye(128, dtype=np.float32)
    m = np.ones(512, np.float32)
    m[0::64] = 0.0
    c[:, 128:640] = m[None, :]
    s = np.arange(64)
    c[0:64, 640:704] = (s[:, None] <= s[None, :]).astype(np.float32)
    return c


class _Stop(Exception):
    pass


def build_hgrn_program(S=8192, dbg=None):
    nc = bass.Bass("TRN2", target_bir_lowering=False)
    x = nc.dram_tensor("x", [S, 1024], F32, kind="ExternalInput").ap()
    modv = nc.dram_tensor("modv", [6, 1024], F32, kind="ExternalInput").ap()
    nrm = nc.dram_tensor("nrm", [1, 1024], F32, kind="ExternalInput").ap()
    wqf = nc.dram_tensor("wqf", [1024, 256], F32, kind="ExternalInput").ap()
    wig = nc.dram_tensor("wig", [1024, 256], F32, kind="ExternalInput").ap()
    lbl = nc.dram_tensor("lbl", [128, 4], F32, kind="ExternalInput").ap()
    og = nc.dram_tensor("og", [1, 128], F32, kind="ExternalInput").ap()
    cst = nc.dram_tensor("cst", [128, 704], F32, kind="ExternalInput").ap()
    ro = nc.dram_tensor("ro", [S, 128], F32, kind="ExternalOutput").ap()
    K = KB(nc)

    def ck(k):
        if dbg == k:
            raise _Stop()
    cst_f = K.sb("cst_f", [128, 704], F32)
    cst_b = K.sb("cst_b", [128, 128], BF16)
    G = K.sb("G", [128, 1024], F32)
    SH = K.sb("SH", [128, 1024], F32)
    tmp = K.sb("tmp", [128, 1024], F32)
    wqf_sb = K.sb("wqf_sb", [128, 8, 256], BF16)
    wig_sb = K.sb("wig_sb", [128, 8, 256], BF16)
    lb_sb = K.sb("lb_sb", [128, 16], F32)
    og_rep = K.sb("og_rep", [64, 128], F32)
    state = K.sb("state", [128, 128], F32)
    state_b = K.sb("state_b", [128, 128], BF16)
    def early(k):
        if dbg == k:
            K.finish()
            return True
        return False
    K.dma("sp", cst_f[:], cst, w=[cst_f])
    K.dma("pool", cst_b[:], cst[:, 0:128], w=[cst_b])
    if early(-1):
        return nc
    with nc.allow_non_contiguous_dma(reason="weight slices"):
        K.dma("pool", wqf_sb[:], wqf.rearrange("(kc p) n -> p kc n", p=128), w=[wqf_sb])
        K.dma("pool", wig_sb[:], wig.rearrange("(kc p) n -> p kc n", p=128), w=[wig_sb])
    if early(-2):
        return nc
    K.dma("sp", lb_sb[:, 0:4], lbl, w=[lb_sb])
    if early(-3):
        return nc
    K.dma("sp", og_rep[:], og.partition_broadcast(64), w=[og_rep])
    if early(-4):
        return nc
    emit_mod_setup(K, modv, nrm, G, SH, tmp)
    if early(-5):
        return nc
    ident_b = cst_b[:, 0:128]
    rmask = cst_f[:, 128:640]
    triT = cst_f[0:64, 640:704]
    L = lambda a, b=None: lb_sb[:, a:(a + 1 if b is None else b)]
    lw = [lb_sb]
    K.dve(lambda e: e.tensor_tensor(out=L(4), in0=L(1), in1=L(0), op=ALU.subtract), r=lw, w=lw)
    K.act(lambda e: e.activation(out=L(5), in_=L(4), func=AF.Exp), r=lw, w=lw)
    K.dve(lambda e: e.tensor_scalar(out=L(5), in0=L(5), scalar1=1.0, scalar2=None, op0=ALU.add), r=lw, w=lw)
    K.dve(lambda e: e.reciprocal(out=L(6), in_=L(5)), r=lw, w=lw)
    K.dve(lambda e: e.tensor_scalar(out=L(7), in0=L(6), scalar1=-1.0, scalar2=1.0, op0=ALU.mult, op1=ALU.add),
          r=lw, w=lw)
    K.dve(lambda e: e.tensor_tensor(out=L(8), in0=L(2), in1=L(6), op=ALU.mult), r=lw, w=lw)
    K.dve(lambda e: e.tensor_tensor(out=L(9), in0=L(3), in1=L(7), op=ALU.mult), r=lw, w=lw)
    K.dve(lambda e: e.tensor_tensor(out=L(8), in0=L(8), in1=L(9), op=ALU.add), r=lw, w=lw)
    K.dve(lambda e: e.tensor_tensor(out=L(10), in0=L(8), in1=L(6), op=ALU.subtract), r=lw, w=lw)
    K.dve(lambda e: e.tensor_scalar(out=L(11), in0=L(10), scalar1=-1.0, scalar2=1.0, op0=ALU.mult, op1=ALU.add),
          r=lw, w=lw)
    LB, OML = L(10), L(11)
    if early(-6):
        return nc
    K.dve(lambda e: e.memset(state[:], 0.0), w=[state])
    K.dve(lambda e: e.memset(state_b[:], 0.0), w=[state_b])

    psT = [K.ps("psT%d" % j, [128, 1024], BF16) for j in range(2)]
    psP = [K.ps("psP%d" % j, [128, 512], F32) for j in range(4)]
    psO = [K.ps("psO%d" % j, [128, 512], F32) for j in range(2)]

    xt = [K.sb("xt%d" % j, [128, 1024], F32) for j in range(2)]
    junk = K.sb("junk", [128, 1024], BF16)
    sm = [K.sb("sm%d" % j, [128, 8], F32) for j in range(2)]
    hf = [K.sb("hf%d" % j, [128, 1024], F32) for j in range(2)]
    hb = [K.sb("hb%d" % j, [128, 1024], BF16) for j in range(2)]
    hT = [K.sb("hT%d" % j, [128, 8, 512], BF16) for j in range(2)]
    qf = K.sb("qf", [128, 512], F32)
    ff = K.sb("ff", [128, 512], F32)
    lf = K.sb("lf", [128, 512], F32)
    kf = K.sb("kf", [128, 512], F32)
    bc = K.sb("bc", [128, 512], F32)
    e1 = K.sb("e1", [128, 512], F32)
    e2 = K.sb("e2", [128, 512], F32)
    Qt = [K.sb("Qt%d" % j, [128, 512], BF16) for j in range(2)]
    Kt = [K.sb("Kt%d" % j, [128, 512], BF16) for j in range(2)]
    Qh = [K.sb("Qh%d" % j, [128, 512], BF16) for j in range(2)]
    Kh = [K.sb("Kh%d" % j, [128, 512], BF16) for j in range(2)]
    ebe = [K.sb("ebe%d" % j, [128, 8], F32) for j in range(2)]
    vi = [K.sb("vi%d" % j, [64, 8, 128], BF16) for j in range(2)]
    gs = [K.sb("gs%d" % j, [64, 8, 128], F32) for j in range(2)]
    KhT = [K.sb("KhT%d" % j, [64, 8, 128], BF16) for j in range(2)]
    att = [K.sb("att%d" % j, [64, 8, 64], BF16) for j in range(2)]
    rsb = [K.sb("rsb%d" % j, [64, 8, 128], F32) for j in range(2)]
    so = [K.sb("so%d" % j, [64, 8], F32) for j in range(2)]
    osb = [K.sb("osb%d" % j, [64, 8, 128], F32) for j in range(2)]
    attf = K.sb("attf", [64, 512], F32)

    NB = S // 512
    try:
      ck(0)
      for blk in range(NB):
          jb = blk % 2
          for ti in range(4):
              g = blk * 4 + ti
              j = g % 2
              K.dma("sp", xt[j][:], x[g * 128:(g + 1) * 128, :], w=[xt[j]])
              emit_norm_T(K, xt[j], G, SH, junk, sm[j], hf[j], hb[j], psT[j], ident_b, cst_b,
                          hT[jb][:, :, ti * 128:(ti + 1) * 128], hT[jb])
          ck(1)
          pq, pf = psP[0], psP[1]
          for (pp, c0) in ((pq, 0), (pf, 128)):
              for kc in range(8):
                  K.pe(lambda e: e.matmul(pp[:], wqf_sb[:, kc, c0:c0 + 128], hT[jb][:, kc, :],
                                          start=(kc == 0), stop=(kc == 7)), r=[wqf_sb, hT[jb]], w=[pp], sig=(kc == 7))
          ck(11)
          for rnd in range(4):
              pv = psP[2 + rnd % 2]
              for cc in range(2):
                  c = rnd * 2 + cc
                  for kc in range(8):
                      K.pe(lambda e: e.matmul(pv[0:64, cc * 256:(cc + 1) * 256], hT[jb][:, kc, c * 64:(c + 1) * 64],
                                              wig_sb[:, kc, :], start=(kc == 0), stop=(kc == 7)),
                           r=[wig_sb, hT[jb]], w=[pv], sig=(kc == 7 and cc == 1))
              ck(12)
              ck(20 + rnd * 3)
              pv3 = pv[0:64, :].rearrange("p (c n) -> p c n", c=2)
              K.dve(lambda e: e.tensor_copy(out=vi[jb][:, rnd * 2:rnd * 2 + 2, :], in_=pv3[:, :, 0:128]),
                    r=[pv], w=[vi[jb]])
              ck(13)
              ck(21 + rnd * 3)
              K.dve(lambda e: e.tensor_copy(out=gs[jb][:, rnd * 2:rnd * 2 + 2, :], in_=pv3[:, :, 128:256]),
                    r=[pv], w=[gs[jb]])
              ck(14)
              ck(22 + rnd * 3)
          K.act(lambda e: e.activation(out=gs[jb][:], in_=gs[jb][:], func=AF.Silu), r=[gs[jb]], w=[gs[jb]])
          ck(15)
          K.dve(lambda e: e.tensor_tensor(out=gs[jb][:], in0=gs[jb][:],
                                          in1=og_rep[:].unsqueeze(1).to_broadcast([64, 8, 128]), op=ALU.mult),
                r=[gs[jb], og_rep], w=[gs[jb]])
          ck(2)
          K.act(lambda e: e.activation(out=qf[:], in_=pq[:], func=AF.Silu), r=[pq], w=[qf])
          K.act(lambda e: e.activation(out=ff[:], in_=pf[:], func=AF.Sigmoid), r=[pf], w=[ff])
          K.dve(lambda e: e.tensor_scalar(out=ff[:], in0=ff[:], scalar1=OML, scalar2=LB, op0=ALU.mult, op1=ALU.add),
                r=[ff, lb_sb], w=[ff])
          K.act(lambda e: e.activation(out=lf[:], in_=ff[:], func=AF.Ln), r=[ff], w=[lf])
          K.dve(lambda e: e.tensor_scalar(out=kf[:], in0=ff[:], scalar1=-1.0, scalar2=1.0, op0=ALU.mult, op1=ALU.add),
                r=[ff], w=[kf])
          K.dve(lambda e: e.tensor_tensor_scan(out=bc[:], data0=rmask, data1=lf[:], initial=0.0,
                                               op0=ALU.mult, op1=ALU.add), r=[lf, cst_f], w=[bc])
          b3 = bc[:].rearrange("p (c t) -> p c t", c=8)
          bm = b3[:, :, 31:32].to_broadcast([128, 8, 64])
          be = b3[:, :, 63:64].to_broadcast([128, 8, 64])
          v3 = lambda t: t[:].rearrange("p (c t) -> p c t", c=8)
          K.dve(lambda e: e.tensor_tensor(out=v3(e1), in0=b3, in1=bm, op=ALU.subtract), r=[bc], w=[e1])
          K.act(lambda e: e.activation(out=e1[:], in_=e1[:], func=AF.Exp), r=[e1], w=[e1])
          K.dve(lambda e: e.tensor_tensor(out=Qt[jb][:], in0=qf[:], in1=e1[:], op=ALU.mult), r=[qf, e1], w=[Qt[jb]])
          K.dve(lambda e: e.reciprocal(out=e1[:], in_=e1[:]), r=[e1], w=[e1])
          K.dve(lambda e: e.tensor_tensor(out=Kt[jb][:], in0=kf[:], in1=e1[:], op=ALU.mult), r=[kf, e1], w=[Kt[jb]])
          K.act(lambda e: e.activation(out=e2[:], in_=bc[:], func=AF.Exp), r=[bc], w=[e2])
          K.dve(lambda e: e.tensor_tensor(out=Qh[jb][:], in0=qf[:], in1=e2[:], op=ALU.mult), r=[qf, e2], w=[Qh[jb]])
          K.act(lambda e: e.activation(out=ebe[jb][:].unsqueeze(2), in_=b3[:, :, 63:64], func=AF.Exp),
                r=[bc], w=[ebe[jb]])
          K.dve(lambda e: e.tensor_tensor(out=v3(e2), in0=be, in1=b3, op=ALU.subtract), r=[bc], w=[e2])
          K.act(lambda e: e.activation(out=e2[:], in_=e2[:], func=AF.Exp), r=[e2], w=[e2])
          K.dve(lambda e: e.tensor_tensor(out=Kh[jb][:], in0=kf[:], in1=e2[:], op=ALU.mult), r=[kf, e2], w=[Kh[jb]])
          ck(3)
          pT = psT[jb]
          for c in range(8):
              K.pe(lambda e: e.transpose(pT[0:64, c * 128:(c + 1) * 128], Kh[jb][:, c * 64:(c + 1) * 64], ident_b),
                   r=[Kh[jb], cst_b], w=[pT], sig=(c == 7))
          K.act(lambda e: e.copy(out=KhT[jb][:], in_=pT[0:64, :].rearrange("p (c n) -> p c n", c=8)),
                r=[pT], w=[KhT[jb]])
          ck(4)
          pa = psP[0]
          for c in range(8):
              K.pe(lambda e: e.matmul(pa[0:64, c * 64:(c + 1) * 64], Kt[jb][:, c * 64:(c + 1) * 64],
                                      Qt[jb][:, c * 64:(c + 1) * 64], start=True, stop=True),
                   r=[Kt[jb], Qt[jb]], w=[pa], sig=(c == 7))
          K.dve(lambda e: e.tensor_scalar(out=attf[:], in0=pa[0:64, :], scalar1=1e30, scalar2=-1e30,
                                          op0=ALU.min, op1=ALU.max), r=[pa], w=[attf])
          K.dve(lambda e: e.tensor_tensor(out=att[jb][:], in0=attf[:].rearrange("p (c t) -> p c t", c=8),
                                          in1=triT.unsqueeze(1).to_broadcast([64, 8, 64]), op=ALU.mult),
                r=[attf, cst_f], w=[att[jb]])
          ck(5)
          for c in range(8):
              po = psO[c % 2]
              K.pe(lambda e: e.matmul(po[0:64, 0:128], att[jb][:, c, :], vi[jb][:, c, :], start=True, stop=False),
                   r=[att[jb], vi[jb]], w=[po], sig=False)
              K.pe(lambda e: e.matmul(po[0:64, 0:128], Qh[jb][:, c * 64:(c + 1) * 64], state_b[:], start=False, stop=True),
                   r=[Qh[jb], state_b], w=[po])
              pst = psP[1] if c % 2 == 0 else psP[2]
              K.pe(lambda e: e.matmul(pst[:, 0:128], KhT[jb][:, c, :], vi[jb][:, c, :], start=True, stop=True),
                   r=[KhT[jb], vi[jb]], w=[pst])
              K.dve(lambda e: e.scalar_tensor_tensor(out=state[:], in0=state[:], scalar=ebe[jb][:, c:c + 1],
                                                     in1=pst[:, 0:128], op0=ALU.mult, op1=ALU.add),
                    r=[state, ebe[jb], pst], w=[state])
              K.act(lambda e: e.copy(out=state_b[:], in_=state[:]), r=[state], w=[state_b])
              K.act(lambda e: e.copy(out=osb[jb][:, c, :], in_=po[0:64, 0:128]), r=[po], w=[osb[jb]])
          ck(6)
          s_ = so[jb]
          K.pool(lambda e: e.tensor_tensor(out=rsb[jb][:], in0=osb[jb][:], in1=osb[jb][:], op=ALU.mult),
                 r=[osb[jb]], w=[rsb[jb]])
          K.dve(lambda e: e.reduce_sum(out=s_[:], in_=rsb[jb][:], axis=AX.X), r=[rsb[jb]], w=[s_])
          K.dve(lambda e: e.tensor_scalar(out=s_[:], in0=s_[:], scalar1=1.0 / 128, scalar2=EPS, op0=ALU.mult, op1=ALU.add),
                r=[s_], w=[s_])
          K.act(lambda e: e.sqrt(out=s_[:], in_=s_[:]), r=[s_], w=[s_])
          K.dve(lambda e: e.reciprocal(out=s_[:], in_=s_[:]), r=[s_], w=[s_])
          K.dve(lambda e: e.tensor_tensor(out=rsb[jb][:], in0=osb[jb][:], in1=s_[:].unsqueeze(2).to_broadcast([64, 8, 128]),
                                          op=ALU.mult), r=[osb[jb], s_], w=[rsb[jb]])
          K.pool(lambda e: e.tensor_tensor(out=rsb[jb][:], in0=rsb[jb][:], in1=gs[jb][:], op=ALU.mult),
                 r=[rsb[jb], gs[jb]], w=[rsb[jb]])
          K.dma("sp", ro[blk * 512:(blk + 1) * 512, :].rearrange("(c t) e -> t c e", t=64), rsb[jb][:], r=[rsb[jb]])
    except _Stop:
        pass
    K.finish()
    return nc


def launch_hgrn(x, mod_l, nrm_l, w_in, lb_logits, o_gain, ei):
    nc = _prog("hgrn", build_hgrn_program)
    cst = hgrn_consts()
    base = 512 + 6 * 128 + 24
    nrm = _f32(nrm_l).reshape(1, 1024)
    in_maps = []
    for c in range(NCORES):
        b, h = c // 4, c % 4
        cq = w_in[:, base + h * 128: base + (h + 1) * 128]
        cf = w_in[:, base + 512 + h * 128: base + 512 + (h + 1) * 128]
        ci = w_in[:, base + 1024 + h * 128: base + 1024 + (h + 1) * 128]
        cg = w_in[:, base + 1536 + h * 128: base + 1536 + (h + 1) * 128]
        lbl = np.zeros((128, 4), np.float32)
        lbl[:, 0] = lb_logits[0, h * 128:(h + 1) * 128]
        lbl[:, 1] = lb_logits[1, h * 128:(h + 1) * 128]
        lbl[:, 2] = 1.0
        lbl[:, 3] = 1.0 if ei >= 1 else 0.0
        in_maps.append({
            "x": _f32(x[b]), "modv": _f32(mod_l[b].reshape(6, 1024)), "nrm": nrm,
            "wqf": _f32(np.concatenate([cq, cf], axis=1)), "wig": _f32(np.concatenate([ci, cg], axis=1)),
            "lbl": lbl, "og": _f32(o_gain).reshape(1, 128), "cst": cst,
        })
    res = run_bass_kernel_spmd(nc, in_maps, core_ids=list(range(NCORES)))
    r = np.empty((2, 8192, 512), np.float32)
    for c in range(NCORES):
        b, h = c // 4, c % 4
        r[b, :, h * 128:(h + 1) * 128] = res.results[c]["ro"]
    return r


def conv_consts():
    c = np.zeros((128, 256), np.float32)
    c[:, 0:128] = np.eye(128, dtype=np.float32)
    c[:, 128:256] = 1.0
    return c


def build_conv_program():
    nc = bass.Bass("TRN2", target_bir_lowering=False)
    T = 2048
    TH = T + 128
    xh = nc.dram_tensor("xh", [TH, 1024], F32, kind="ExternalInput").ap()
    modv = nc.dram_tensor("modv", [6, 1024], F32, kind="ExternalInput").ap()
    nrm = nc.dram_tensor("nrm", [1, 1024], F32, kind="ExternalInput").ap()
    wpw1 = nc.dram_tensor("wpw1", [1024, 2048], F32, kind="ExternalInput").ap()
    chan = nc.dram_tensor("chan", [128, 8 * 34], F32, kind="ExternalInput").ap()
    flag = nc.dram_tensor("flag", [128, 1], F32, kind="ExternalInput").ap()
    cst = nc.dram_tensor("cst", [128, 256], F32, kind="ExternalInput").ap()
    zT = nc.dram_tensor("zT", [1024, T], F32, kind="ExternalOutput").ap()
    K = KB(nc)
    cst_b = K.sb("cst_b", [128, 256], BF16)
    chan_sb = K.sb("chan_sb", [128, 8, 34], F32)
    flag_sb = K.sb("flag_sb", [128, 1], F32)
    G = K.sb("G", [128, 1024], F32)
    SH = K.sb("SH", [128, 1024], F32)
    tmp = K.sb("tmp", [128, 1024], F32)
    w1_sb = K.sb("w1_sb", [128, 8, 2048], BF16)
    hT_all = K.sb("hT_all", [128, 8, TH], BF16)
    vT = K.sb("vT", [128, 8, T], BF16)
    K.dma("pool", cst_b[:], cst, w=[cst_b])
    K.dma("sp", chan_sb[:], chan.rearrange("p (c n) -> p c n", c=8), w=[chan_sb])
    K.dma("sp", flag_sb[:], flag, w=[flag_sb])
    K.dma("pool", w1_sb[:], wpw1.rearrange("(kc p) n -> p kc n", p=128), w=[w1_sb])
    emit_mod_setup(K, modv, nrm, G, SH, tmp)
    ident_b = cst_b[:, 0:128]
    ones_b = cst_b[:, 128:256]
    psT = [K.ps("psT%d" % j, [128, 1024], BF16) for j in range(2)]
    psP = [K.ps("psP%d" % j, [128, 512], F32) for j in range(4)]
    psV = [K.ps("psV%d" % j, [128, 512], F32) for j in range(2)]

    K.push()
    xt = [K.sb("xt%d" % j, [128, 1024], F32) for j in range(2)]
    junk = K.sb("junk", [128, 1024], BF16)
    sm = [K.sb("sm%d" % j, [128, 8], F32) for j in range(2)]
    hf = [K.sb("hf%d" % j, [128, 1024], F32) for j in range(2)]
    hb = [K.sb("hb%d" % j, [128, 1024], BF16) for j in range(2)]
    hT_tiles = [Tile(hT_all.t, "hT_all_%d" % g) for g in range(TH // 128)]
    for g in range(TH // 128):
        j = g % 2
        K.dma("sp", xt[j][:], xh[g * 128:(g + 1) * 128, :], w=[xt[j]])
        emit_norm_T(K, xt[j], G, SH, junk, sm[j], hf[j], hb[j], psT[j], ident_b, cst_b,
                    hT_all[:, :, g * 128:(g + 1) * 128], hT_tiles[g])
    K.pop()

    K.push()
    uT = [K.sb("uT%d" % j, [128, TH], BF16) for j in range(2)]
    diag = [K.sb("diag%d" % j, [128, 31, 128], BF16) for j in range(2)]
    sgt = [K.sb("sgt%d" % j, [128, 512], F32) for j in range(2)]
    blocks = [(0, 128)] + [(128 + k * 512, 128 + (k + 1) * 512) for k in range(4)]
    for cc in range(8):
        j = cc % 2
        K.dve(lambda e: e.tensor_tensor(out=diag[j][:], in0=ident_b.unsqueeze(1).to_broadcast([128, 31, 128]),
                                        in1=chan_sb[:, cc, 0:31].unsqueeze(2).to_broadcast([128, 31, 128]),
                                        op=ALU.mult), r=[cst_b, chan_sb], w=[diag[j]])
        for tb, (t0, t1) in enumerate(blocks):
            n = t1 - t0
            pa = psP[(2 * tb) % 4]
            pg = psP[(2 * tb + 1) % 4]
            for (pp, c0) in ((pa, cc * 128), (pg, 1024 + cc * 128)):
                for kc in range(8):
                    K.pe(lambda e: e.matmul(pp[:, 0:n], w1_sb[:, kc, c0:c0 + 128], hT_all[:, kc, t0:t1],
                                            start=(kc == 0), stop=(kc == 7)), r=[w1_sb, hT_all], w=[pp], sig=(kc == 7))
            sg = sgt[tb % 2]
            K.act(lambda e: e.activation(out=sg[:, 0:n], in_=pg[:, 0:n], func=AF.Sigmoid), r=[pg], w=[sg])
            if tb == 0:
                K.dve(lambda e: e.scalar_tensor_tensor(out=uT[j][:, t0:t1], in0=pa[:, 0:n], scalar=flag_sb[:, 0:1],
                                                       in1=sg[:, 0:n], op0=ALU.mult, op1=ALU.mult),
                      r=[pa, sg, flag_sb], w=[uT[j]])
            else:
                K.dve(lambda e: e.tensor_tensor(out=uT[j][:, t0:t1], in0=pa[:, 0:n], in1=sg[:, 0:n], op=ALU.mult),
                      r=[pa, sg], w=[uT[j]])
        for tb in range(4):
            pv = psV[tb % 2]
            for jt in range(31):
                o = 128 + tb * 512 - 30 + jt
                K.pe(lambda e: e.matmul(pv[:], diag[j][:, jt, :], uT[j][:, o:o + 512],
                                        start=(jt == 0), stop=(jt == 30)), r=[diag[j], uT[j]], w=[pv], sig=(jt == 30))
            K.act(lambda e: e.activation(out=vT[:, cc, tb * 512:(tb + 1) * 512], in_=pv[:], func=AF.Identity,
                                         bias=chan_sb[:, cc, 31:32]), r=[pv, chan_sb], w=[vT])
    K.pop()

    K.push()
    sqb = [K.sb("sqb%d" % j, [128, 512], BF16) for j in range(2)]
    mu = K.sb("mu", [128, 512], F32)
    ex2 = K.sb("ex2", [128, 512], F32)
    rstd = K.sb("rstd", [128, 512], F32)
    t1b = [K.sb("t1b%d" % j, [128, 512], F32) for j in range(2)]
    t2b = [K.sb("t2b%d" % j, [128, 512], F32) for j in range(2)]
    zo = [K.sb("zo%d" % j, [128, 512], F32) for j in range(2)]
    for tb in range(4):
        S1, S2 = psP[0], psP[1]
        sl = slice(tb * 512, (tb + 1) * 512)
        for cc in range(8):
            sq = sqb[cc % 2]
            K.act(lambda e: e.activation(out=sq[:], in_=vT[:, cc, sl], func=AF.Square), r=[vT], w=[sq])
            K.pe(lambda e: e.matmul(S1[:], ones_b, vT[:, cc, sl], start=(cc == 0), stop=(cc == 7)),
                 r=[cst_b, vT], w=[S1], sig=(cc == 7))
            K.pe(lambda e: e.matmul(S2[:], ones_b, sq[:], start=(cc == 0), stop=(cc == 7)),
                 r=[cst_b, sq], w=[S2])
        K.act(lambda e: e.mul(out=mu[:], in_=S1[:], mul=1.0 / 1024), r=[S1], w=[mu])
        K.dve(lambda e: e.tensor_scalar(out=ex2[:], in0=S2[:], scalar1=1.0 / 1024, scalar2=EPS,
                                        op0=ALU.mult, op1=ALU.add), r=[S2], w=[ex2])
        K.pool(lambda e: e.tensor_tensor(out=rstd[:], in0=mu[:], in1=mu[:], op=ALU.mult), r=[mu], w=[rstd])
        K.dve(lambda e: e.tensor_tensor(out=ex2[:], in0=ex2[:], in1=rstd[:], op=ALU.subtract), r=[ex2, rstd], w=[ex2])
        K.act(lambda e: e.sqrt(out=ex2[:], in_=ex2[:]), r=[ex2], w=[ex2])
        K.dve(lambda e: e.reciprocal(out=rstd[:], in_=ex2[:]), r=[ex2], w=[rstd])
        for cc in range(8):
            j = cc % 2
            K.dve(lambda e: e.tensor_tensor(out=t1b[j][:], in0=vT[:, cc, sl], in1=mu[:], op=ALU.subtract),
                  r=[vT, mu], w=[t1b[j]])
            K.pool(lambda e: e.tensor_tensor(out=t2b[j][:], in0=t1b[j][:], in1=rstd[:], op=ALU.mult),
                   r=[t1b[j], rstd], w=[t2b[j]])
            K.act(lambda e: e.activation(out=zo[j][:], in_=t2b[j][:], func=AF.Silu, scale=chan_sb[:, cc, 32:33],
                                         bias=chan_sb[:, cc, 33:34]), r=[t2b[j], chan_sb], w=[zo[j]])
            K.dma("sp", zT[cc * 128:(cc + 1) * 128, sl], zo[j][:], r=[zo[j]])
    K.pop()
    K.finish()
    return nc


def launch_conv(x, mod_l, nrm_l, w_pw1, dw, dw_b, ln_g, ln_b):
    nc = _prog("conv", build_conv_program)
    cst = conv_consts()
    nrm = _f32(nrm_l).reshape(1, 1024)
    chan = np.zeros((128, 8, 34), np.float32)
    chan[:, :, 0:31] = dw.T.reshape(8, 128, 31).transpose(1, 0, 2)
    chan[:, :, 31] = dw_b.reshape(8, 128).T
    chan[:, :, 32] = ln_g.reshape(8, 128).T
    chan[:, :, 33] = ln_b.reshape(8, 128).T
    chan = _f32(chan.reshape(128, 8 * 34))
    w_pw1 = _f32(w_pw1)
    in_maps = []
    for c in range(NCORES):
        b, q = c // 4, c % 4
        xh = np.zeros((2048 + 128, 1024), np.float32)
        xh[128:] = x[b, q * 2048:(q + 1) * 2048]
        if q > 0:
            xh[:128] = x[b, q * 2048 - 128:q * 2048]
        in_maps.append({
            "xh": xh, "modv": _f32(mod_l[b].reshape(6, 1024)), "nrm": nrm, "wpw1": w_pw1, "chan": chan,
            "flag": np.full((128, 1), 1.0 if q > 0 else 0.0, np.float32), "cst": cst,
        })
    res = run_bass_kernel_spmd(nc, in_maps, core_ids=list(range(NCORES)))
    z = np.empty((2, 8192, 1024), np.float32)
    for c in range(NCORES):
        b, q = c // 4, c % 4
        z[b, q * 2048:(q + 1) * 2048] = res.results[c]["zT"].T
    return z


NSA_NEG = -1e30
_NC_ID, _NC_TLE, _NC_TGT, _NC_A, _NC_V0, _NC_KEEP, _NC_BIAS, _NC_END = 0, 128, 256, 384, 896, 1024, 1279, 1534


def nsa_consts():
    c = np.zeros((128, _NC_END), np.float32)
    p = np.arange(128)
    c[:, _NC_ID:_NC_ID + 128] = np.eye(128, dtype=np.float32)
    c[:, _NC_TLE:_NC_TLE + 128] = (p[:, None] <= p[None, :]).astype(np.float32)
    c[:, _NC_TGT:_NC_TGT + 128] = (p[:, None] > p[None, :]).astype(np.float32)
    A = np.zeros((512, 128), np.float32)
    wts = [1.0, 2.0, 2.0, 2.0, 1.0]
    for m in range(128):
        for o, w in enumerate(wts):
            n = 4 * m + o - 1
            if 0 <= n < 511:
                A[n, m] += w
    c[:, _NC_A:_NC_A + 512] = A.reshape(4, 128, 128).transpose(1, 0, 2).reshape(128, 512)
    c[:, _NC_V0:_NC_V0 + 128] = 16.0 * p[:, None] + 31.0 - p[None, :]
    keep = np.ones((128, 255), np.float32)
    bias = np.zeros((128, 255), np.float32)
    for q in range(128):
        hi = 1 if q >= 64 else 0
        for ui in range(255):
            u = ui - 127
            rel = u - hi
            if rel > 0:
                keep[q, ui], bias[q, ui] = 0.0, NSA_NEG
            elif rel == 0:
                keep[q, ui], bias[q, ui] = 0.0, 2e4
            elif rel == -1:
                keep[q, ui], bias[q, ui] = 0.0, 3e4
    c[:, _NC_KEEP:_NC_KEEP + 255] = keep
    c[:, _NC_BIAS:_NC_BIAS + 255] = bias
    wexp = np.zeros((128, 8192), np.float32)
    for m in range(128):
        wexp[m, m * 64:(m + 1) * 64] = 1.0
    return c, wexp


def build_nsa_program(S=8192, dbg=None):
    nc = bass.Bass("TRN2", target_bir_lowering=False)
    NTL = S // 128
    x = nc.dram_tensor("x", [S, 1024], F32, kind="ExternalInput").ap()
    modv = nc.dram_tensor("modv", [6, 1024], F32, kind="ExternalInput").ap()
    nrm = nc.dram_tensor("nrm", [1, 1024], F32, kind="ExternalInput").ap()
    wtm = nc.dram_tensor("wtm", [1024, 524], F32, kind="ExternalInput").ap()
    wfm = nc.dram_tensor("wfm", [1024, 128], F32, kind="ExternalInput").ap()
    wc = nc.dram_tensor("wc", [64, 2 * 32 * 64], F32, kind="ExternalInput").ap()
    peT = nc.dram_tensor("peT", [64, 32 * 128], F32, kind="ExternalInput").ap()
    gn = nc.dram_tensor("gn", [128, 448], F32, kind="ExternalInput").ap()
    cst = nc.dram_tensor("cst", [128, _NC_END], F32, kind="ExternalInput").ap()
    wexp = nc.dram_tensor("wexp", [128, 8192], F32, kind="ExternalInput").ap()
    ao = nc.dram_tensor("ao", [S, 128], F32, kind="ExternalOutput").ap()
    K = KB(nc)
    cst_f = K.sb("cst_f", [128, _NC_END], F32)
    cst_b = K.sb("cst_b", [128, _NC_V0], BF16)
    wexp_b = K.sb("wexp_b", [128, S], BF16)
    G = K.sb("G", [128, 1024], F32)
    SH = K.sb("SH", [128, 1024], F32)
    tmp = K.sb("tmp", [128, 1024], F32)
    wtm_sb = K.sb("wtm_sb", [128, 8, 524], BF16)
    wfm_sb = K.sb("wfm_sb", [128, 8, 128], BF16)
    wc_sb = K.sb("wc_sb", [64, 2, 32, 64], BF16)
    pe_sb = K.sb("pe_sb", [64, 32, 128], BF16)
    gn_sb = K.sb("gn_sb", [128, 448], F32)
    cpe_rep = K.sb("cpe_rep", [128, 128], F32)
    ksT = K.sb("ksT", [64, S], BF16)
    kwT = K.sb("kwT", [64, S], BF16)
    NCT = (S // 16 - 1 + 127) // 128
    kvcT = K.sb("kvcT", [64, max(S, NCT * 2048) + 32], BF16)
    vvcT = K.sb("vvcT", [64, max(S, NCT * 2048) + 32], BF16)
    kcT = K.sb("kcT", [64, 512], BF16)
    vs_aug = K.sb("vs_aug", [128, NTL, 65], BF16)
    vw_aug = K.sb("vw_aug", [128, NTL, 65], BF16)
    Rc = K.sb("Rc", [128, 4, 193], BF16)
    K.dma("sp", cst_f[:], cst, w=[cst_f])
    K.dma("pool", cst_b[:], cst[:, 0:_NC_V0], w=[cst_b])
    K.dma("pool", wexp_b[:], wexp[:, 0:S], w=[wexp_b])
    with nc.allow_non_contiguous_dma(reason="weight slices"):
        K.dma("pool", wtm_sb[:], wtm.rearrange("(kc p) n -> p kc n", p=128), w=[wtm_sb])
        K.dma("pool", wfm_sb[:], wfm.rearrange("(kc p) n -> p kc n", p=128), w=[wfm_sb])
    K.dma("pool", wc_sb[:], wc.rearrange("p (h l e) -> p h l e", h=2, l=32), w=[wc_sb])
    K.dma("pool", pe_sb[:], peT.rearrange("p (l n) -> p l n", l=32), w=[pe_sb])
    K.dma("sp", gn_sb[:], gn, w=[gn_sb])
    emit_mod_setup(K, modv, nrm, G, SH, tmp)
    ident_b = cst_b[:, _NC_ID:_NC_ID + 128]
    tle_b = cst_b[:, _NC_TLE:_NC_TLE + 128]
    tgt_b = cst_b[:, _NC_TGT:_NC_TGT + 128]
    V0 = cst_f[:, _NC_V0:_NC_V0 + 128]
    K.dve(lambda e: e.memset(kvcT[:], 0.0), w=[kvcT])
    K.dve(lambda e: e.memset(vvcT[:], 0.0), w=[vvcT])
    K.dve(lambda e: e.memset(kcT[:], 0.0), w=[kcT])
    K.dve(lambda e: e.memset(vs_aug[:], 1.0), w=[vs_aug])
    K.dve(lambda e: e.memset(vw_aug[:], 1.0), w=[vw_aug])
    K.dve(lambda e: e.memset(Rc[:], 1.0), w=[Rc])
    K.dve(lambda e: e.tensor_copy(out=Rc[:, :, 65:193], in_=cst_b[:, _NC_A:_NC_A + 512].rearrange("p (c m) -> p c m", c=4)),
          r=[cst_b], w=[Rc])

    psT = [K.ps("psT%d" % j, [128, 1024], BF16) for j in range(2)]
    B = [K.ps("B%d" % j, [128, 512], F32) for j in range(6)]

    for half in range(2):
        for l in range(32):
            K.pe(lambda e: e.matmul(B[1][:, half * 64:(half + 1) * 64], pe_sb[:, l, :], wc_sb[:, half, l, :],
                                    start=(l == 0), stop=(l == 31)), r=[pe_sb, wc_sb], w=[B[1]], sig=(l == 31))
    K.dve(lambda e: e.tensor_copy(out=cpe_rep[:], in_=B[1][:, 0:128]), r=[B[1]], w=[cpe_rep])

    xt = [K.sb("xt%d" % j, [128, 1024], F32) for j in range(2)]
    junk = K.sb("junk", [128, 1024], BF16)
    sm = [K.sb("sm%d" % j, [128, 8], F32) for j in range(2)]
    hf = [K.sb("hf%d" % j, [128, 1024], F32) for j in range(2)]
    hb = [K.sb("hb%d" % j, [128, 1024], BF16) for j in range(2)]
    hT = [K.sb("hT%d" % j, [128, 8, 128], BF16) for j in range(2)]
    sq = K.sb("sq", [128, 384], F32)
    qkn = K.sb("qkn", [128, 384], BF16)
    st = [K.sb("st%d" % j, [128, 80], F32) for j in range(2)]
    qT = [K.sb("qT%d" % j, [64, 512], BF16) for j in range(2)]
    kcn = K.sb("kcn", [128, 64], F32)
    kcb = K.sb("kcb", [128, 64], BF16)
    ec = [K.sb("ec%d" % j, [128, 512], BF16) for j in range(2)]
    es = [K.sb("es%d" % j, [128, 256], BF16) for j in range(8)]
    mk = [K.sb("mk%d" % j, [128, 128], BF16) for j in range(2)]
    imp = K.sb("imp", [128, 128], F32)
    score = K.sb("score", [128, 128], F32)
    sc2 = K.sb("sc2", [128, 128], F32)
    sel = K.sb("sel", [128, 128], BF16)
    selT = K.sb("selT", [128, 128], BF16)
    oacc = [K.sb("oacc%d" % j, [128, 128], F32) for j in range(2)]
    kvT_tiles = [Tile(kvcT.t, "kvcT%d" % g) for g in range(NTL)]
    vvT_tiles = [Tile(vvcT.t, "vvcT%d" % g) for g in range(NTL)]
    ks_tiles = [Tile(ksT.t, "ksT%d" % g) for g in range(NTL)]
    kw_tiles = [Tile(kwT.t, "kwT%d" % g) for g in range(NTL)]
    vs_tiles = [Tile(vs_aug.t, "vs%d" % g) for g in range(NTL)]
    vw_tiles = [Tile(vw_aug.t, "vw%d" % g) for g in range(NTL)]
    kc_tiles = [Tile(kcT.t, "kc%d" % g) for g in range(4)]
    rc_tiles = [Tile(Rc.t, "rc%d" % g) for g in range(4)]
    for t_ in vvT_tiles + kvT_tiles + ks_tiles + kw_tiles + vs_tiles + vw_tiles + kc_tiles + rc_tiles:
        t_.w = (K.engs["dve"].sid, K.engs["dve"].count)
    nsl = [0]

    def ck(k, i):
        if dbg is not None and dbg == k:
            raise _Stop()
    if True:
     def P(i):
         j = i % 2
         S_ = st[j]
         sl = slice(i * 128, (i + 1) * 128)
         K.dma("sp", xt[j][:], x[sl, :], w=[xt[j]])
         emit_norm_T(K, xt[j], G, SH, junk, sm[j], hf[j], hb[j], psT[0], ident_b, cst_b, hT[j][:], hT[j])
         pm1, pm2 = B[4], B[1]
         for kc in range(8):
             K.pe(lambda e: e.matmul(pm1[:], hT[j][:, kc, :], wtm_sb[:, kc, 0:512], start=(kc == 0), stop=(kc == 7)),
                  r=[hT[j], wtm_sb], w=[pm1], sig=(kc == 7))
         for kc in range(8):
             K.pe(lambda e: e.matmul(pm2[:, 0:12], hT[j][:, kc, :], wtm_sb[:, kc, 512:524], start=(kc == 0), stop=(kc == 7)),
                  r=[hT[j], wtm_sb], w=[pm2], sig=False)
         for half in range(2):
             for kc in range(8):
                 K.pe(lambda e: e.matmul(pm2[0:64, 128 + half * 128:256 + half * 128], wfm_sb[:, kc, half * 64:(half + 1) * 64],
                                         hT[j][:, kc, :], start=(kc == 0), stop=(kc == 7)),
                      r=[hT[j], wfm_sb], w=[pm2], sig=(kc == 7 and half == 1))
         K.dve(lambda e: e.tensor_copy(out=kvcT[:, sl], in_=pm2[0:64, 128:256]), r=[pm2], w=[kvT_tiles[i]])
         K.dve(lambda e: e.tensor_copy(out=vvcT[:, sl], in_=pm2[0:64, 256:384]), r=[pm2], w=[vvT_tiles[i]])
         K.dve(lambda e: e.tensor_copy(out=S_[:, 0:12], in_=pm2[:, 0:12]), r=[pm2], w=[S_])
         K.act(lambda e: e.activation(out=S_[:, 0:12], in_=S_[:, 0:12], func=AF.Sigmoid), r=[S_], w=[S_])
         K.dve(lambda e: e.tensor_copy(out=vs_aug[:, i, 0:64], in_=pm1[:, 384:448]), r=[pm1], w=[vs_tiles[i]])
         K.dve(lambda e: e.tensor_copy(out=vw_aug[:, i, 0:64], in_=pm1[:, 448:512]), r=[pm1], w=[vw_tiles[i]])
         K.act(lambda e: e.activation(out=sq[:], in_=pm1[:, 0:384], func=AF.Square), r=[pm1], w=[sq])
         K.dve(lambda e: e.reduce_sum(out=S_[:, 16:22], in_=sq[:].rearrange("p (s d) -> p s d", s=6), axis=AX.X),
               r=[sq], w=[S_])
         K.dve(lambda e: e.tensor_scalar(out=S_[:, 16:22], in0=S_[:, 16:22], scalar1=1.0 / 64, scalar2=EPS,
                                         op0=ALU.mult, op1=ALU.add), r=[S_], w=[S_])
         K.act(lambda e: e.sqrt(out=S_[:, 16:22], in_=S_[:, 16:22]), r=[S_], w=[S_])
         K.dve(lambda e: e.reciprocal(out=S_[:, 16:22], in_=S_[:, 16:22]), r=[S_], w=[S_])
         K.dve(lambda e: e.tensor_tensor(out=sq[:].rearrange("p (s d) -> p s d", s=6),
                                         in0=pm1[:, 0:384].rearrange("p (s d) -> p s d", s=6),
                                         in1=S_[:, 16:22].unsqueeze(2).to_broadcast([128, 6, 64]), op=ALU.mult),
               r=[pm1, S_], w=[sq])
         K.dve(lambda e: e.tensor_tensor(out=qkn[:], in0=sq[:], in1=gn_sb[:, 0:384], op=ALU.mult),
               r=[sq, gn_sb], w=[qkn])
         pT = psT[1]
         for s6 in range(6):
             K.pe(lambda e: e.transpose(pT[0:64, s6 * 128:(s6 + 1) * 128], qkn[:, s6 * 64:(s6 + 1) * 64], ident_b),
                  r=[qkn, cst_b], w=[pT], sig=(s6 == 5))
         K.dve(lambda e: e.tensor_copy(out=qT[j][:], in_=pT[0:64, 0:512]), r=[pT], w=[qT[j]])
         K.dve(lambda e: e.tensor_copy(out=ksT[:, sl], in_=pT[0:64, 512:640]), r=[pT], w=[ks_tiles[i]])
         K.dve(lambda e: e.tensor_copy(out=kwT[:, sl], in_=pT[0:64, 640:768]), r=[pT], w=[kw_tiles[i]])
         chi = (8 * i + 6) // 128
         ctiles = [chi] if (i % 16 != 0 or i == 0) else [chi - 1, chi]
         first_tok_tile = lambda c: c * 16
         for c in ctiles:
             rdeps = kvT_tiles[c * 16:min(NTL, c * 16 + 17)] + vvT_tiles[c * 16:min(NTL, c * 16 + 17)]
             pk, pv = B[1], B[4]
             for (pp, srcT, half) in ((pk, kvcT, 0), (pv, vvcT, 1)):
                 for l in range(32):
                     src = srcT[:, c * 2048 + l:c * 2048 + l + 2033:16]
                     K.pe(lambda e: e.matmul(pp[:, 0:64], src, wc_sb[:, half, l, :], start=(l == 0), stop=(l == 31)),
                          r=rdeps + [wc_sb], w=[pp], sig=(l == 31))
             K.dve(lambda e: e.tensor_tensor(out=Rc[:, c, 0:64], in0=pv[:, 0:64], in1=cpe_rep[:, 64:128], op=ALU.add),
                   r=[pv, cpe_rep], w=[rc_tiles[c]])
             K.dve(lambda e: e.tensor_tensor(out=kcn[:], in0=pk[:, 0:64], in1=cpe_rep[:, 0:64], op=ALU.add),
                   r=[pk, cpe_rep], w=[kcn])
             K.act(lambda e: e.activation(out=junk[:, 0:64], in_=kcn[:], func=AF.Square, accum_out=S_[:, 24:25]),
                   r=[kcn], w=[junk, S_])
             K.dve(lambda e: e.tensor_scalar(out=S_[:, 24:25], in0=S_[:, 24:25], scalar1=1.0 / 64, scalar2=EPS,
                                             op0=ALU.mult, op1=ALU.add), r=[S_], w=[S_])
             K.act(lambda e: e.sqrt(out=S_[:, 24:25], in_=S_[:, 24:25]), r=[S_], w=[S_])
             K.dve(lambda e: e.reciprocal(out=S_[:, 24:25], in_=S_[:, 24:25]), r=[S_], w=[S_])
             K.dve(lambda e: e.scalar_tensor_tensor(out=kcb[:], in0=kcn[:], scalar=S_[:, 24:25], in1=gn_sb[:, 384:448],
                                                    op0=ALU.mult, op1=ALU.mult), r=[kcn, S_, gn_sb], w=[kcb])
             K.pe(lambda e: e.transpose(pT[0:64, 768:896], kcb[:], ident_b), r=[kcb, cst_b], w=[pT])
             K.dve(lambda e: e.tensor_copy(out=kcT[:, c * 128:(c + 1) * 128], in_=pT[0:64, 768:896]), r=[pT], w=[kc_tiles[c]])
     def C(i):
         j = i % 2
         S_ = st[j]
         chi = (8 * i + 6) // 128
         pT = psT[1]
         pO = (B[3], B[5])
         cvalid = []
         for c in range(chi + 1):
             thr = 128 * i - 2048 * c
             if thr < -96:
                 continue
             cvalid.append((c, thr))
         for ci, (c, thr) in enumerate(cvalid):
             pS = B[2]
             K.pe(lambda e: e.matmul(pS[:], kcT[:, c * 128:(c + 1) * 128], qT[j][:], start=True, stop=True),
                  r=[kc_tiles[c], qT[j]], w=[pS])
             e_ = ec[ci % 2]
             K.act(lambda e: e.activation(out=e_[:], in_=pS[:], func=AF.Exp, scale=0.125), r=[pS], w=[e_])
             if thr < 2063:
                 K.dve(lambda e: e.scalar_tensor_tensor(
                     out=e_[:].rearrange("p (h q) -> p h q", h=4), in0=V0.unsqueeze(1).to_broadcast([128, 4, 128]),
                     scalar=float(thr), in1=e_[:].rearrange("p (h q) -> p h q", h=4), op0=ALU.is_le, op1=ALU.mult),
                     r=[cst_f, e_], w=[e_])
             for h4 in range(4):
                 po = pO[h4 // 2]
                 o0 = (h4 % 2) * 193
                 K.pe(lambda e: e.matmul(po[:, o0:o0 + 193], e_[:, h4 * 128:(h4 + 1) * 128], Rc[:, c, :],
                                         start=(ci == 0 and h4 % 2 == 0), stop=(ci == len(cvalid) - 1 and h4 % 2 == 1)),
                      r=[e_, rc_tiles[c]], w=[po], sig=(h4 % 2 == 1))
         oa = oacc[j]
         if cvalid:
             for h4 in range(4):
                 po = pO[h4 // 2]
                 o0 = (h4 % 2) * 193
                 K.dve(lambda e: e.tensor_scalar(out=S_[:, 32 + h4:33 + h4], in0=po[:, o0 + 64:o0 + 65], scalar1=1e-30,
                                                 scalar2=None, op0=ALU.max), r=[po], w=[S_])
             K.dve(lambda e: e.reciprocal(out=S_[:, 32:36], in_=S_[:, 32:36]), r=[S_], w=[S_])
             for h4 in range(4):
                 po = pO[h4 // 2]
                 o0 = (h4 % 2) * 193
                 if h4 == 0:
                     K.dve(lambda e: e.tensor_scalar(out=imp[:], in0=po[:, o0 + 65:o0 + 193], scalar1=S_[:, 32:33],
                                                     scalar2=None, op0=ALU.mult), r=[po, S_], w=[imp])
                 else:
                     K.dve(lambda e: e.scalar_tensor_tensor(out=imp[:], in0=po[:, o0 + 65:o0 + 193],
                                                            scalar=S_[:, 32 + h4:33 + h4], in1=imp[:],
                                                            op0=ALU.mult, op1=ALU.add), r=[po, S_, imp], w=[imp])
             for h2 in range(2):
                 K.dve(lambda e: e.tensor_tensor(out=S_[:, 36 + h2:37 + h2], in0=S_[:, 32 + h2:33 + h2],
                                                 in1=S_[:, 3 * h2:3 * h2 + 1], op=ALU.mult), r=[S_], w=[S_])
                 K.dve(lambda e: e.tensor_scalar(out=oa[:, h2 * 64:(h2 + 1) * 64], in0=pO[0][:, h2 * 193:h2 * 193 + 64],
                                                 scalar1=S_[:, 36 + h2:37 + h2], scalar2=None, op0=ALU.mult),
                       r=[pO[0], S_], w=[oa])
         else:
             K.dve(lambda e: e.memset(imp[:], 0.0), w=[imp])
             K.dve(lambda e: e.memset(oa[:], 0.0), w=[oa])
         u0 = 127 - 2 * i
         K.dve(lambda e: e.tensor_tensor(out=score[:], in0=imp[:], in1=cst_f[:, _NC_KEEP + u0:_NC_KEEP + u0 + 128],
                                         op=ALU.mult), r=[imp, cst_f], w=[score])
         K.dve(lambda e: e.tensor_tensor(out=score[:], in0=score[:], in1=cst_f[:, _NC_BIAS + u0:_NC_BIAS + u0 + 128],
                                         op=ALU.add), r=[score, cst_f], w=[score])
         K.dve(lambda e: e.memset(score[:, 0:1], 1e4), w=[score])
         K.dve(lambda e: e.max(out=S_[:, 40:48], in_=score[:]), r=[score], w=[S_])
         K.dve(lambda e: e.match_replace(out=sc2[:], in_to_replace=S_[:, 40:48], in_values=score[:], imm_value=-3e38),
               r=[score, S_], w=[sc2])
         K.dve(lambda e: e.max(out=S_[:, 48:56], in_=sc2[:]), r=[sc2], w=[S_])
         K.dve(lambda e: e.tensor_scalar(out=S_[:, 56:57], in0=S_[:, 55:56], scalar1=-1e29, scalar2=None, op0=ALU.max),
               r=[S_], w=[S_])
         K.dve(lambda e: e.tensor_scalar(out=sel[:], in0=score[:], scalar1=S_[:, 56:57], scalar2=None, op0=ALU.is_ge),
               r=[score, S_], w=[sel])
         K.pe(lambda e: e.transpose(pT[:, 896:1024], sel[:], ident_b), r=[sel, cst_b], w=[pT])
         K.act(lambda e: e.copy(out=selT[:], in_=pT[:, 896:1024]), r=[pT], w=[selT])
     def J(i, mid):
         j = i % 2
         S_ = st[j]
         sl = slice(i * 128, (i + 1) * 128)
         oa = oacc[j]
         pO2 = B[0]
         qown = qT[j][:, 0:256]
         jobs = [("s", c) for c in range(i + 1)] + [("w", c) for c in range(max(0, i - 4), i + 1)]
         ns = i + 1
         nw = i + 1 - max(0, i - 4)
         cnt = {"s": 0, "w": 0}
         sbanks = (B[2], B[5], B[3])
         LOOK = 2

         def emit_A(kind, c, slot):
             pS2 = sbanks[slot % len(sbanks)]
             kt = ks_tiles[c] if kind == "s" else kw_tiles[c]
             kk = ksT if kind == "s" else kwT
             K.pe(lambda e: e.matmul(pS2[:, 0:256], kk[:, c * 128:(c + 1) * 128], qown, start=True, stop=True),
                  r=[kt, qT[j]], w=[pS2], sig=(kind == "w"))
             e2 = es[slot % 8]
             if kind == "s":
                 K.pe(lambda e: e.matmul(pS2[:, 256:384], wexp_b[:, c * 128:(c + 1) * 128], selT[:], start=True, stop=True),
                      r=[wexp_b, selT], w=[pS2])
             K.act(lambda e: e.activation(out=e2[:], in_=pS2[:, 0:256], func=AF.Exp, scale=0.125), r=[pS2], w=[e2])
             e3 = e2[:].rearrange("p (h q) -> p h q", h=2)
             if kind == "s":
                 if c == i:
                     m_ = mk[slot % 2]
                     K.dve(lambda e: e.tensor_tensor(out=m_[:], in0=pS2[:, 256:384], in1=tle_b, op=ALU.mult),
                           r=[pS2, cst_b], w=[m_])
                     K.dve(lambda e: e.tensor_tensor(out=e3, in0=e3, in1=m_[:].unsqueeze(1).to_broadcast([128, 2, 128]),
                                                     op=ALU.mult), r=[e2, m_], w=[e2])
                 else:
                     K.dve(lambda e: e.tensor_tensor(out=e3, in0=e3,
                                                     in1=pS2[:, 256:384].unsqueeze(1).to_broadcast([128, 2, 128]),
                                                     op=ALU.mult), r=[e2, pS2], w=[e2])
             else:
                 mm = None
                 if c == i:
                     mm = tle_b
                 elif c == i - 4:
                     mm = tgt_b
                 if mm is not None:
                     K.dve(lambda e: e.tensor_tensor(out=e3, in0=e3, in1=mm.unsqueeze(1).to_broadcast([128, 2, 128]),
                                                     op=ALU.mult), r=[e2, cst_b], w=[e2])

         def emit_B(kind, c, slot):
             e2 = es[slot % 8]
             va = vs_aug if kind == "s" else vw_aug
             vt = vs_tiles[c] if kind == "s" else vw_tiles[c]
             ob = 0 if kind == "s" else 256
             n_ = ns if kind == "s" else nw
             for h2 in range(2):
                 K.pe(lambda e: e.matmul(pO2[:, ob + h2 * 65:ob + h2 * 65 + 65], e2[:, h2 * 128:(h2 + 1) * 128], va[:, c, :],
                                         start=(kind == "s" and cnt[kind] == 0 and h2 == 0),
                                         stop=(kind == "w" and cnt[kind] == n_ - 1 and h2 == 1)),
                      r=[e2, vt], w=[pO2], sig=(h2 == 1))
             cnt[kind] += 1

         pend = []
         for jn, (kind, c) in enumerate(jobs):
             if jn == min(2, len(jobs) - 1) and mid is not None:
                 mid()
             emit_A(kind, c, nsl[0])
             pend.append((kind, c, nsl[0]))
             nsl[0] += 1
             if len(pend) > LOOK:
                 emit_B(*pend.pop(0))
         while pend:
             emit_B(*pend.pop(0))
         for bi, ob in ((1, 0), (2, 256)):
             for h2 in range(2):
                 cidx = 60 + bi * 2 + h2
                 K.dve(lambda e: e.tensor_scalar(out=S_[:, cidx:cidx + 1], in0=pO2[:, ob + h2 * 65 + 64:ob + h2 * 65 + 65],
                                                 scalar1=1e-30, scalar2=None, op0=ALU.max), r=[pO2], w=[S_])
                 K.dve(lambda e: e.reciprocal(out=S_[:, cidx:cidx + 1], in_=S_[:, cidx:cidx + 1]), r=[S_], w=[S_])
                 K.dve(lambda e: e.tensor_tensor(out=S_[:, cidx:cidx + 1], in0=S_[:, cidx:cidx + 1],
                                                 in1=S_[:, 3 * h2 + bi:3 * h2 + bi + 1], op=ALU.mult), r=[S_], w=[S_])
                 K.dve(lambda e: e.scalar_tensor_tensor(out=oa[:, h2 * 64:(h2 + 1) * 64],
                                                        in0=pO2[:, ob + h2 * 65:ob + h2 * 65 + 64],
                                                        scalar=S_[:, cidx:cidx + 1], in1=oa[:, h2 * 64:(h2 + 1) * 64],
                                                        op0=ALU.mult, op1=ALU.add), r=[pO2, S_, oa], w=[oa])
         K.dma("sp", ao[sl, :], oa[:], r=[oa])
     P(0)
     C(0)
     for i in range(NTL):
         J(i, (lambda i=i: P(i + 1)) if i + 1 < NTL else None)
         if i + 1 < NTL:
             C(i + 1)
    K.finish()
    return nc


def launch_nsa(x, mod_l, nrm_l, w_in, w_ck, w_cv, pe, q_gain, k_gain, S=8192, dbg=None):
    nc = _prog("nsa%d_%s" % (S, dbg), lambda: build_nsa_program(S, dbg))
    cst, wexp = nsa_consts()
    nrm = _f32(nrm_l).reshape(1, 1024)
    wc = np.zeros((64, 2, 32, 64), np.float32)
    wc[:, 0] = w_ck.reshape(32, 64, 64).transpose(1, 0, 2)
    wc[:, 1] = w_cv.reshape(32, 64, 64).transpose(1, 0, 2)
    wc = _f32(wc.reshape(64, 4096))
    peT = _f32(np.repeat(pe.T[:, :, None], 128, axis=2).reshape(64, 4096))
    gn = np.zeros((128, 448), np.float32)
    gn[:, 0:256] = np.tile(q_gain, 4)[None, :]
    gn[:, 256:320] = k_gain[1][None, :]
    gn[:, 320:384] = k_gain[2][None, :]
    gn[:, 384:448] = k_gain[0][None, :]
    in_maps = []
    for c in range(NCORES):
        b, g, hh = c // 4, (c // 2) % 2, c % 2
        heads = [g * 4 + 2 * hh, g * 4 + 2 * hh + 1, g * 4 + 2 * (1 - hh), g * 4 + 2 * (1 - hh) + 1]
        qcols = np.concatenate([w_in[:, h * 64:(h + 1) * 64] for h in heads], axis=1)
        kv = lambda slot: w_in[:, 512 + slot * 128 + g * 64: 512 + slot * 128 + (g + 1) * 64]
        gcols = np.concatenate([w_in[:, 1280 + h * 3:1280 + h * 3 + 3] for h in heads[:2]] +
                               [w_in[:, 1280 + h * 3:1280 + h * 3 + 3] for h in heads[2:]], axis=1)
        wtm = np.concatenate([qcols, kv(2), kv(4), kv(3), kv(5), gcols], axis=1)
        wfm = np.concatenate([kv(0), kv(1)], axis=1)
        in_maps.append({"x": _f32(x[b, :S]), "modv": _f32(mod_l[b].reshape(6, 1024)), "nrm": nrm,
                        "wtm": _f32(wtm), "wfm": _f32(wfm), "wc": wc, "peT": peT, "gn": gn, "cst": cst, "wexp": wexp})
    res = run_bass_kernel_spmd(nc, in_maps, core_ids=list(range(NCORES)))
    a = np.empty((2, S, 512), np.float32)
    for c in range(NCORES):
        b, g, hh = c // 4, (c // 2) % 2, c % 2
        h0 = g * 4 + 2 * hh
        a[b, :, h0 * 64:(h0 + 2) * 64] = res.results[c]["ao"]
    return a


def kernel(x, c, ada_w, ada_b, norm_mix, norm_ffn, mix_w_in, mix_w_out, nsa_cmp_wk, nsa_cmp_wv, nsa_cmp_pe,
           nsa_q_gain, nsa_k_gain, hgrn_lb_logits, hgrn_o_gain, conv_w_pw1, conv_dw, conv_dw_b, conv_ln_g,
           conv_ln_b, conv_w_pw2, moe_w_group, moe_w_expert, moe_w1, moe_w3, moe_w2):
    x = np.asarray(x, dtype=np.float32)
    mod = launch_mod(np.asarray(c), np.asarray(ada_w), np.asarray(ada_b))
    for layer in range(4):
        i = layer // 2
        if layer % 2 == 0:
            a = launch_nsa(x, mod[layer], norm_mix[layer], np.asarray(mix_w_in[i]), np.asarray(nsa_cmp_wk[i]),
                           np.asarray(nsa_cmp_wv[i]), np.asarray(nsa_cmp_pe[i]), np.asarray(nsa_q_gain[i]),
                           np.asarray(nsa_k_gain[i]))
            r = launch_hgrn(x, mod[layer], norm_mix[layer], np.asarray(mix_w_in[i]), np.asarray(hgrn_lb_logits),
                            np.asarray(hgrn_o_gain[i]), i)
            mix = np.concatenate([a, r], axis=-1)
            w_o = mix_w_out[i]
        else:
            mix = launch_conv(x, mod[layer], norm_mix[layer], np.asarray(conv_w_pw1[i]), np.asarray(conv_dw[i]),
                              np.asarray(conv_dw_b[i]), np.asarray(conv_ln_g[i]), np.asarray(conv_ln_b[i]))
            w_o = conv_w_pw2[i]
        x = launch_moe(x, mix, np.asarray(w_o), mod[layer], norm_ffn[layer], np.asarray(moe_w_group[layer]),
                       np.asarray(moe_w_expert[layer]), np.asarray(moe_w1[layer]), np.asarray(moe_w3[layer]),
                       np.asarray(moe_w2[layer]))
    return x
```
